# Optimizing a Trainium2 kernel written in Bass

```python
import math
import jax
import jax.numpy as jnp
from jax import lax
import numpy as np

D_MODEL = 1024
BATCH = 2
SEQ = 8192
DEPTH = 1

CHUNK = 64
EPS = 1e-6

A_HEADS = 8
A_DK = 128
A_DV = 128
A_QK = A_HEADS * A_DK
A_V = A_HEADS * A_DV
CONV_W = 4
CONV_CH = 2 * A_QK + A_V

B_HEADS = 8
B_DK = 128
B_DV = 128
B_K = B_HEADS * B_DK
B_V = B_HEADS * B_DV

IN_SPLITS = (A_QK, A_QK, A_V, A_HEADS, A_HEADS, A_V, B_K, B_V, B_K, B_V, D_MODEL, D_MODEL)
IN_WIDTH = 3 * A_QK + 2 * A_V + 2 * A_HEADS + 2 * B_K + 2 * B_V + 2 * D_MODEL

N_KEYS = 128
N_EXPERTS = N_KEYS * N_KEYS
P_HEADS = 8
P_DKEY = 256
P_DHALF = P_DKEY // 2
P_TOPK = 16
P_TOKEN_BLOCK = 128

kernel_name = 'hybrid_gdn_hgrn2_peer_block'


def _rmsnorm(x, g):
    xf = x.astype(jnp.float32)
    y = xf * lax.rsqrt(jnp.mean(xf * xf, axis=-1, keepdims=True) + EPS) * g.astype(jnp.float32)
    return y.astype(x.dtype)


def _l2norm(x):
    return x * lax.rsqrt(jnp.sum(x * x, axis=-1, keepdims=True) + EPS)


def _split_cols(p):
    outs = []
    off = 0
    for w in IN_SPLITS:
        outs.append(p[..., off:off + w])
        off += w
    return outs


def _causal_conv_silu(x, w):
    k_w = w.shape[0]
    t = x.shape[1]
    xp = jnp.pad(x, ((0, 0), (k_w - 1, 0), (0, 0)))
    y = sum(xp[:, j:j + t] * w[j] for j in range(k_w))
    return jax.nn.silu(y)


def _to_chunks(x):
    b, t, h, d = x.shape
    return x.reshape(b, t // CHUNK, CHUNK, h, d).transpose(1, 0, 3, 2, 4)


def _to_chunks_s(x):
    b, t, h = x.shape
    return x.reshape(b, t // CHUNK, CHUNK, h).transpose(1, 0, 3, 2)


def _from_chunks(o):
    n, b, h, c, d = o.shape
    return o.transpose(1, 0, 3, 2, 4).reshape(b, n * c, h, d)


def _gated_delta_rule(q, k, v, beta, g):
    dk = q.shape[-1]
    dv = v.shape[-1]
    q = _l2norm(q) * (dk ** -0.5)
    k = _l2norm(k)
    qc, kc, vc = _to_chunks(q), _to_chunks(k), _to_chunks(v)
    bc = _to_chunks_s(beta)
    G = jnp.cumsum(_to_chunks_s(g), axis=-1)
    causal = jnp.tril(jnp.ones((CHUNK, CHUNK), dtype=bool))
    diff = G[..., :, None] - G[..., None, :]
    decay = jnp.where(causal, jnp.exp(jnp.where(causal, diff, 0.0)), 0.0)
    kb = kc * bc[..., None]
    a_kk = jnp.einsum('nbhid,nbhjd->nbhij', kb, kc) * decay
    lhs = jnp.eye(CHUNK, dtype=jnp.float32) + jnp.tril(a_kk, -1)
    rhs = jnp.concatenate([vc * bc[..., None], kb * jnp.exp(G)[..., None]], axis=-1)
    sol = lax.linalg.triangular_solve(lhs, rhs, left_side=True, lower=True, unit_diagonal=True)
    u, w = sol[..., :dv], sol[..., dv:]
    a_qk = jnp.einsum('nbhid,nbhjd->nbhij', qc, kc) * decay
    q_dec = qc * jnp.exp(G)[..., None]
    k_dec = kc * jnp.exp(G[..., -1:] - G)[..., None]
    g_last = jnp.exp(G[..., -1])

    def step(S, inp):
        u_n, w_n, aqk_n, qd_n, kd_n, gl_n = inp
        v_new = u_n - jnp.einsum('bhcd,bhde->bhce', w_n, S)
        o = jnp.einsum('bhcd,bhde->bhce', qd_n, S) + jnp.einsum('bhij,bhje->bhie', aqk_n, v_new)
        S = S * gl_n[..., None, None] + jnp.einsum('bhcd,bhce->bhde', kd_n, v_new)
        return S, o

    s0 = jnp.zeros((q.shape[0], q.shape[2], dk, dv), jnp.float32)
    _, o = lax.scan(step, s0, (u, w, a_qk, q_dec, k_dec, g_last))
    return _from_chunks(o)


def _hgrn2_recurrence(q, k, v, log_f):
    dk = q.shape[-1]
    dv = v.shape[-1]
    qc, kc, vc = _to_chunks(q), _to_chunks(k), _to_chunks(v)
    bc = jnp.cumsum(_to_chunks(log_f), axis=-2)
    causal3 = jnp.tril(jnp.ones((CHUNK, CHUNK), dtype=bool))[:, :, None]

    def step(S, inp):
        q_n, k_n, v_n, b_n = inp
        diff = b_n[..., :, None, :] - b_n[..., None, :, :]
        dec = jnp.where(causal3, jnp.exp(jnp.where(causal3, diff, 0.0)), 0.0)
        a = jnp.einsum('bhid,bhjd,bhijd->bhij', q_n, k_n, dec)
        o = jnp.einsum('bhid,bhde->bhie', q_n * jnp.exp(b_n), S) + jnp.einsum('bhij,bhje->bhie', a, v_n)
        b_last = b_n[..., -1:, :]
        S = S * jnp.exp(b_last)[..., 0, :, None] + jnp.einsum('bhjd,bhje->bhde', k_n * jnp.exp(b_last - b_n), v_n)
        return S, o

    s0 = jnp.zeros((q.shape[0], q.shape[2], dk, dv), jnp.float32)
    _, o = lax.scan(step, s0, (qc, kc, vc, bc))
    return _from_chunks(o)


def _peer(x, w_pq, sub_keys, expert_u, expert_v):
    b, t, d = x.shape
    n_tok = b * t
    xf = x.reshape(n_tok, d)
    q = (xf @ w_pq).reshape(n_tok, P_HEADS, 2, P_DHALF)
    s = jnp.einsum('thpd,phkd->thpk', q, sub_keys)
    top_s, top_i = lax.top_k(s, P_TOPK)
    cand_s = (top_s[:, :, 0, :, None] + top_s[:, :, 1, None, :]).reshape(n_tok, P_HEADS, P_TOPK * P_TOPK)
    cand_i = (top_i[:, :, 0, :, None] * N_KEYS + top_i[:, :, 1, None, :]).reshape(n_tok, P_HEADS, P_TOPK * P_TOPK)
    best_s, pos = lax.top_k(cand_s, P_TOPK)
    idx = jnp.take_along_axis(cand_i, pos, axis=-1)
    gate = jax.nn.softmax(best_s.astype(jnp.float32), axis=-1).astype(x.dtype)
    n_blk = n_tok // P_TOKEN_BLOCK
    xb = xf.reshape(n_blk, P_TOKEN_BLOCK, d)
    ib = idx.reshape(n_blk, P_TOKEN_BLOCK, P_HEADS * P_TOPK)
    gb = gate.reshape(n_blk, P_TOKEN_BLOCK, P_HEADS * P_TOPK)

    def block(args):
        xt, it, gt = args
        u = expert_u[it]
        h = jax.nn.gelu(jnp.einsum('td,tkd->tk', xt, u), approximate=False)
        v = expert_v[it]
        return jnp.einsum('tk,tkd->td', gt * h, v)

    y = lax.map(block, (xb, ib, gb))
    return y.reshape(b, t, d)


def setup_inputs(seed: int = 0) -> dict:
    key = jax.random.key(seed)
    ks = jax.random.split(key, 20)

    def nrm(k, shape, scale):
        return jax.random.normal(k, shape, jnp.float32) * scale

    x = nrm(ks[0], (BATCH, SEQ, D_MODEL), 1.0)
    norm1 = 1.0 + nrm(ks[1], (DEPTH, D_MODEL), 0.02)
    w_in = nrm(ks[2], (DEPTH, D_MODEL, IN_WIDTH), D_MODEL ** -0.5)
    conv_a = nrm(ks[3], (DEPTH, CONV_W, CONV_CH), CONV_W ** -0.5)
    a_log = jnp.log(jax.random.uniform(ks[4], (DEPTH, A_HEADS), jnp.float32, 1.0, 16.0))
    dt = jnp.exp(jax.random.uniform(ks[5], (DEPTH, A_HEADS), jnp.float32, math.log(1e-3), math.log(1e-1)))
    dt_bias = dt + jnp.log(-jnp.expm1(-dt))
    a_onorm = 1.0 + nrm(ks[6], (DEPTH, A_DV), 0.02)
    b_lower_bound = 1.0 + nrm(ks[7], (DEPTH + 1, B_K), 0.1)
    b_onorm = 1.0 + nrm(ks[8], (DEPTH, B_DV), 0.02)
    w_branch_a = nrm(ks[9], (DEPTH, A_V, D_MODEL), A_V ** -0.5)
    w_branch_b = nrm(ks[10], (DEPTH, B_V, D_MODEL), B_V ** -0.5)
    w_out = nrm(ks[11], (DEPTH, D_MODEL, D_MODEL), D_MODEL ** -0.5)
    norm2 = 1.0 + nrm(ks[12], (DEPTH, D_MODEL), 0.02)
    w_pq = nrm(ks[13], (DEPTH, D_MODEL, P_HEADS * P_DKEY), D_MODEL ** -0.5)
    sub_keys = nrm(ks[14], (DEPTH, 2, P_HEADS, N_KEYS, P_DHALF), P_DHALF ** -0.5)
    expert_u = nrm(ks[15], (DEPTH, N_EXPERTS, D_MODEL), D_MODEL ** -0.5)
    expert_v = nrm(ks[16], (DEPTH, N_EXPERTS, D_MODEL), P_HEADS ** -0.5)
    final_norm = 1.0 + nrm(ks[17], (D_MODEL,), 0.02)
    return {'x': x, 'norm1': norm1, 'w_in': w_in, 'conv_a': conv_a, 'a_log': a_log,
            'dt_bias': dt_bias, 'a_onorm': a_onorm, 'b_lower_bound': b_lower_bound,
            'b_onorm': b_onorm, 'w_branch_a': w_branch_a, 'w_branch_b': w_branch_b,
            'w_out': w_out, 'norm2': norm2, 'w_pq': w_pq, 'sub_keys': sub_keys,
            'expert_u': expert_u, 'expert_v': expert_v, 'final_norm': final_norm}


def reference(x, norm1, w_in, conv_a, a_log, dt_bias, a_onorm, b_lower_bound, b_onorm,
              w_branch_a, w_branch_b, w_out, norm2, w_pq, sub_keys, expert_u, expert_v,
              final_norm):
    dt = x.dtype
    bsz, t, _ = x.shape
    f32 = jnp.float32
    lower_bounds = jnp.cumsum(jax.nn.softmax(b_lower_bound.astype(f32), axis=0), axis=0)
    for l in range(DEPTH):
        h = _rmsnorm(x, norm1[l])
        proj = h @ w_in[l]
        a_q, a_k, a_v, a_beta, a_alpha, a_gate, b_f, b_i, b_q, b_gate, g_a, g_b = _split_cols(proj)

        qkv = _causal_conv_silu(jnp.concatenate([a_q, a_k, a_v], axis=-1), conv_a[l]).astype(f32)
        qa = qkv[..., :A_QK].reshape(bsz, t, A_HEADS, A_DK)
        ka = qkv[..., A_QK:2 * A_QK].reshape(bsz, t, A_HEADS, A_DK)
        va = qkv[..., 2 * A_QK:].reshape(bsz, t, A_HEADS, A_DV)
        beta = jax.nn.sigmoid(a_beta.astype(f32))
        log_alpha = -jnp.exp(a_log[l].astype(f32)) * jax.nn.softplus(a_alpha.astype(f32) + dt_bias[l].astype(f32))
        o_a = _gated_delta_rule(qa, ka, va, beta, log_alpha)
        o_a = _rmsnorm(o_a, a_onorm[l]) * jax.nn.silu(a_gate.astype(f32)).reshape(bsz, t, A_HEADS, A_DV)
        y_a = o_a.reshape(bsz, t, A_V).astype(dt) @ w_branch_a[l]

        lb = lower_bounds[l].reshape(B_HEADS, B_DK)
        f_gate = lb + (1.0 - lb) * jax.nn.sigmoid(b_f.astype(f32).reshape(bsz, t, B_HEADS, B_DK))
        qb = jax.nn.silu(b_q.astype(f32)).reshape(bsz, t, B_HEADS, B_DK)
        ib = b_i.astype(f32).reshape(bsz, t, B_HEADS, B_DV)
        o_b = _hgrn2_recurrence(qb, 1.0 - f_gate, ib, jnp.log(f_gate))
        o_b = _rmsnorm(o_b, b_onorm[l]) * jax.nn.sigmoid(b_gate.astype(f32)).reshape(bsz, t, B_HEADS, B_DV)
        y_b = o_b.reshape(bsz, t, B_V).astype(dt) @ w_branch_b[l]

        mix = jax.nn.sigmoid(g_a) * y_a + jax.nn.sigmoid(g_b) * y_b
        x = x + mix @ w_out[l]

        h2 = _rmsnorm(x, norm2[l])
        x = x + _peer(h2, w_pq[l], sub_keys[l], expert_u[l], expert_v[l])
    return _rmsnorm(x, final_norm)
```

```python
import numpy as np
from contextlib import ExitStack
import concourse.bass as bass
import concourse.mybir as mybir
from concourse.bass_utils import run_bass_kernel_spmd

F32 = mybir.dt.float32
BF16 = mybir.dt.bfloat16
I32 = mybir.dt.int32
U32 = mybir.dt.uint32
ALU = mybir.AluOpType
AF = mybir.ActivationFunctionType
AX = mybir.AxisListType

D = 1024
SEQ = 8192
NCORE = 8
BLK = 512
CH = 64
NCH = BLK // CH
EPS = 1e-6
NWA = 2560
CB_AQ, CB_AK, CB_AV, CB_ABETA, CB_AALPHA, CB_AGATE, CB_BF, CB_BI, CB_BQ, CB_BGATE = range(10)
CV_N1, CV_N2, CV_CONV, CV_ALOG, CV_DTB, CV_AON, CV_BON, CV_BLB = 0, 8, 16, 40, 42, 44, 45, 46
NCV = 50
CM_ID, CM_ML, CM_MUS, CM_MUI, CM_I64, CM_RST, CM_ONE, CM_IOTA, CM_THR = 0, 128, 192, 256, 320, 384, 896, 1024, 1040
NCM = 1056


class Reg:
    __slots__ = ("name", "w", "r", "dsem", "dcnt")

    def __init__(self, name):
        self.name = name
        self.w = None
        self.r = []
        self.dsem = None
        self.dcnt = 0


class Tile:
    def __init__(self, t, name, nparts=1):
        self.t = t
        self.name = name
        self.p = [Reg(f"{name}.{i}") for i in range(nparts)]

    def __getitem__(self, idx):
        return self.t[idx]


def _leaves(xs):
    out = []
    for x in xs:
        if x is None:
            continue
        if isinstance(x, Tile):
            out.extend(x.p)
        elif isinstance(x, (list, tuple)):
            out.extend(_leaves(x))
        else:
            out.append(x)
    return out


class K:
    def __init__(self, nc, es):
        self.nc = nc
        self.es = es
        self.es_sem = es
        self.eng = {"pe": nc.tensor, "dve": nc.vector, "act": nc.scalar, "pool": nc.gpsimd, "sp": nc.sync}
        self.sem = {}
        self.cnt = {}
        for e in self.eng:
            self.sem[e] = es.enter_context(nc.semaphore(f"s_{e}"))
            self.cnt[e] = 0
        self.seen = {e: {} for e in self.eng}
        self.dsems = []
        self.n_ins = 0

    def sb(self, name, shape, dt, nparts=1):
        t = self.es.enter_context(self.nc.sbuf_tensor("sb_" + name, list(shape), dt))
        return Tile(t, name, nparts)

    def ps(self, name, shape, dt=F32, nparts=1):
        t = self.es.enter_context(self.nc.psum_tensor("ps_" + name, list(shape), dt))
        return Tile(t, name, nparts)

    def dram(self, name, shape, dt, kind="Internal", nparts=1):
        t = self.nc.dram_tensor(name, list(shape), dt, kind=kind)
        return Tile(t, name, nparts)

    def _wait(self, e, toks):
        E = self.eng[e]
        seen = self.seen[e]
        best = {}
        for (sem, val, owner) in toks:
            k = id(sem)
            if best.get(k, (None, 0))[1] < val:
                best[k] = (sem, val)
        for k, (sem, val) in best.items():
            if seen.get(k, 0) < val:
                E.wait_ge(sem, val)
                seen[k] = val

    def _deps(self, e, reads, writes):
        toks = []
        for r in reads:
            if r.w is not None:
                if r.w[2] == e and e == "pe":
                    continue
                toks.append(r.w)
        for w in writes:
            if w.w is not None and (w.w[2] != e or e != "pe"):
                toks.append(w.w)
            for t in w.r:
                if t[2] != e or e != "pe":
                    toks.append(t)
        return toks

    def op(self, e, fn, reads=(), writes=()):
        reads = _leaves(reads)
        writes = _leaves(writes)
        self._wait(e, self._deps(e, reads, writes))
        ins = fn(self.eng[e])
        self.cnt[e] += 1
        ins.then_inc(self.sem[e], 1)
        tok = (self.sem[e], self.cnt[e], e)
        for w in writes:
            w.w = tok
            w.r = []
        for r in reads:
            r.r.append(tok)
        self.n_ins += 1
        return ins

    def _dsem(self, reg):
        if reg.dsem is None:
            reg.dsem = self.es_sem.enter_context(self.nc.semaphore(f"d{len(self.dsems)}"))
            self.dsems.append(reg)
        return reg.dsem

    def dma(self, q, out, in_, reads=(), writes=(), fn=None, inc=16, nowaw=False, **kw):
        reads = _leaves(reads)
        writes = _leaves(writes)
        deps = self._deps("dma", reads, writes)
        if nowaw:
            mine = {id(w.dsem) for w in writes if w.dsem is not None}
            deps = [t for t in deps if not (t[2] == "dma" and id(t[0]) in mine)]
        self._wait(q, deps)
        holder = writes[0] if writes else reads[0]
        sem = self._dsem(holder)
        if fn is None:
            ins = self.eng[q].dma_start(out=out, in_=in_, **kw)
        else:
            ins = fn(self.eng[q])
        ins.then_inc(sem, inc)
        holder.dcnt += inc
        tok = (sem, holder.dcnt, "dma")
        for w in writes:
            w.w = tok
            w.r = []
        for r in reads:
            r.r.append(tok)
        self.n_ins += 1
        return ins

    def barrier(self):
        toks = [(self.sem[e], self.cnt[e], e) for e in self.eng if self.cnt[e] > 0]
        toks += [(r.dsem, r.dcnt, "dma") for r in self.dsems if r.dcnt > 0]
        for e in self.eng:
            self._wait(e, [t for t in toks if t[2] != e])

    def finish(self):
        toks = [(r.dsem, r.dcnt, "dma") for r in self.dsems if r.dcnt > 0]
        toks += [(self.sem[e], self.cnt[e], e) for e in self.eng if self.cnt[e] > 0 and e != "sp"]
        self._wait("sp", toks)


def build(nblk=SEQ // BLK, dbg=None, phase3=True, do_a=True, do_b=True, ntile3=16, stop3=None):
    nc = bass.Bass("TRN2", target_bir_lowering=False)
    es = ExitStack()
    k = K(nc, es)
    dbg = dbg or {}

    x_d = nc.dram_tensor("x", [SEQ, D], F32, kind="ExternalInput")
    wA_d = nc.dram_tensor("wA", [D, NWA], F32, kind="ExternalInput")
    cvec_d = nc.dram_tensor("cvec", [128, NCV], F32, kind="ExternalInput")
    cmat_d = nc.dram_tensor("cmat", [128, NCM], F32, kind="ExternalInput")
    dbg_d = {}
    for name, shape in dbg.items():
        dbg_d[name] = k.dram(name, shape, F32, kind="ExternalOutput")

    cvec = k.sb("cvec", [128, NCV], F32)
    cmat = k.sb("cmat", [128, NCM], F32)
    k.dma("sp", cvec[:], cvec_d.ap()[:, :], writes=[cvec])
    k.dma("sp", cmat[:], cmat_d.ap()[:, :], writes=[cmat])
    ident_bf = k.sb("ident_bf", [128, 128], BF16)
    ones_bf = k.sb("ones_bf", [128, 128], BF16)
    k.op("dve", lambda e: e.tensor_copy(ident_bf[:], cmat[:, CM_ID:CM_ID + 128]), [cmat], [ident_bf])
    k.op("dve", lambda e: e.tensor_copy(ones_bf[:], cmat[:, CM_ONE:CM_ONE + 128]), [cmat], [ones_bf])
    ML = cmat.t[0:64, CM_ML:CM_ML + 64]
    MUS = cmat.t[0:64, CM_MUS:CM_MUS + 64]
    MUI = cmat.t[0:64, CM_MUI:CM_MUI + 64]
    I64 = cmat.t[0:64, CM_I64:CM_I64 + 64]
    RST = cmat.t[:, CM_RST:CM_RST + 512]

    def bc8(m):
        return m.unsqueeze(1).to_broadcast([64, NCH, 64])

    cder = k.sb("cder", [128, 8], F32)
    for h in range(2):
        k.op("act", lambda e, h=h: e.activation(out=cder[:, h:h + 1], in_=cvec[:, CV_ALOG + h:CV_ALOG + h + 1], func=AF.Exp),
             [cvec], [cder])
        k.op("dve", lambda e, h=h: e.tensor_scalar(out=cder[:, h:h + 1], in0=cder[:, h:h + 1], scalar1=-1.0, scalar2=None,
                                                   op0=ALU.mult), [cder], [cder])
        k.op("dve", lambda e, h=h: e.tensor_tensor(out=cder[:, 6 + h:7 + h], in0=cvec[:, CV_BLB + 2 * h:CV_BLB + 2 * h + 1],
                                                   in1=cvec[:, CV_BLB + 2 * h + 1:CV_BLB + 2 * h + 2], op=ALU.subtract),
             [cvec], [cder])
        k.op("act", lambda e, h=h: e.activation(out=cder[:, 2 + h:3 + h], in_=cder[:, 6 + h:7 + h], func=AF.Sigmoid),
             [cder], [cder])
        k.op("dve", lambda e, h=h: e.tensor_scalar(out=cder[:, 4 + h:5 + h], in0=cder[:, 2 + h:3 + h], scalar1=-1.0, scalar2=1.0,
                                                   op0=ALU.mult, op1=ALU.add), [cder], [cder])

    k.op("dve", lambda e: e.memset(cder[:, 7:8], EPS), [], [cder])
    epsc = cder[:, 7:8]
    cone = k.sb("cone", [128, 1], F32)
    k.op("dve", lambda e: e.memset(cone[:], 1.0), [], [cone])
    onec = cone[:, 0:1]
    xt = k.sb("xt", [128, 2, D], F32, nparts=2)
    junk = k.sb("junk", [128, D], BF16)
    xn = k.sb("xn", [128, 2, D], BF16, nparts=2)
    st = k.sb("st", [128, 4], F32, nparts=2)
    hT = k.sb("hT", [128, 8, BLK], BF16)

    pT = k.ps("pT", [128, 8, 128], BF16)
    pp = [k.ps("pp0", [128, BLK], F32)]
    pm = k.ps("pm", [128, BLK], F32)
    es1 = ExitStack()
    k.es = es1
    W = k.sb("W", [128, 8, NWA], BF16)
    wA_v = wA_d.ap().rearrange("(kc p) c -> p kc c", p=128)
    for i in range(8):
        k.dma("pool", W[:, :, i * 320:(i + 1) * 320], wA_v[:, :, i * 320:(i + 1) * 320], writes=[W], nowaw=True)
    if phase3 and ntile3 > 0:
        euv_d = nc.dram_tensor("euv", [16384, 2 * D], F32, kind="ExternalInput")
        euvb = k.dram("euvb", [16384, 2 * D], BF16)
        for i in range(16):
            k.dma("pool", euvb[i * 1024:(i + 1) * 1024, :], euv_d.ap()[i * 1024:(i + 1) * 1024, :], writes=[euvb], nowaw=True)

    k.es = es1
    pa = k.ps("pa", [128, BLK], F32)
    pb = k.ps("pb", [128, BLK], F32)
    ptr = k.ps("ptr", [128, NCH, 128], BF16)
    po = k.ps("po", [128, BLK], F32)
    prw = k.ps("prw", [128, 2, 128], F32, nparts=2)

    x_v = x_d.ap()
    pp_i = [0]

    def front(tok0, x_v=x_v):
        for tt in range(4):
            s = tt % 2
            k.dma("sp", xt[:, s, :], x_v[tok0 + tt * 128: tok0 + (tt + 1) * 128, :], writes=[xt.p[s]])
            k.op("act", lambda e: e.activation(out=junk[:], in_=xt[:, s, :], func=AF.Square, accum_out=st[:, 2 * s:2 * s + 1]),
                 [xt.p[s]], [junk, st.p[s]])
            k.op("act", lambda e: e.activation(out=st[:, 2 * s + 1:2 * s + 2], in_=st[:, 2 * s:2 * s + 1], func=AF.Sqrt,
                                               bias=epsc, scale=1.0 / D), [st.p[s], cder], [st.p[s]])
            k.op("dve", lambda e: e.reciprocal(out=st[:, 2 * s + 1:2 * s + 2], in_=st[:, 2 * s + 1:2 * s + 2]), [st.p[s]], [st.p[s]])
            k.op("act", lambda e: e.activation(out=xn[:, s, :], in_=xt[:, s, :], func=AF.Copy, scale=st[:, 2 * s + 1:2 * s + 2]),
                 [xt.p[s], st.p[s]], [xn.p[s]])
            for kc in range(8):
                k.op("pe", lambda e, kc=kc: e.transpose(out=pT[:, kc, :], in_=xn[:, s, kc * 128:(kc + 1) * 128], identity=ident_bf[:]),
                     [xn.p[s], ident_bf], [pT])
            k.op("dve", lambda e: e.tensor_tensor(out=hT[:, :, tt * 128:(tt + 1) * 128], in0=pT[:],
                                                  in1=cvec[:, CV_N1:CV_N1 + 8].unsqueeze(2).to_broadcast([128, 8, 128]),
                                                  op=ALU.mult), [pT, cvec], [hT])
            yield 'p'

    def proj(col0, Wt=None):
        Wt = Wt or W
        p = pp[0]
        for kc in range(8):
            k.op("pe", lambda e, kc=kc: e.matmul(p[:], lhsT=Wt[:, kc, col0:col0 + 128], rhs=hT[:, kc, :],
                                                 start=(kc == 0), stop=(kc == 7)), [Wt, hT], [p])
        return p

    def dump(name, src_tile, src_ap, dst_ap):
        k.dma("sp", dst_ap, src_ap, reads=[src_tile], writes=[dbg_d[name]])

    def T(name, cols=BLK, dt=F32, rows=128):
        return k.sb(name, [rows, cols], dt)

    b_sg, b_lf, b_cs, b_eb, b_enb, b_qs, b_omf = [T(f"b_{n}") for n in ("sg", "lf", "cs", "eb", "enb", "qs", "omf")]
    b_df = b_lf
    b_qe, b_ke, b_kdT, b_vT, b_aT, b_sq, b_ob = [T(f"b_{n}", dt=BF16) for n in ("qe", "ke", "kdT", "vT", "aT", "sq", "ob")]
    b_vtm = k.sb("b_vtm", [64, NCH, 128], BF16)
    b_kd = k.sb("b_kd", [64, NCH, 128], BF16)
    b_gate, b_or, b_rs = T("b_gate"), T("b_or"), T("b_rs")
    b_S = [k.sb(f"b_S{h}", [128, 128], F32) for h in range(2)]
    b_Sb = [k.sb(f"b_Sb{h}", [128, 128], BF16) for h in range(2)]
    for h in range(2):
        k.op("dve", lambda e, h=h: e.memset(b_S[h][:], 0.0), [], [b_S[h]])
        k.op("dve", lambda e, h=h: e.memset(b_Sb[h][:], 0.0), [], [b_Sb[h]])
    NB1 = SEQ // BLK
    o_scr = [k.dram(f"o_scr{q}", [512, BLK], BF16) for q in range(NB1)]
    gath = [k.dram(f"gath{q}", [4 * 512, BLK], BF16) for q in range(NB1)]
    ob_scr, oa_scr = 256, 0

    def out_norm(h, blk, o_raw, gate, onorm_col, scr, sq, rs, ob):
        k.op("act", lambda e: e.activation(out=sq[:], in_=o_raw[:], func=AF.Square), [o_raw], [sq])
        k.op("pe", lambda e: e.matmul(pm[:], lhsT=ones_bf[:], rhs=sq[:], start=True, stop=True), [ones_bf, sq], [pm])
        k.op("act", lambda e: e.activation(out=rs[:], in_=pm[:], func=AF.Sqrt, bias=epsc, scale=1.0 / 128), [pm, cder], [rs])
        k.op("dve", lambda e: e.reciprocal(out=rs[:], in_=rs[:]), [rs], [rs])
        k.op("dve", lambda e: e.scalar_tensor_tensor(out=rs[:], in0=o_raw[:], scalar=cvec[:, onorm_col:onorm_col + 1], in1=rs[:],
                                                     op0=ALU.mult, op1=ALU.mult), [o_raw, cvec, rs], [rs])
        k.op("dve", lambda e: e.tensor_tensor(out=ob[:], in0=rs[:], in1=gate[:], op=ALU.mult), [rs, gate], [ob])
        sq_ = o_scr[blk]
        k.dma("sp", sq_[scr + h * 128:scr + (h + 1) * 128, :], ob[:], reads=[ob], writes=[sq_])

    def mixer_b(h, blk):
        c0 = h * 1280
        lb = cder[:, 2 + h:3 + h]
        oml = cder[:, 4 + h:5 + h]
        p = proj(c0 + CB_BF * 128)
        k.op("act", lambda e: e.activation(out=b_sg[:], in_=p[:], func=AF.Sigmoid), [p], [b_sg])
        yield 'p'
        k.op("dve", lambda e: e.tensor_scalar(out=b_sg[:], in0=b_sg[:], scalar1=oml, scalar2=lb, op0=ALU.mult, op1=ALU.add),
             [b_sg, cder], [b_sg])
        yield 'p'
        k.op("act", lambda e: e.activation(out=b_lf[:], in_=b_sg[:], func=AF.Ln), [b_sg], [b_lf])
        yield 'p'
        k.op("dve", lambda e: e.tensor_tensor_scan(out=b_cs[:], data0=RST, data1=b_lf[:], initial=0.0, op0=ALU.mult, op1=ALU.add),
             [cmat, b_lf], [b_cs])
        yield 'p'
        k.op("act", lambda e: e.activation(out=b_eb[:], in_=b_cs[:], func=AF.Exp), [b_cs], [b_eb])
        yield 'p'
        k.op("act", lambda e: e.activation(out=b_enb[:], in_=b_cs[:], func=AF.Exp, scale=-1.0), [b_cs], [b_enb])
        yield 'p'
        k.op("dve", lambda e: e.tensor_scalar(out=b_omf[:], in0=b_sg[:], scalar1=-1.0, scalar2=1.0, op0=ALU.mult, op1=ALU.add),
             [b_sg], [b_omf])
        yield 'p'
        k.op("dve", lambda e: e.tensor_tensor(out=b_ke[:], in0=b_omf[:], in1=b_enb[:], op=ALU.mult), [b_omf, b_enb], [b_ke])
        yield 'p'
        cs3 = b_cs.t[:, :].rearrange("p (c j) -> p c j", j=CH)
        k.op("dve", lambda e: e.tensor_tensor(out=b_df[:, :].rearrange("p (c j) -> p c j", j=CH),
                                              in0=cs3[:, :, CH - 1:CH].to_broadcast([128, NCH, CH]), in1=cs3, op=ALU.subtract),
             [b_cs], [b_df])
        yield 'p'
        k.op("act", lambda e: e.activation(out=b_df[:], in_=b_df[:], func=AF.Exp), [b_df], [b_df])
        yield 'p'
        k.op("dve", lambda e: e.tensor_tensor(out=b_kdT[:], in0=b_omf[:], in1=b_df[:], op=ALU.mult), [b_omf, b_df], [b_kdT])
        yield 'p'
        p = proj(c0 + CB_BQ * 128)
        k.op("act", lambda e: e.activation(out=b_qs[:], in_=p[:], func=AF.Silu), [p], [b_qs])
        yield 'p'
        k.op("dve", lambda e: e.tensor_tensor(out=b_qe[:], in0=b_qs[:], in1=b_eb[:], op=ALU.mult), [b_qs, b_eb], [b_qe])
        yield 'p'
        p = proj(c0 + CB_BI * 128)
        k.op("act", lambda e: e.activation(out=b_vT[:], in_=p[:], func=AF.Copy), [p], [b_vT])
        yield 'p'
        p = proj(c0 + CB_BGATE * 128)
        k.op("act", lambda e: e.activation(out=b_gate[:], in_=p[:], func=AF.Sigmoid), [p], [b_gate])
        yield 'p'
        for src, dst in ((b_vT, b_vtm), (b_kdT, b_kd)):
            for c in range(NCH):
                k.op("pe", lambda e, c=c, src=src: e.transpose(out=ptr[0:64, c, :], in_=src[:, c * CH:(c + 1) * CH], identity=ident_bf[:]),
                     [src, ident_bf], [ptr])
                yield 'p'
            k.op("dve", lambda e, dst=dst: e.tensor_copy(dst[:], ptr[0:64, :, :]), [ptr], [dst])
            yield 'p'
        for c in range(NCH):
            k.op("pe", lambda e, c=c: e.matmul(pm[0:64, c * CH:(c + 1) * CH], lhsT=b_ke[:, c * CH:(c + 1) * CH],
                                               rhs=b_qe[:, c * CH:(c + 1) * CH], start=True, stop=True), [b_ke, b_qe], [pm])
            yield 'p'
        k.op("dve", lambda e: e.tensor_tensor(out=b_aT[0:64, :].rearrange("p (c j) -> p c j", j=CH),
                                              in0=pm[0:64, :].rearrange("p (c j) -> p c j", j=CH), in1=bc8(MUI), op=ALU.mult),
             [pm, cmat], [b_aT])
        yield 'p'
        yield 'REC'
        S, Sb = b_S[h], b_Sb[h]
        for c in range(NCH):
            cs = slice(c * CH, (c + 1) * CH)
            k.op("pe", lambda e: e.matmul(po[:, cs], lhsT=Sb[:], rhs=b_qe[:, cs], start=True, stop=False), [Sb, b_qe], [po])
            k.op("pe", lambda e: e.matmul(po[:, cs], lhsT=b_vtm[0:64, c, :], rhs=b_aT[0:64, cs], start=False, stop=True),
                 [b_vtm, b_aT], [po])
            k.op("pe", lambda e: e.matmul(prw[:, 1, :], lhsT=b_kd[0:64, c, :], rhs=b_vtm[0:64, c, :], start=True, stop=True),
                 [b_kd, b_vtm], [prw.p[1]])
            k.op("dve", lambda e: e.scalar_tensor_tensor(out=S[:], in0=S[:], scalar=b_eb[:, c * CH + CH - 1:c * CH + CH], in1=prw[:, 1, :],
                                                         op0=ALU.mult, op1=ALU.add), [S, b_eb, prw.p[1]], [S])
            k.op("act", lambda e: e.activation(out=Sb[:], in_=S[:], func=AF.Copy), [S], [Sb])
            yield 'r'
        k.op("act", lambda e: e.activation(out=b_or[:], in_=po[:], func=AF.Copy), [po], [b_or])
        if "ob_raw" in dbg:
            dump("ob_raw", b_or, b_or[:], dbg_d["ob_raw"][h * 128:(h + 1) * 128, blk * BLK:(blk + 1) * BLK])
        out_norm(h, blk, b_or, b_gate, CV_BON, ob_scr, b_sq, b_rs, b_ob)
        yield 'r'

    a_beta, a_g, a_Gb, a_cv, a_qs, a_ks, a_vs, a_rn, a_kb, a_or, a_rs = [
        T(f"a_{n}") for n in ("beta", "g", "Gb", "cv", "qs", "ks", "vs", "rn", "kb", "or", "rs")]
    a_EL, a_qn, a_kn = a_g, a_qs, a_ks
    a_sq, a_qnb, a_knb, a_kbb, a_rwT, a_kdT, a_ruT, a_ob, a_sq2 = [
        T(f"a_{n}", dt=BF16) for n in ("sq", "qnb", "knb", "kbb", "rwT", "kdT", "ruT", "ob", "sq2")]
    a_E2 = [T(f"a_E{i}") for i in range(2)]
    a_gate2 = [T(f"a_gate{i}") for i in range(2)]
    a_qd2 = [T(f"a_qd{i}", dt=BF16) for i in range(2)]
    a_aqk2 = [T(f"a_aqk{i}", dt=BF16, rows=64) for i in range(2)]
    a_t64, a_d, a_Dm, a_DTs, a_DTi = [T(f"a_{n}", rows=64) for n in ("t64", "d", "Dm", "DTs", "DTi")]
    a_Gtm = k.sb("a_Gtm", [64, NCH], F32)
    a_A = [T(f"a_A{i}", rows=64) for i in range(2)]
    a_B = [T(f"a_B{i}", rows=64) for i in range(2)]
    a_Y = [T(f"a_Y{i}", rows=64) for i in range(2)]
    a_Y5 = T("a_Y5", dt=BF16, rows=64)
    a_ru = k.sb("a_ru", [64, NCH, 128], BF16)
    a_rw = k.sb("a_rw", [64, NCH, 128], BF16)
    a_kd2 = [k.sb(f"a_kd{i}", [64, NCH, 128], BF16) for i in range(2)]
    a_u2 = [k.sb(f"a_u{i}", [64, NCH, 128], F32) for i in range(2)]
    a_wT2 = [T(f"a_wT{i}", dt=BF16) for i in range(2)]
    a_vn = k.sb("a_vn", [64, 128], BF16)
    a_x = [[k.sb(f"a_x{h}{qi}", [128, BLK + 3], F32) for qi in range(3)] for h in range(2)]
    a_S = [k.sb(f"a_S{h}", [128, 128], F32) for h in range(2)]
    a_Sb = [k.sb(f"a_Sb{h}", [128, 128], BF16) for h in range(2)]
    for h in range(2):
        k.op("dve", lambda e: e.memset(a_S[h][:], 0.0), [], [a_S[h]])
        k.op("dve", lambda e: e.memset(a_Sb[h][:], 0.0), [], [a_Sb[h]])
        for qi in range(3):
            k.op("dve", lambda e: e.memset(a_x[h][qi][:], 0.0), [], [a_x[h][qi]])

    def v3(t, rows=128):
        return t.t[0:rows, :].rearrange("p (c j) -> p c j", j=CH)

    print("sbuf bytes remaining (phase 1):", nc.sbuf_bytes_remaining)

    def mixer_a(h, blk):
        a_E, a_gate, a_qd, a_aqk = a_E2[h], a_gate2[h], a_qd2[h], a_aqk2[h]
        a_kd, a_u, a_wT = a_kd2[h], a_u2[h], a_wT2[h]
        c0 = h * 1280
        nA = cder[:, h:h + 1]
        dtb = cvec[:, CV_DTB + h:CV_DTB + h + 1]
        p = proj(c0 + CB_ABETA * 128)
        k.op("act", lambda e: e.activation(out=a_beta[:], in_=p[:], func=AF.Sigmoid), [p], [a_beta])
        yield 'p'
        p = proj(c0 + CB_AALPHA * 128)
        k.op("act", lambda e: e.activation(out=a_g[:], in_=p[:], func=AF.Exp, bias=dtb), [p, cvec], [a_g])
        yield 'p'
        k.op("act", lambda e: e.activation(out=a_g[:], in_=a_g[:], func=AF.Ln, bias=onec), [a_g, cone], [a_g])
        yield 'p'
        k.op("dve", lambda e: e.tensor_scalar(out=a_g[:], in0=a_g[:], scalar1=nA, scalar2=None, op0=ALU.mult), [a_g, cder], [a_g])
        yield 'p'
        k.op("dve", lambda e: e.tensor_tensor_scan(out=a_Gb[:], data0=RST, data1=a_g[:], initial=0.0, op0=ALU.mult, op1=ALU.add),
             [cmat, a_g], [a_Gb])
        yield 'p'
        k.op("act", lambda e: e.activation(out=a_E[:], in_=a_Gb[:], func=AF.Exp), [a_Gb], [a_E])
        yield 'p'
        G3 = v3(a_Gb)
        k.op("dve", lambda e: e.tensor_tensor(out=v3(a_EL), in0=G3[:, :, CH - 1:CH].to_broadcast([128, NCH, CH]), in1=G3, op=ALU.subtract),
             [a_Gb], [a_EL])
        yield 'p'
        k.op("act", lambda e: e.activation(out=a_EL[:], in_=a_EL[:], func=AF.Exp), [a_EL], [a_EL])
        yield 'p'
        for qi, (cb, dst) in enumerate(((CB_AQ, a_qs), (CB_AK, a_ks), (CB_AV, a_vs))):
            xb = a_x[h][qi]
            p = proj(c0 + cb * 128)
            k.op("dve", lambda e: e.tensor_copy(xb[:, 0:3], xb[:, BLK:BLK + 3]), [xb], [xb])
            yield 'p'
            k.op("act", lambda e: e.activation(out=xb[:, 3:BLK + 3], in_=p[:], func=AF.Copy), [p], [xb])
            yield 'p'
            wc = CV_CONV + h * 12 + qi * 4
            k.op("dve", lambda e: e.tensor_scalar(out=a_cv[:], in0=xb[:, 0:BLK], scalar1=cvec[:, wc:wc + 1], scalar2=None, op0=ALU.mult),
                 [xb, cvec], [a_cv])
            yield 'p'
            for j in range(1, 4):
                k.op("dve", lambda e: e.scalar_tensor_tensor(out=a_cv[:], in0=xb[:, j:j + BLK], scalar=cvec[:, wc + j:wc + j + 1],
                                                             in1=a_cv[:], op0=ALU.mult, op1=ALU.add), [xb, cvec, a_cv], [a_cv])
                yield 'p'
            k.op("act", lambda e: e.activation(out=dst[:], in_=a_cv[:], func=AF.Silu), [a_cv], [dst])
            yield 'p'
        for src, dst, scl in ((a_qs, a_qn, 128.0 ** -0.5), (a_ks, a_kn, 1.0)):
            k.op("act", lambda e: e.activation(out=a_sq[:], in_=src[:], func=AF.Square), [src], [a_sq])
            yield 'p'
            k.op("pe", lambda e: e.matmul(pm[:], lhsT=ones_bf[:], rhs=a_sq[:], start=True, stop=True), [ones_bf, a_sq], [pm])
            yield 'p'
            k.op("act", lambda e: e.activation(out=a_rn[:], in_=pm[:], func=AF.Sqrt, bias=epsc), [pm, cder], [a_rn])
            yield 'p'
            k.op("dve", lambda e: e.reciprocal(out=a_rn[:], in_=a_rn[:]), [a_rn], [a_rn])
            yield 'p'
            k.op("dve", lambda e: e.scalar_tensor_tensor(out=dst[:], in0=src[:], scalar=scl, in1=a_rn[:], op0=ALU.mult, op1=ALU.mult),
                 [src, a_rn], [dst])
            yield 'p'
        k.op("act", lambda e: e.activation(out=a_qnb[:], in_=a_qn[:], func=AF.Copy), [a_qn], [a_qnb])
        yield 'p'
        k.op("act", lambda e: e.activation(out=a_knb[:], in_=a_kn[:], func=AF.Copy), [a_kn], [a_knb])
        yield 'p'
        k.op("dve", lambda e: e.tensor_tensor(out=a_qd[:], in0=a_qn[:], in1=a_E[:], op=ALU.mult), [a_qn, a_E], [a_qd])
        yield 'p'
        k.op("dve", lambda e: e.tensor_tensor(out=a_kb[:], in0=a_kn[:], in1=a_beta[:], op=ALU.mult), [a_kn, a_beta], [a_kb])
        yield 'p'
        k.op("act", lambda e: e.activation(out=a_kbb[:], in_=a_kb[:], func=AF.Copy), [a_kb], [a_kbb])
        yield 'p'
        k.op("dve", lambda e: e.tensor_tensor(out=a_rwT[:], in0=a_kb[:], in1=a_E[:], op=ALU.mult), [a_kb, a_E], [a_rwT])
        yield 'p'
        k.op("dve", lambda e: e.tensor_tensor(out=a_kdT[:], in0=a_kn[:], in1=a_EL[:], op=ALU.mult), [a_kn, a_EL], [a_kdT])
        yield 'p'
        k.op("dve", lambda e: e.tensor_tensor(out=a_ruT[:], in0=a_vs[:], in1=a_beta[:], op=ALU.mult), [a_vs, a_beta], [a_ruT])
        yield 'p'
        p = proj(c0 + CB_AGATE * 128)
        k.op("act", lambda e: e.activation(out=a_gate[:], in_=p[:], func=AF.Silu), [p], [a_gate])
        yield 'p'
        k.op("dve", lambda e: e.tensor_tensor(out=v3(a_t64, 64), in0=v3(a_Gb, 64), in1=bc8(I64), op=ALU.mult), [a_Gb, cmat], [a_t64])
        yield 'p'
        k.op("dve", lambda e: e.tensor_reduce(out=a_Gtm[:], in_=v3(a_t64, 64), axis=AX.X, op=ALU.add), [a_t64], [a_Gtm])
        yield 'p'
        k.op("dve", lambda e: e.tensor_tensor(out=v3(a_d, 64), in0=v3(a_Gb, 64), in1=a_Gtm.t[:, :].unsqueeze(2).to_broadcast([64, NCH, CH]),
                                              op=ALU.subtract), [a_Gb, a_Gtm], [a_d])
        yield 'p'
        k.op("dve", lambda e: e.tensor_scalar(out=a_t64[:], in0=a_d[:], scalar1=0.0, scalar2=None, op0=ALU.max), [a_d], [a_t64])
        yield 'p'
        k.op("act", lambda e: e.activation(out=a_t64[:], in_=a_t64[:], func=AF.Exp, scale=-1.0), [a_t64], [a_t64])
        yield 'p'
        k.op("dve", lambda e: e.tensor_tensor(out=v3(a_Dm, 64), in0=v3(a_t64, 64), in1=bc8(ML), op=ALU.mult), [a_t64, cmat], [a_Dm])
        yield 'p'
        k.op("dve", lambda e: e.tensor_scalar(out=a_d[:], in0=a_d[:], scalar1=0.0, scalar2=None, op0=ALU.min), [a_d], [a_d])
        yield 'p'
        k.op("act", lambda e: e.activation(out=a_d[:], in_=a_d[:], func=AF.Exp), [a_d], [a_d])
        yield 'p'
        k.op("dve", lambda e: e.tensor_tensor(out=v3(a_DTs, 64), in0=v3(a_d, 64), in1=bc8(MUS), op=ALU.mult), [a_d, cmat], [a_DTs])
        yield 'p'
        k.op("dve", lambda e: e.tensor_tensor(out=v3(a_DTi, 64), in0=v3(a_d, 64), in1=bc8(MUI), op=ALU.mult), [a_d, cmat], [a_DTi])
        yield 'p'
        for (l, r, msk, dst) in ((a_kbb, a_knb, a_Dm, a_A[0]), (a_knb, a_kbb, a_DTs, a_B[0]), (a_knb, a_qnb, a_DTi, a_aqk)):
            for c in range(NCH):
                cs = slice(c * CH, (c + 1) * CH)
                k.op("pe", lambda e: e.matmul(pm[0:64, cs], lhsT=l[:, cs], rhs=r[:, cs], start=True, stop=True), [l, r], [pm])
                yield 'p'
            k.op("dve", lambda e: e.tensor_tensor(out=dst[:], in0=pm[0:64, :], in1=msk[:], op=ALU.mult), [pm, msk], [dst])
            yield 'p'
        k.op("dve", lambda e: e.tensor_tensor(out=v3(a_Y[0], 64), in0=bc8(I64), in1=v3(a_B[0], 64), op=ALU.subtract), [cmat, a_B[0]], [a_Y[0]])
        yield 'p'
        for s_ in range(1, 6):
            A0, B0, Y0 = a_A[(s_ - 1) % 2], a_B[(s_ - 1) % 2], a_Y[(s_ - 1) % 2]
            A1, B1, Y1 = a_A[s_ % 2], a_B[s_ % 2], a_Y[s_ % 2]
            for c in range(NCH):
                cs = slice(c * CH, (c + 1) * CH)
                k.op("pe", lambda e: e.matmul(pa[0:64, cs], lhsT=B0[:, cs], rhs=A0[:, cs], start=True, stop=True), [A0, B0], [pa])
                yield 'p'
            k.op("act", lambda e: e.activation(out=A1[:], in_=pa[0:64, :], func=AF.Copy), [pa], [A1])
            yield 'p'
            if s_ < 5:
                for c in range(NCH):
                    cs = slice(c * CH, (c + 1) * CH)
                    k.op("pe", lambda e: e.matmul(pb[0:64, cs], lhsT=A0[:, cs], rhs=B0[:, cs], start=True, stop=True), [A0, B0], [pb])
                k.op("act", lambda e: e.activation(out=B1[:], in_=pb[0:64, :], func=AF.Copy), [pb], [B1])
                yield 'p'
            for c in range(NCH):
                cs = slice(c * CH, (c + 1) * CH)
                k.op("pe", lambda e: e.matmul(pm[0:64, cs], lhsT=A1[:, cs], rhs=Y0[:, cs], start=True, stop=True), [A1, Y0], [pm])
                yield 'p'
            Yd = a_Y5 if s_ == 5 else Y1
            k.op("dve", lambda e: e.tensor_tensor(out=Yd[:], in0=Y0[:], in1=pm[0:64, :], op=ALU.add), [Y0, pm], [Yd])
            yield 'p'
        for src, dst in ((a_ruT, a_ru), (a_rwT, a_rw), (a_kdT, a_kd)):
            for c in range(NCH):
                k.op("pe", lambda e: e.transpose(out=ptr[0:64, c, :], in_=src[:, c * CH:(c + 1) * CH], identity=ident_bf[:]),
                     [src, ident_bf], [ptr])
                yield 'p'
            k.op("dve", lambda e: e.tensor_copy(dst[:], ptr[0:64, :, :]), [ptr], [dst])
            yield 'p'
        for half, pu in ((0, pa), (1, pb)):
            for c4 in range(4):
                c = half * 4 + c4
                cs = slice(c * CH, (c + 1) * CH)
                k.op("pe", lambda e: e.matmul(pu[0:64, c4 * 128:(c4 + 1) * 128], lhsT=a_Y5[:, cs], rhs=a_ru[:, c, :], start=True, stop=True),
                     [a_Y5, a_ru], [pu])
                yield 'p'
            k.op("act", lambda e: e.activation(out=a_u[:, half * 4:half * 4 + 4, :], in_=pu[0:64, :].rearrange("p (c d) -> p c d", d=128),
                                               func=AF.Copy), [pu], [a_u])
            yield 'p'
        for c in range(NCH):
            cs = slice(c * CH, (c + 1) * CH)
            k.op("pe", lambda e: e.matmul(pm[:, cs], lhsT=a_rw[:, c, :], rhs=a_Y5[:, cs], start=True, stop=True), [a_rw, a_Y5], [pm])
            yield 'p'
        k.op("act", lambda e: e.activation(out=a_wT[:], in_=pm[:], func=AF.Copy), [pm], [a_wT])
        yield 'p'
        yield 'REC'
        S, Sb = a_S[h], a_Sb[h]
        for c in range(NCH):
            cs = slice(c * CH, (c + 1) * CH)
            k.op("pe", lambda e: e.matmul(prw[0:64, 0, :], lhsT=a_wT[:, cs], rhs=Sb[:], start=True, stop=True), [a_wT, Sb], [prw.p[0]])
            k.op("dve", lambda e: e.tensor_tensor(out=a_vn[:], in0=a_u[:, c, :], in1=prw[0:64, 0, :], op=ALU.subtract),
                 [a_u, prw.p[0]], [a_vn])
            k.op("pe", lambda e: e.matmul(po[:, cs], lhsT=Sb[:], rhs=a_qd[:, cs], start=True, stop=False), [Sb, a_qd], [po])
            k.op("pe", lambda e: e.matmul(po[:, cs], lhsT=a_vn[:], rhs=a_aqk[:, cs], start=False, stop=True), [a_vn, a_aqk], [po])
            k.op("pe", lambda e: e.matmul(prw[:, 1, :], lhsT=a_kd[:, c, :], rhs=a_vn[:], start=True, stop=True), [a_kd, a_vn], [prw.p[1]])
            k.op("dve", lambda e: e.scalar_tensor_tensor(out=S[:], in0=S[:], scalar=a_E[:, c * CH + CH - 1:c * CH + CH], in1=prw[:, 1, :],
                                                         op0=ALU.mult, op1=ALU.add), [S, a_E, prw.p[1]], [S])
            k.op("act", lambda e: e.activation(out=Sb[:], in_=S[:], func=AF.Copy), [S], [Sb])
            yield 'r'
        k.op("act", lambda e: e.activation(out=a_or[:], in_=po[:], func=AF.Copy), [po], [a_or])
        if "oa_raw" in dbg:
            dump("oa_raw", a_or, a_or[:], dbg_d["oa_raw"][h * 128:(h + 1) * 128, blk * BLK:(blk + 1) * BLK])
        out_norm(h, blk, a_or, a_gate, CV_AON, oa_scr, a_sq2, a_rs, a_ob)
        yield 'r'

    def coll(blk):
        k.dma("pool", None, None, reads=[o_scr[blk]], writes=[gath[blk]],
              fn=lambda e: e.collective_compute("AllGather", ALU.bypass, replica_groups=[[0, 1, 2, 3], [4, 5, 6, 7]],
                                                ins=[o_scr[blk].t.ap().opt()], outs=[gath[blk].t.ap().opt()]), inc=1)

    def unit(kind, h, blk):
        if kind == "b" and h == 0:
            yield from front(blk * BLK)
        if kind == "b":
            yield from mixer_b(h, blk)
        else:
            yield from mixer_a(h, blk)
        if phase3 and kind == last_kind and h == 1:
            coll(blk)

    kinds = (["b"] if do_b else []) + (["a"] if do_a else [])
    last_kind = kinds[-1]
    units = [unit(kd, h, blk) for blk in range(nblk) for h in range(2) for kd in kinds]
    PRE_PER_STEP = 24

    def run_pre(g):
        for v in g:
            if v == 'REC':
                return True
        return False

    nU = len(units)
    ready = [False] * nU
    is_a = [(kd == "a") for blk in range(nblk) for h in range(2) for kd in kinds]

    def adv(ui_, n):
        c = 0
        while c < n and not ready[ui_]:
            v2 = next(units[ui_], 'END')
            c += 1
            if v2 == 'REC' or v2 == 'END':
                ready[ui_] = True
        return c

    adv(0, 10 ** 9)
    for ui in range(nU):
        for v in units[ui]:
            budget = PRE_PER_STEP
            if ui + 1 < nU:
                budget -= adv(ui + 1, budget)
            if budget > 0 and is_a[ui] and ui + 2 < nU and ready[ui + 1] and is_a[ui + 2]:
                adv(ui + 2, budget)
        if ui + 1 < nU:
            adv(ui + 1, 10 ** 9)

    if phase3:
        NT3 = SEQ // 4
        wG_d = nc.dram_tensor("wG", [D, 2048], F32, kind="ExternalInput")
        wBa_d = nc.dram_tensor("wBa", [D, D], F32, kind="ExternalInput")
        wBb_d = nc.dram_tensor("wBb", [D, D], F32, kind="ExternalInput")
        wO_d = nc.dram_tensor("wO", [D, D], F32, kind="ExternalInput")
        wPQ_d = nc.dram_tensor("wPQ", [D, 2048], F32, kind="ExternalInput")
        skT_d = nc.dram_tensor("skT", [128, 2048], F32, kind="ExternalInput")
        n2b_d = nc.dram_tensor("n2b", [128, D], F32, kind="ExternalInput")
        fnb_d = nc.dram_tensor("fnb", [128, D], F32, kind="ExternalInput")

        x3_d = nc.dram_tensor("x3", [NT3, D], F32, kind="ExternalInput")
        sel_d = nc.dram_tensor("sel", [128, 4], F32, kind="ExternalInput")
        out_d = k.dram("out", [NT3, D], F32, kind="ExternalOutput")
        x1_scr = k.dram("x1_scr", [NT3, D], F32)
        k.barrier()
        es1.close()

        if stop3 == "gather":
            k.finish()
            return nc, es
        es3 = ExitStack()
        k.es = es3
        wst2 = k.sb("wst2", [128, 2, 8, 256], F32, nparts=2)
        ld_i = [0]

        def load_w(dst, src_d, ncols):
            v = src_d.ap().rearrange("(kc p) c -> p kc c", p=128)
            for i in range(ncols // 256):
                s_ = ld_i[0] % 2
                ld_i[0] += 1
                k.dma("sp", wst2[:, s_, :, :], v[:, :, i * 256:(i + 1) * 256], writes=[wst2.p[s_]])
                if s_:
                    k.op("act", lambda e: e.activation(out=dst[:, :, i * 256:(i + 1) * 256], in_=wst2[:, s_, :, :], func=AF.Copy),
                         [wst2.p[s_]], [dst])
                else:
                    k.op("dve", lambda e: e.tensor_copy(dst[:, :, i * 256:(i + 1) * 256], wst2[:, s_, :, :]), [wst2.p[s_]], [dst])

        Wg = k.sb("Wg", [128, 8, 2048], BF16)
        Wa = k.sb("Wa", [128, 8, D], BF16)
        Wb = k.sb("Wb", [128, 8, D], BF16)
        Wo = k.sb("Wo", [128, 8, D], BF16)
        load_w(Wg, wG_d, 2048)
        load_w(Wa, wBa_d, D)
        load_w(Wb, wBb_d, D)
        load_w(Wo, wO_d, D)
        sga = k.sb("sga", [128, 8, BLK], BF16)
        sgb = k.sb("sgb", [128, 8, BLK], BF16)
        oaT = k.sb("oaT", [128, 8, BLK], BF16)
        obT = k.sb("obT", [128, 8, BLK], BF16)
        mixT = k.sb("mixT", [128, 8, BLK], BF16)
        m_t1 = k.sb("m_t1", [128, BLK], F32)
        m_t2 = k.sb("m_t2", [128, BLK], F32)
        xr = k.sb("xr", [128, D], F32)
        x1t = k.sb("x1t", [128, D], F32)
        sel = k.sb("sel", [128, 4], F32)
        k.dma("sp", sel[:], sel_d.ap()[:, :], writes=[sel])
        gv = [g_.t.ap().rearrange("(r ab hl p) t -> ab p r hl t", r=4, ab=2, hl=2, p=128) for g_ in gath]
        x3_v = x3_d.ap()
        cand = k.sb("cand", [128, 2, 8, BLK], BF16, nparts=8)
        for blk in range(NT3 // BLK):
            for _ in front(blk * BLK, x3_v):
                pass
            ci = 0
            for ab, dst in ((0, oaT), (1, obT)):
                for q in range(4):
                    s_ = ci % 2
                    ci += 1
                    gi = q * 4 + blk
                    for r in range(4):
                        k.dma("sp", cand[:, s_, 2 * r:2 * r + 2, :], gv[gi][ab][:, r, :, :],
                              reads=[gath[gi]], writes=[cand.p[s_ * 4 + r]])
                    if q == 0:
                        k.op("dve", lambda e: e.tensor_scalar(out=dst[:], in0=cand[:, s_, :, :], scalar1=sel[:, 0:1], scalar2=None, op0=ALU.mult),
                             [cand.p[s_ * 4:s_ * 4 + 4], sel], [dst])
                    else:
                        k.op("dve", lambda e: e.scalar_tensor_tensor(out=dst[:], in0=cand[:, s_, :, :], scalar=sel[:, q:q + 1], in1=dst[:],
                                                                     op0=ALU.mult, op1=ALU.add), [cand.p[s_ * 4:s_ * 4 + 4], sel, dst], [dst])
            for cb in range(8):
                p = proj(cb * 128, Wg)
                k.op("act", lambda e: e.activation(out=sga[:, cb, :], in_=p[:], func=AF.Sigmoid), [p], [sga])
                p = proj(D + cb * 128, Wg)
                k.op("act", lambda e: e.activation(out=sgb[:, cb, :], in_=p[:], func=AF.Sigmoid), [p], [sgb])
            for cb in range(8):
                for kc in range(8):
                    k.op("pe", lambda e: e.matmul(pm[:], lhsT=Wa[:, kc, cb * 128:(cb + 1) * 128], rhs=oaT[:, kc, :],
                                                  start=(kc == 0), stop=(kc == 7)), [Wa, oaT], [pm])
                k.op("dve", lambda e: e.tensor_tensor(out=m_t1[:], in0=pm[:], in1=sga[:, cb, :], op=ALU.mult), [pm, sga], [m_t1])
                for kc in range(8):
                    k.op("pe", lambda e: e.matmul(pm[:], lhsT=Wb[:, kc, cb * 128:(cb + 1) * 128], rhs=obT[:, kc, :],
                                                  start=(kc == 0), stop=(kc == 7)), [Wb, obT], [pm])
                k.op("dve", lambda e: e.tensor_tensor(out=m_t2[:], in0=pm[:], in1=sgb[:, cb, :], op=ALU.mult), [pm, sgb], [m_t2])
                k.op("dve", lambda e: e.tensor_tensor(out=mixT[:, cb, :], in0=m_t1[:], in1=m_t2[:], op=ALU.add), [m_t1, m_t2], [mixT])
            for tt in range(4):
                r0 = blk * BLK + tt * 128
                k.dma("sp", xr[:], x3_v[r0:r0 + 128, :], writes=[xr])
                for half in range(2):
                    p = pp[0]
                    for kc in range(8):
                        k.op("pe", lambda e: e.matmul(p[:], lhsT=mixT[:, kc, tt * 128:(tt + 1) * 128], rhs=Wo[:, kc, half * 512:(half + 1) * 512],
                                                      start=(kc == 0), stop=(kc == 7)), [mixT, Wo], [p])
                    k.op("dve", lambda e: e.tensor_tensor(out=x1t[:, half * 512:(half + 1) * 512], in0=p[:], in1=xr[:, half * 512:(half + 1) * 512],
                                                          op=ALU.add), [p, xr], [x1t])
                k.dma("sp", x1_scr[r0:r0 + 128, :], x1t[:], reads=[x1t], writes=[x1_scr])
                if "x1" in dbg:
                    dump("x1", x1t, x1t[:], dbg_d["x1"][r0:r0 + 128, :])
        k.barrier()
        es3.close()
        if stop3 == "3a":
            k.finish()
            return nc, es

        es4 = ExitStack()
        k.es = es4
        Wpq = k.sb("Wpq", [128, 8, 2048], BF16)
        wpq_v = wPQ_d.ap().rearrange("(kc p) c -> p kc c", p=128)
        for i in range(4):
            k.dma("pool", Wpq[:, :, i * 512:(i + 1) * 512], wpq_v[:, :, i * 512:(i + 1) * 512], writes=[Wpq], nowaw=True)
        skT = k.sb("skTb", [128, 16, 128], BF16)
        k.dma("pool", skT[:, :, :].rearrange("p g k -> p (g k)"), skT_d.ap()[:, :], writes=[skT])
        n2b = k.sb("n2bs", [128, D], F32)
        fnb = k.sb("fnbs", [128, D], F32)
        k.dma("sp", n2b[:], n2b_d.ap()[:, :], writes=[n2b])
        k.dma("sp", fnb[:], fnb_d.ap()[:, :], writes=[fnb])
        x1p2 = [k.sb(f"x1p{i}", [128, D], F32) for i in range(2)]
        h2n2 = [k.sb(f"h2n{i}", [128, D], F32) for i in range(2)]
        h2b2 = [k.sb(f"h2b{i}", [128, D], BF16) for i in range(2)]
        prodb = k.sb("prodb", [128, 2, D], BF16, nparts=2)
        h2T = k.sb("h2T", [128, 8, 128], BF16)
        qTb = k.sb("qTb", [128, 16, 128], BF16)
        s_sb = k.sb("s_sb", [128, 16, 128], F32)
        tv = k.sb("tv", [128, 16, 16], F32)
        ti = k.sb("ti", [128, 16, 16], U32)
        tif = k.sb("tif", [128, 16, 16], F32)
        cs_ = k.sb("cand_s", [128, 8, 256], F32)
        bs_ = k.sb("best_s", [128, 8, 16], F32)
        pos = k.sb("pos", [128, 8, 16], U32)
        posf = k.sb("posf", [128, 8, 16], F32)
        pa_ = k.sb("pa_", [128, 8, 16], F32)
        pb_ = k.sb("pb_", [128, 8, 16], F32)
        eq = k.sb("eq", [128, 8, 16, 16], F32)
        i1 = k.sb("i1", [128, 8, 16], F32)
        i2 = k.sb("i2", [128, 8, 16], F32)
        idxf = k.sb("idxf", [128, 128], F32)
        idxu2 = [k.sb(f"idxu{i}", [128, 128], U32) for i in range(2)]
        gsm = k.sb("gsm", [128, 16], F32)
        gate2 = [k.sb(f"gate{i}", [128, 8, 16], F32) for i in range(2)]
        gatef2 = [g_.t[:, :, :].rearrange("p h k -> p (h k)") for g_ in gate2]
        BG_PER_STEP = 8
        NG = 14
        GS = 4
        NGR = 128 // GS
        hpre = k.sb("hpre", [128, 128], F32, nparts=32)
        gcol = k.sb("gcol", [128, 128], F32, nparts=8)
        gw = k.sb("gw", [128, 128], F32, nparts=8)
        UVg = k.sb("UVg", [128, NG, 2 * D], BF16, nparts=NG)
        Dm = k.sb("Dm", [128, NG, 128], BF16, nparts=NG)
        scrU = k.sb("scrU", [128, D], BF16)
        x2t = k.sb("x2t", [128, D], F32)
        obuf = k.sb("obuf", [128, D], F32)
        print("sbuf bytes remaining (3b):", nc.sbuf_bytes_remaining)
        ps_s = k.ps("ps_s", [128, 16, 128], F32, nparts=4)
        py1 = k.ps("py1", [128, 512], F32)
        pq = pm
        py0 = pp[0]
        IOTA = cmat.t[:, CM_IOTA:CM_IOTA + 16]
        THR = cmat.t[:, CM_THR:CM_THR + 16]
        tv4 = tv.t[:, :, :].rearrange("p (h two) k -> p h two k", two=2)
        tif4 = tif.t[:, :, :].rearrange("p (h two) k -> p h two k", two=2)
        euv_v = euvb.t.ap()
        def tile_front(tile_i):
            pb_i = tile_i % 2
            x1p, h2n, idxu, gate, gatef = x1p2[pb_i], h2n2[pb_i], idxu2[pb_i], gate2[pb_i], gatef2[pb_i]
            h2b = h2b2[pb_i]
            r0 = tile_i * 128
            k.dma("sp", x1p[:], x1_scr[r0:r0 + 128, :], reads=[x1_scr], writes=[x1p])
            yield
            k.op("act", lambda e: e.activation(out=junk[:], in_=x1p[:], func=AF.Square, accum_out=st[:, 0:1]), [x1p], [junk, st.p[0]])
            yield
            k.op("act", lambda e: e.activation(out=st[:, 1:2], in_=st[:, 0:1], func=AF.Sqrt, bias=epsc, scale=1.0 / D), [st.p[0], cder], [st.p[0]])
            yield
            k.op("dve", lambda e: e.reciprocal(out=st[:, 1:2], in_=st[:, 1:2]), [st.p[0]], [st.p[0]])
            yield
            k.op("dve", lambda e: e.scalar_tensor_tensor(out=h2n[:], in0=x1p[:], scalar=st[:, 1:2], in1=n2b[:], op0=ALU.mult, op1=ALU.mult),
                 [x1p, st.p[0], n2b], [h2n])
            yield
            k.op("act", lambda e: e.activation(out=h2b[:], in_=h2n[:], func=AF.Copy), [h2n], [h2b])
            yield
            for kc in range(8):
                k.op("pe", lambda e: e.transpose(out=pT[:, kc, :], in_=h2b[:, kc * 128:(kc + 1) * 128], identity=ident_bf[:]), [h2b, ident_bf], [pT])
                yield
            k.op("act", lambda e: e.activation(out=h2T[:], in_=pT[:], func=AF.Copy), [pT], [h2T])
            yield
            for g4 in range(4):
                for gg in range(4):
                    g = g4 * 4 + gg
                    for kc in range(8):
                        k.op("pe", lambda e: e.matmul(pq[:, gg * 128:(gg + 1) * 128], lhsT=Wpq[:, kc, g * 128:(g + 1) * 128], rhs=h2T[:, kc, :],
                                                      start=(kc == 0), stop=(kc == 7)), [Wpq, h2T], [pq])
                k.op("act", lambda e: e.activation(out=qTb[:, g4 * 4:(g4 + 1) * 4, :], in_=pq[:, :].rearrange("p (g t) -> p g t", t=128),
                                                   func=AF.Copy), [pq], [qTb])
                yield
            for g in range(16):
                k.op("pe", lambda e: e.matmul(ps_s[:, g, :], lhsT=qTb[:, g, :], rhs=skT[:, g, :], start=True, stop=True), [qTb, skT], [ps_s.p[g // 4]])
                yield
            for q4 in range(4):
                k.op("act", lambda e: e.activation(out=s_sb[:, q4 * 4:(q4 + 1) * 4, :], in_=ps_s[:, q4 * 4:(q4 + 1) * 4, :], func=AF.Copy),
                     [ps_s.p[q4]], [s_sb])
                yield
            for g in range(16):
                k.op("dve", lambda e: e.max(out=tv[:, g, 0:8], in_=s_sb[:, g, :]), [s_sb], [tv])
                yield
                k.op("dve", lambda e: e.max_index(out=ti[:, g, 0:8], in_max=tv[:, g, 0:8], in_values=s_sb[:, g, :]), [tv, s_sb], [ti])
                yield
                k.op("dve", lambda e: e.match_replace(out=s_sb[:, g, :], in_to_replace=tv[:, g, 0:8], in_values=s_sb[:, g, :], imm_value=-1e30),
                     [tv, s_sb], [s_sb])
                yield
                k.op("dve", lambda e: e.max(out=tv[:, g, 8:16], in_=s_sb[:, g, :]), [s_sb], [tv])
                yield
                k.op("dve", lambda e: e.max_index(out=ti[:, g, 8:16], in_max=tv[:, g, 8:16], in_values=s_sb[:, g, :]), [tv, s_sb], [ti])
                yield
            k.op("dve", lambda e: e.tensor_copy(tif[:], ti[:]), [ti], [tif])
            yield
            cs4 = cs_.t[:, :, :].rearrange("p h (a b) -> p h a b", b=16)
            k.op("dve", lambda e: e.tensor_tensor(out=cs4, in0=tv4[:, :, 0, :].unsqueeze(3).to_broadcast([128, 8, 16, 16]),
                                                  in1=tv4[:, :, 1, :].unsqueeze(2).to_broadcast([128, 8, 16, 16]), op=ALU.add), [tv], [cs_])
            yield
            for h in range(8):
                k.op("dve", lambda e: e.max(out=bs_[:, h, 0:8], in_=cs_[:, h, :]), [cs_], [bs_])
                yield
                k.op("dve", lambda e: e.max_index(out=pos[:, h, 0:8], in_max=bs_[:, h, 0:8], in_values=cs_[:, h, :]), [bs_, cs_], [pos])
                yield
                k.op("dve", lambda e: e.match_replace(out=cs_[:, h, :], in_to_replace=bs_[:, h, 0:8], in_values=cs_[:, h, :], imm_value=-1e30),
                     [bs_, cs_], [cs_])
                yield
                k.op("dve", lambda e: e.max(out=bs_[:, h, 8:16], in_=cs_[:, h, :]), [cs_], [bs_])
                yield
                k.op("dve", lambda e: e.max_index(out=pos[:, h, 8:16], in_max=bs_[:, h, 8:16], in_values=cs_[:, h, :]), [bs_, cs_], [pos])
                yield
            k.op("dve", lambda e: e.tensor_copy(posf[:], pos[:]), [pos], [posf])
            yield
            k.op("dve", lambda e: e.tensor_tensor(out=eq[:], in0=posf.t[:, :, :].unsqueeze(3).to_broadcast([128, 8, 16, 16]),
                                                  in1=THR.unsqueeze(1).unsqueeze(1).to_broadcast([128, 8, 16, 16]), op=ALU.is_ge), [posf, cmat], [eq])
            yield
            k.op("dve", lambda e: e.tensor_reduce(out=pa_[:], in_=eq[:], axis=AX.X, op=ALU.add), [eq], [pa_])
            yield
            k.op("dve", lambda e: e.scalar_tensor_tensor(out=pb_[:], in0=pa_[:], scalar=-16.0, in1=posf[:], op0=ALU.mult, op1=ALU.add),
                 [pa_, posf], [pb_])
            yield
            for (pp_, half, dst) in ((pa_, 0, i1), (pb_, 1, i2)):
                k.op("dve", lambda e: e.tensor_tensor(out=eq[:], in0=pp_.t[:, :, :].unsqueeze(3).to_broadcast([128, 8, 16, 16]),
                                                      in1=IOTA.unsqueeze(1).unsqueeze(1).to_broadcast([128, 8, 16, 16]), op=ALU.is_equal),
                     [pp_, cmat], [eq])
                yield
                k.op("dve", lambda e: e.tensor_tensor(out=eq[:], in0=eq[:], in1=tif4[:, :, half, :].unsqueeze(2).to_broadcast([128, 8, 16, 16]),
                                                      op=ALU.mult), [eq, tif], [eq])
                yield
                k.op("dve", lambda e: e.tensor_reduce(out=dst[:], in_=eq[:], axis=AX.X, op=ALU.add), [eq], [dst])
                yield
            k.op("dve", lambda e: e.scalar_tensor_tensor(out=idxf[:, :].rearrange("p (h k) -> p h k", k=16), in0=i1[:], scalar=128.0, in1=i2[:],
                                                         op0=ALU.mult, op1=ALU.add), [i1, i2], [idxf])
            yield
            k.op("dve", lambda e: e.tensor_copy(idxu[:], idxf[:]), [idxf], [idxu])
            yield
            k.op("dve", lambda e: e.tensor_tensor(out=gate[:], in0=bs_[:], in1=bs_[:, :, 0:1].to_broadcast([128, 8, 16]), op=ALU.subtract), [bs_], [gate])
            yield
            k.op("act", lambda e: e.activation(out=gate[:], in_=gate[:], func=AF.Exp), [gate], [gate])
            yield
            k.op("dve", lambda e: e.tensor_reduce(out=gsm[:, 0:8], in_=gate[:], axis=AX.X, op=ALU.add), [gate], [gsm])
            yield
            k.op("dve", lambda e: e.reciprocal(out=gsm[:, 8:16], in_=gsm[:, 0:8]), [gsm], [gsm])
            yield
            k.op("dve", lambda e: e.tensor_tensor(out=gate[:], in0=gate[:], in1=gsm[:, 8:16].unsqueeze(2).to_broadcast([128, 8, 16]), op=ALU.mult),
                 [gate, gsm], [gate])
            yield

            yield

        def tile_slots(tile_i):
            pb_i = tile_i % 2
            x1p, h2n, idxu, gate, gatef = x1p2[pb_i], h2n2[pb_i], idxu2[pb_i], gate2[pb_i], gatef2[pb_i]
            h2b = h2b2[pb_i]
            r0 = tile_i * 128
            def vside(gr):
                lf = gr % 8
                c0_, c1_ = gr * GS, (gr + 1) * GS
                for sl in range(c0_, c1_):
                    j = sl % NG
                    k.op("act", lambda e: e.activation(out=gw[:, sl:sl + 1], in_=gcol[:, sl:sl + 1], func=AF.Copy, scale=gatef[:, sl:sl + 1]),
                         [gcol.p[lf], gate], [gw.p[lf]])
                    k.op("act", lambda e: e.activation(out=Dm[:, j, :], in_=ident_bf[:], func=AF.Copy, scale=gw[:, sl:sl + 1]),
                         [ident_bf, gw.p[lf]], [Dm.p[j]])
                    k.op("pe", lambda e: e.matmul(py0[:], lhsT=Dm[:, j, :], rhs=UVg[:, j, D:D + 512], start=(sl == 0), stop=(sl == 127)),
                         [Dm.p[j], UVg.p[j]], [py0])
                    k.op("pe", lambda e: e.matmul(py1[:], lhsT=Dm[:, j, :], rhs=UVg[:, j, D + 512:2 * D], start=(sl == 0), stop=(sl == 127)),
                         [Dm.p[j], UVg.p[j]], [py1])

            for gr in range(NGR):
                lf = gr % 8
                for sl in range(gr * GS, (gr + 1) * GS):
                    j = sl % NG
                    k.dma("pool", None, None, reads=[idxu, euvb], writes=[UVg.p[j]],
                          fn=lambda e: e.indirect_dma_start(out=UVg[:, j, :], out_offset=None, in_=euv_v[:, :],
                                                            in_offset=bass.IndirectOffsetOnAxis(ap=idxu[:, sl:sl + 1], axis=0)))
                    if sl % 2 == 1:
                        pj = (sl // 2) % 2
                        k.op("dve", lambda e: e.tensor_tensor(out=prodb[:, pj, :], in0=UVg[:, j, 0:D], in1=h2b[:], op=ALU.mult),
                             [UVg.p[j], h2b], [prodb.p[pj]])
                        k.op("act", lambda e: e.activation(out=junk[:], in_=prodb[:, pj, :], func=AF.Copy, accum_out=hpre[:, sl:sl + 1]),
                             [prodb.p[pj]], [junk, hpre.p[sl % 32]])
                    else:
                        k.op("dve", lambda e: e.scalar_tensor_tensor(out=scrU[:], in0=UVg[:, j, 0:D], scalar=1.0, in1=h2n[:], op0=ALU.mult,
                                                                     op1=ALU.mult, accum_out=hpre[:, sl:sl + 1]),
                             [UVg.p[j], h2n], [scrU, hpre.p[sl % 32]])
                k.op("act", lambda e: e.activation(out=gcol[:, gr * GS:(gr + 1) * GS], in_=hpre[:, gr * GS:(gr + 1) * GS], func=AF.Gelu),
                     [hpre.p[(gr * GS) % 32:(gr * GS) % 32 + GS]], [gcol.p[lf]])
                vside(gr)
                yield
            k.op("dve", lambda e: e.tensor_tensor(out=x2t[:, 0:512], in0=py0[:], in1=x1p[:, 0:512], op=ALU.add), [py0, x1p], [x2t])
            k.op("dve", lambda e: e.tensor_tensor(out=x2t[:, 512:D], in0=py1[:], in1=x1p[:, 512:D], op=ALU.add), [py1, x1p], [x2t])
            if "x2" in dbg:
                dump("x2", x2t, x2t[:], dbg_d["x2"][r0:r0 + 128, :])
            k.op("act", lambda e: e.activation(out=junk[:], in_=x2t[:], func=AF.Square, accum_out=st[:, 2:3]), [x2t], [junk, st.p[1]])
            k.op("act", lambda e: e.activation(out=st[:, 3:4], in_=st[:, 2:3], func=AF.Sqrt, bias=epsc, scale=1.0 / D), [st.p[1], cder], [st.p[1]])
            k.op("dve", lambda e: e.reciprocal(out=st[:, 3:4], in_=st[:, 3:4]), [st.p[1]], [st.p[1]])
            k.op("dve", lambda e: e.scalar_tensor_tensor(out=obuf[:], in0=x2t[:], scalar=st[:, 3:4], in1=fnb[:], op0=ALU.mult, op1=ALU.mult),
                 [x2t, st.p[1], fnb], [obuf])
            k.dma("sp", out_d[r0:r0 + 128, :], obuf[:], reads=[obuf], writes=[out_d])

            yield

        fr = tile_front(0)
        for _ in fr:
            pass
        for tile_i in range(ntile3):
            bg = tile_front(tile_i + 1) if tile_i + 1 < ntile3 else None
            for _ in tile_slots(tile_i):
                if bg is not None:
                    for _n in range(BG_PER_STEP):
                        if next(bg, 'end') == 'end':
                            bg = None
                            break
            if bg is not None:
                for _ in bg:
                    pass

    k.finish()
    print("instructions:", k.n_ins, "dma sems:", len(k.dsems))
    return nc, es


def host_prep(inputs):
    x = np.asarray(inputs["x"], np.float32)
    w_in = np.asarray(inputs["w_in"], np.float32)[0]
    conv = np.asarray(inputs["conv_a"], np.float32)[0]
    cmat = np.zeros((128, NCM), np.float32)
    cmat[:, CM_ID:CM_ID + 128] = np.eye(128, dtype=np.float32)
    i = np.arange(64)
    cmat[0:64, CM_ML:CM_ML + 64] = (i[:, None] > i[None, :])
    cmat[0:64, CM_MUS:CM_MUS + 64] = (i[:, None] < i[None, :])
    cmat[0:64, CM_MUI:CM_MUI + 64] = (i[:, None] <= i[None, :])
    cmat[0:64, CM_I64:CM_I64 + 64] = np.eye(64, dtype=np.float32)
    cmat[:, CM_RST:CM_RST + 512] = (np.arange(512) % 64 != 0)[None, :]
    cmat[:, CM_ONE:CM_ONE + 128] = 1.0
    cmat[:, CM_IOTA:CM_IOTA + 16] = np.arange(16, dtype=np.float32)[None, :]
    thr = 16.0 * (np.arange(16, dtype=np.float32) + 1.0)
    thr[15] = 1e9
    cmat[:, CM_THR:CM_THR + 16] = thr[None, :]
    offs = {CB_AQ: 0, CB_AK: 1024, CB_AV: 2048, CB_AGATE: 3088, CB_BF: 4112, CB_BI: 5136, CB_BQ: 6160, CB_BGATE: 7184}
    w_pq = np.asarray(inputs["w_pq"], np.float32)[0]
    sk = np.asarray(inputs["sub_keys"], np.float32)[0]
    skT = np.ascontiguousarray(sk.transpose(3, 1, 0, 2).reshape(128, 16 * 128))
    shared = {
        "wG": np.ascontiguousarray(w_in[:, 8208:8208 + 2048]),
        "wBa": np.ascontiguousarray(inputs["w_branch_a"][0]), "wBb": np.ascontiguousarray(inputs["w_branch_b"][0]),
        "wO": np.ascontiguousarray(inputs["w_out"][0]), "wPQ": w_pq, "skT": skT,
        "n2b": np.ascontiguousarray(np.broadcast_to(inputs["norm2"][0][None, :], (128, D))),
        "fnb": np.ascontiguousarray(np.broadcast_to(inputs["final_norm"][None, :], (128, D))),
        "euv": np.concatenate([inputs["expert_u"][0], inputs["expert_v"][0]], axis=1),
    }
    maps = []
    for c in range(NCORE):
        b, hp = c // 4, c % 4
        wA = np.empty((D, NWA), np.float32)
        cvec = np.zeros((128, NCV), np.float32)
        cvec[:, CV_N1:CV_N1 + 8] = inputs["norm1"][0].reshape(8, 128).T
        cvec[:, CV_N2:CV_N2 + 8] = inputs["norm2"][0].reshape(8, 128).T
        cvec[:, CV_AON] = inputs["a_onorm"][0]
        cvec[:, CV_BON] = inputs["b_onorm"][0]
        for hl in range(2):
            h = 2 * hp + hl
            for cb, o in offs.items():
                wA[:, hl * 1280 + cb * 128: hl * 1280 + (cb + 1) * 128] = w_in[:, o + 128 * h: o + 128 * (h + 1)]
            wA[:, hl * 1280 + CB_ABETA * 128: hl * 1280 + (CB_ABETA + 1) * 128] = w_in[:, 3072 + h: 3073 + h]
            wA[:, hl * 1280 + CB_AALPHA * 128: hl * 1280 + (CB_AALPHA + 1) * 128] = w_in[:, 3080 + h: 3081 + h]
            for qi in range(3):
                cvec[:, CV_CONV + hl * 12 + qi * 4: CV_CONV + hl * 12 + qi * 4 + 4] = conv[:, qi * 1024 + 128 * h: qi * 1024 + 128 * (h + 1)].T
            cvec[:, CV_ALOG + hl] = inputs["a_log"][0, h]
            cvec[:, CV_DTB + hl] = inputs["dt_bias"][0, h]
            cvec[:, CV_BLB + 2 * hl] = inputs["b_lower_bound"][0, 128 * h:128 * (h + 1)]
            cvec[:, CV_BLB + 2 * hl + 1] = inputs["b_lower_bound"][1, 128 * h:128 * (h + 1)]
        ts = c % 4
        sel = np.zeros((128, 4), np.float32)
        sel[:, ts] = 1.0
        m = {"x": np.ascontiguousarray(x[b]), "wA": wA, "cvec": cvec, "cmat": cmat,
             "x3": np.ascontiguousarray(x[b, ts * 2048:(ts + 1) * 2048]), "sel": sel}
        m.update(shared)
        maps.append(m)
    return maps


_CACHE = {}


def kernel(**inputs):
    inputs = {k_: np.asarray(v) for k_, v in inputs.items()}
    maps = host_prep(inputs)
    if "nc" not in _CACHE:
        _CACHE["nc"] = build()
    nc, _ = _CACHE["nc"]
    res = run_bass_kernel_spmd(nc, maps, core_ids=list(range(NCORE)))
    out = np.empty((2, SEQ, D), np.float32)
    for c in range(NCORE):
        b, ts = c // 4, c % 4
        out[b, ts * 2048:(ts + 1) * 2048] = res.results[c]["out"]
    return out
```

```python
import numpy as np
from contextlib import ExitStack
import concourse.bass as bass
import concourse.mybir as mybir
from concourse.bass_utils import run_bass_kernel_spmd

F32 = mybir.dt.float32
BF16 = mybir.dt.bfloat16
I32 = mybir.dt.int32
U32 = mybir.dt.uint32
ALU = mybir.AluOpType
AF = mybir.ActivationFunctionType
AX = mybir.AxisListType

D = 1024
SEQ = 8192
NCORE = 8
BLK = 512
CH = 64
NCH = BLK // CH
EPS = 1e-6
NWA = 2560
CB_AQ, CB_AK, CB_AV, CB_ABETA, CB_AALPHA, CB_AGATE, CB_BF, CB_BI, CB_BQ, CB_BGATE = range(10)
CV_N1, CV_N2, CV_CONV, CV_ALOG, CV_DTB, CV_AON, CV_BON, CV_BLB = 0, 8, 16, 40, 42, 44, 45, 46
NCV = 50
CM_ID, CM_ML, CM_MUS, CM_MUI, CM_I64, CM_RST, CM_ONE, CM_IOTA, CM_THR = 0, 128, 192, 256, 320, 384, 896, 1024, 1040
NCM = 1056


class Reg:
    __slots__ = ("name", "w", "r", "dsem", "dcnt")

    def __init__(self, name):
        self.name = name
        self.w = None
        self.r = []
        self.dsem = None
        self.dcnt = 0


class Tile:
    def __init__(self, t, name, nparts=1):
        self.t = t
        self.name = name
        self.p = [Reg(f"{name}.{i}") for i in range(nparts)]

    def __getitem__(self, idx):
        return self.t[idx]


def _leaves(xs):
    out = []
    for x in xs:
        if x is None:
            continue
        if isinstance(x, Tile):
            out.extend(x.p)
        elif isinstance(x, (list, tuple)):
            out.extend(_leaves(x))
        else:
            out.append(x)
    return out


class K:
    def __init__(self, nc, es):
        self.nc = nc
        self.es = es
        self.es_sem = es
        self.eng = {"pe": nc.tensor, "dve": nc.vector, "act": nc.scalar, "pool": nc.gpsimd, "sp": nc.sync}
        self.sem = {}
        self.cnt = {}
        for e in self.eng:
            self.sem[e] = es.enter_context(nc.semaphore(f"s_{e}"))
            self.cnt[e] = 0
        self.seen = {e: {} for e in self.eng}
        self.dsems = []
        self.n_ins = 0

    def sb(self, name, shape, dt, nparts=1):
        t = self.es.enter_context(self.nc.sbuf_tensor("sb_" + name, list(shape), dt))
        return Tile(t, name, nparts)

    def ps(self, name, shape, dt=F32, nparts=1):
        t = self.es.enter_context(self.nc.psum_tensor("ps_" + name, list(shape), dt))
        return Tile(t, name, nparts)

    def dram(self, name, shape, dt, kind="Internal", nparts=1):
        t = self.nc.dram_tensor(name, list(shape), dt, kind=kind)
        return Tile(t, name, nparts)

    def _wait(self, e, toks):
        E = self.eng[e]
        seen = self.seen[e]
        best = {}
        for (sem, val, owner) in toks:
            k = id(sem)
            if best.get(k, (None, 0))[1] < val:
                best[k] = (sem, val)
        for k, (sem, val) in best.items():
            if seen.get(k, 0) < val:
                E.wait_ge(sem, val)
                seen[k] = val

    def _deps(self, e, reads, writes):
        toks = []
        for r in reads:
            if r.w is not None:
                if r.w[2] == e and e == "pe":
                    continue
                toks.append(r.w)
        for w in writes:
            if w.w is not None and (w.w[2] != e or e != "pe"):
                toks.append(w.w)
            for t in w.r:
                if t[2] != e or e != "pe":
                    toks.append(t)
        return toks

    def op(self, e, fn, reads=(), writes=()):
        reads = _leaves(reads)
        writes = _leaves(writes)
        self._wait(e, self._deps(e, reads, writes))
        ins = fn(self.eng[e])
        self.cnt[e] += 1
        ins.then_inc(self.sem[e], 1)
        tok = (self.sem[e], self.cnt[e], e)
        for w in writes:
            w.w = tok
            w.r = []
        for r in reads:
            r.r.append(tok)
        self.n_ins += 1
        return ins

    def _dsem(self, reg):
        if reg.dsem is None:
            reg.dsem = self.es_sem.enter_context(self.nc.semaphore(f"d{len(self.dsems)}"))
            self.dsems.append(reg)
        return reg.dsem

    def dma(self, q, out, in_, reads=(), writes=(), fn=None, inc=16, nowaw=False, **kw):
        reads = _leaves(reads)
        writes = _leaves(writes)
        deps = self._deps("dma", reads, writes)
        if nowaw:
            mine = {id(w.dsem) for w in writes if w.dsem is not None}
            deps = [t for t in deps if not (t[2] == "dma" and id(t[0]) in mine)]
        self._wait(q, deps)
        holder = writes[0] if writes else reads[0]
        sem = self._dsem(holder)
        if fn is None:
            ins = self.eng[q].dma_start(out=out, in_=in_, **kw)
        else:
            ins = fn(self.eng[q])
        ins.then_inc(sem, inc)
        holder.dcnt += inc
        tok = (sem, holder.dcnt, "dma")
        for w in writes:
            w.w = tok
            w.r = []
        for r in reads:
            r.r.append(tok)
        self.n_ins += 1
        return ins

    def barrier(self):
        toks = [(self.sem[e], self.cnt[e], e) for e in self.eng if self.cnt[e] > 0]
        toks += [(r.dsem, r.dcnt, "dma") for r in self.dsems if r.dcnt > 0]
        for e in self.eng:
            self._wait(e, [t for t in toks if t[2] != e])

    def finish(self):
        toks = [(r.dsem, r.dcnt, "dma") for r in self.dsems if r.dcnt > 0]
        toks += [(self.sem[e], self.cnt[e], e) for e in self.eng if self.cnt[e] > 0 and e != "sp"]
        self._wait("sp", toks)


def build(nblk=SEQ // BLK, dbg=None, phase3=True, do_a=True, do_b=True, ntile3=16, stop3=None):
    nc = bass.Bass("TRN2", target_bir_lowering=False)
    es = ExitStack()
    k = K(nc, es)
    dbg = dbg or {}

    x_d = nc.dram_tensor("x", [SEQ, D], F32, kind="ExternalInput")
    wA_d = nc.dram_tensor("wA", [D, NWA], F32, kind="ExternalInput")
    cvec_d = nc.dram_tensor("cvec", [128, NCV], F32, kind="ExternalInput")
    cmat_d = nc.dram_tensor("cmat", [128, NCM], F32, kind="ExternalInput")
    dbg_d = {}
    for name, shape in dbg.items():
        dbg_d[name] = k.dram(name, shape, F32, kind="ExternalOutput")

    cvec = k.sb("cvec", [128, NCV], F32)
    cmat = k.sb("cmat", [128, NCM], F32)
    k.dma("sp", cvec[:], cvec_d.ap()[:, :], writes=[cvec])
    k.dma("sp", cmat[:], cmat_d.ap()[:, :], writes=[cmat])
    ident_bf = k.sb("ident_bf", [128, 128], BF16)
    ones_bf = k.sb("ones_bf", [128, 128], BF16)
    k.op("dve", lambda e: e.tensor_copy(ident_bf[:], cmat[:, CM_ID:CM_ID + 128]), [cmat], [ident_bf])
    k.op("dve", lambda e: e.tensor_copy(ones_bf[:], cmat[:, CM_ONE:CM_ONE + 128]), [cmat], [ones_bf])
    ML = cmat.t[0:64, CM_ML:CM_ML + 64]
    MUS = cmat.t[0:64, CM_MUS:CM_MUS + 64]
    MUI = cmat.t[0:64, CM_MUI:CM_MUI + 64]
    I64 = cmat.t[0:64, CM_I64:CM_I64 + 64]
    RST = cmat.t[:, CM_RST:CM_RST + 512]

    def bc8(m):
        return m.unsqueeze(1).to_broadcast([64, NCH, 64])

    cder = k.sb("cder", [128, 8], F32)
    for h in range(2):
        k.op("act", lambda e, h=h: e.activation(out=cder[:, h:h + 1], in_=cvec[:, CV_ALOG + h:CV_ALOG + h + 1], func=AF.Exp),
             [cvec], [cder])
        k.op("dve", lambda e, h=h: e.tensor_scalar(out=cder[:, h:h + 1], in0=cder[:, h:h + 1], scalar1=-1.0, scalar2=None,
                                                   op0=ALU.mult), [cder], [cder])
        k.op("dve", lambda e, h=h: e.tensor_tensor(out=cder[:, 6 + h:7 + h], in0=cvec[:, CV_BLB + 2 * h:CV_BLB + 2 * h + 1],
                                                   in1=cvec[:, CV_BLB + 2 * h + 1:CV_BLB + 2 * h + 2], op=ALU.subtract),
             [cvec], [cder])
        k.op("act", lambda e, h=h: e.activation(out=cder[:, 2 + h:3 + h], in_=cder[:, 6 + h:7 + h], func=AF.Sigmoid),
             [cder], [cder])
        k.op("dve", lambda e, h=h: e.tensor_scalar(out=cder[:, 4 + h:5 + h], in0=cder[:, 2 + h:3 + h], scalar1=-1.0, scalar2=1.0,
                                                   op0=ALU.mult, op1=ALU.add), [cder], [cder])

    k.op("dve", lambda e: e.memset(cder[:, 7:8], EPS), [], [cder])
    epsc = cder[:, 7:8]
    cone = k.sb("cone", [128, 1], F32)
    k.op("dve", lambda e: e.memset(cone[:], 1.0), [], [cone])
    onec = cone[:, 0:1]
    xt = k.sb("xt", [128, 2, D], F32, nparts=2)
    junk = k.sb("junk", [128, D], BF16)
    xn = k.sb("xn", [128, 2, D], BF16, nparts=2)
    st = k.sb("st", [128, 4], F32, nparts=2)
    hT = k.sb("hT", [128, 8, BLK], BF16)

    pT = k.ps("pT", [128, 8, 128], BF16)
    pp = [k.ps("pp0", [128, BLK], F32)]
    pm = k.ps("pm", [128, BLK], F32)
    es1 = ExitStack()
    k.es = es1
    W = k.sb("W", [128, 8, NWA], BF16)
    wA_v = wA_d.ap().rearrange("(kc p) c -> p kc c", p=128)
    for i in range(8):
        k.dma("pool", W[:, :, i * 320:(i + 1) * 320], wA_v[:, :, i * 320:(i + 1) * 320], writes=[W], nowaw=True)
    if phase3 and ntile3 > 0:
        euv_d = nc.dram_tensor("euv", [16384, 2 * D], F32, kind="ExternalInput")
        euvb = k.dram("euvb", [16384, 2 * D], BF16)
        for i in range(16):
            k.dma("pool", euvb[i * 1024:(i + 1) * 1024, :], euv_d.ap()[i * 1024:(i + 1) * 1024, :], writes=[euvb], nowaw=True)

    k.es = es1
    pa = k.ps("pa", [128, BLK], F32)
    pb = k.ps("pb", [128, BLK], F32)
    ptr = k.ps("ptr", [128, NCH, 128], BF16)
    po = k.ps("po", [128, BLK], F32)
    prw = k.ps("prw", [128, 2, 128], F32, nparts=2)

    x_v = x_d.ap()
    pp_i = [0]

    def front(tok0, x_v=x_v):
        for tt in range(4):
            s = tt % 2
            k.dma("sp", xt[:, s, :], x_v[tok0 + tt * 128: tok0 + (tt + 1) * 128, :], writes=[xt.p[s]])
            k.op("act", lambda e: e.activation(out=junk[:], in_=xt[:, s, :], func=AF.Square, accum_out=st[:, 2 * s:2 * s + 1]),
                 [xt.p[s]], [junk, st.p[s]])
            k.op("act", lambda e: e.activation(out=st[:, 2 * s + 1:2 * s + 2], in_=st[:, 2 * s:2 * s + 1], func=AF.Sqrt,
                                               bias=epsc, scale=1.0 / D), [st.p[s], cder], [st.p[s]])
            k.op("dve", lambda e: e.reciprocal(out=st[:, 2 * s + 1:2 * s + 2], in_=st[:, 2 * s + 1:2 * s + 2]), [st.p[s]], [st.p[s]])
            k.op("act", lambda e: e.activation(out=xn[:, s, :], in_=xt[:, s, :], func=AF.Copy, scale=st[:, 2 * s + 1:2 * s + 2]),
                 [xt.p[s], st.p[s]], [xn.p[s]])
            for kc in range(8):
                k.op("pe", lambda e, kc=kc: e.transpose(out=pT[:, kc, :], in_=xn[:, s, kc * 128:(kc + 1) * 128], identity=ident_bf[:]),
                     [xn.p[s], ident_bf], [pT])
            k.op("dve", lambda e: e.tensor_tensor(out=hT[:, :, tt * 128:(tt + 1) * 128], in0=pT[:],
                                                  in1=cvec[:, CV_N1:CV_N1 + 8].unsqueeze(2).to_broadcast([128, 8, 128]),
                                                  op=ALU.mult), [pT, cvec], [hT])
            yield 'p'

    def proj(col0, Wt=None):
        Wt = Wt or W
        p = pp[0]
        for kc in range(8):
            k.op("pe", lambda e, kc=kc: e.matmul(p[:], lhsT=Wt[:, kc, col0:col0 + 128], rhs=hT[:, kc, :],
                                                 start=(kc == 0), stop=(kc == 7)), [Wt, hT], [p])
        return p

    def dump(name, src_tile, src_ap, dst_ap):
        k.dma("sp", dst_ap, src_ap, reads=[src_tile], writes=[dbg_d[name]])

    def T(name, cols=BLK, dt=F32, rows=128):
        return k.sb(name, [rows, cols], dt)

    b_sg, b_lf, b_cs, b_eb, b_enb, b_qs, b_omf = [T(f"b_{n}") for n in ("sg", "lf", "cs", "eb", "enb", "qs", "omf")]
    b_df = b_lf
    b_qe, b_ke, b_kdT, b_vT, b_aT, b_sq, b_ob = [T(f"b_{n}", dt=BF16) for n in ("qe", "ke", "kdT", "vT", "aT", "sq", "ob")]
    b_vtm = k.sb("b_vtm", [64, NCH, 128], BF16)
    b_kd = k.sb("b_kd", [64, NCH, 128], BF16)
    b_gate, b_or, b_rs = T("b_gate"), T("b_or"), T("b_rs")
    b_S = [k.sb(f"b_S{h}", [128, 128], F32) for h in range(2)]
    b_Sb = [k.sb(f"b_Sb{h}", [128, 128], BF16) for h in range(2)]
    for h in range(2):
        k.op("dve", lambda e, h=h: e.memset(b_S[h][:], 0.0), [], [b_S[h]])
        k.op("dve", lambda e, h=h: e.memset(b_Sb[h][:], 0.0), [], [b_Sb[h]])
    NB1 = SEQ // BLK
    o_scr = [k.dram(f"o_scr{q}", [512, BLK], BF16) for q in range(NB1)]
    gath = [k.dram(f"gath{q}", [4 * 512, BLK], BF16) for q in range(NB1)]
    ob_scr, oa_scr = 256, 0

    def out_norm(h, blk, o_raw, gate, onorm_col, scr, sq, rs, ob):
        k.op("act", lambda e: e.activation(out=sq[:], in_=o_raw[:], func=AF.Square), [o_raw], [sq])
        k.op("pe", lambda e: e.matmul(pm[:], lhsT=ones_bf[:], rhs=sq[:], start=True, stop=True), [ones_bf, sq], [pm])
        k.op("act", lambda e: e.activation(out=rs[:], in_=pm[:], func=AF.Sqrt, bias=epsc, scale=1.0 / 128), [pm, cder], [rs])
        k.op("dve", lambda e: e.reciprocal(out=rs[:], in_=rs[:]), [rs], [rs])
        k.op("dve", lambda e: e.scalar_tensor_tensor(out=rs[:], in0=o_raw[:], scalar=cvec[:, onorm_col:onorm_col + 1], in1=rs[:],
                                                     op0=ALU.mult, op1=ALU.mult), [o_raw, cvec, rs], [rs])
        k.op("dve", lambda e: e.tensor_tensor(out=ob[:], in0=rs[:], in1=gate[:], op=ALU.mult), [rs, gate], [ob])
        sq_ = o_scr[blk]
        k.dma("sp", sq_[scr + h * 128:scr + (h + 1) * 128, :], ob[:], reads=[ob], writes=[sq_])

    def mixer_b(h, blk):
        c0 = h * 1280
        lb = cder[:, 2 + h:3 + h]
        oml = cder[:, 4 + h:5 + h]
        p = proj(c0 + CB_BF * 128)
        k.op("act", lambda e: e.activation(out=b_sg[:], in_=p[:], func=AF.Sigmoid), [p], [b_sg])
        yield 'p'
        k.op("dve", lambda e: e.tensor_scalar(out=b_sg[:], in0=b_sg[:], scalar1=oml, scalar2=lb, op0=ALU.mult, op1=ALU.add),
             [b_sg, cder], [b_sg])
        yield 'p'
        k.op("act", lambda e: e.activation(out=b_lf[:], in_=b_sg[:], func=AF.Ln), [b_sg], [b_lf])
        yield 'p'
        k.op("dve", lambda e: e.tensor_tensor_scan(out=b_cs[:], data0=RST, data1=b_lf[:], initial=0.0, op0=ALU.mult, op1=ALU.add),
             [cmat, b_lf], [b_cs])
        yield 'p'
        k.op("act", lambda e: e.activation(out=b_eb[:], in_=b_cs[:], func=AF.Exp), [b_cs], [b_eb])
        yield 'p'
        k.op("act", lambda e: e.activation(out=b_enb[:], in_=b_cs[:], func=AF.Exp, scale=-1.0), [b_cs], [b_enb])
        yield 'p'
        k.op("dve", lambda e: e.tensor_scalar(out=b_omf[:], in0=b_sg[:], scalar1=-1.0, scalar2=1.0, op0=ALU.mult, op1=ALU.add),
             [b_sg], [b_omf])
        yield 'p'
        k.op("dve", lambda e: e.tensor_tensor(out=b_ke[:], in0=b_omf[:], in1=b_enb[:], op=ALU.mult), [b_omf, b_enb], [b_ke])
        yield 'p'
        cs3 = b_cs.t[:, :].rearrange("p (c j) -> p c j", j=CH)
        k.op("dve", lambda e: e.tensor_tensor(out=b_df[:, :].rearrange("p (c j) -> p c j", j=CH),
                                              in0=cs3[:, :, CH - 1:CH].to_broadcast([128, NCH, CH]), in1=cs3, op=ALU.subtract),
             [b_cs], [b_df])
        yield 'p'
        k.op("act", lambda e: e.activation(out=b_df[:], in_=b_df[:], func=AF.Exp), [b_df], [b_df])
        yield 'p'
        k.op("dve", lambda e: e.tensor_tensor(out=b_kdT[:], in0=b_omf[:], in1=b_df[:], op=ALU.mult), [b_omf, b_df], [b_kdT])
        yield 'p'
        p = proj(c0 + CB_BQ * 128)
        k.op("act", lambda e: e.activation(out=b_qs[:], in_=p[:], func=AF.Silu), [p], [b_qs])
        yield 'p'
        k.op("dve", lambda e: e.tensor_tensor(out=b_qe[:], in0=b_qs[:], in1=b_eb[:], op=ALU.mult), [b_qs, b_eb], [b_qe])
        yield 'p'
        p = proj(c0 + CB_BI * 128)
        k.op("act", lambda e: e.activation(out=b_vT[:], in_=p[:], func=AF.Copy), [p], [b_vT])
        yield 'p'
        p = proj(c0 + CB_BGATE * 128)
        k.op("act", lambda e: e.activation(out=b_gate[:], in_=p[:], func=AF.Sigmoid), [p], [b_gate])
        yield 'p'
        for src, dst in ((b_vT, b_vtm), (b_kdT, b_kd)):
            for c in range(NCH):
                k.op("pe", lambda e, c=c, src=src: e.transpose(out=ptr[0:64, c, :], in_=src[:, c * CH:(c + 1) * CH], identity=ident_bf[:]),
                     [src, ident_bf], [ptr])
                yield 'p'
            k.op("dve", lambda e, dst=dst: e.tensor_copy(dst[:], ptr[0:64, :, :]), [ptr], [dst])
            yield 'p'
        for c in range(NCH):
            k.op("pe", lambda e, c=c: e.matmul(pm[0:64, c * CH:(c + 1) * CH], lhsT=b_ke[:, c * CH:(c + 1) * CH],
                                               rhs=b_qe[:, c * CH:(c + 1) * CH], start=True, stop=True), [b_ke, b_qe], [pm])
            yield 'p'
        k.op("dve", lambda e: e.tensor_tensor(out=b_aT[0:64, :].rearrange("p (c j) -> p c j", j=CH),
                                              in0=pm[0:64, :].rearrange("p (c j) -> p c j", j=CH), in1=bc8(MUI), op=ALU.mult),
             [pm, cmat], [b_aT])
        yield 'p'
        yield 'REC'
        S, Sb = b_S[h], b_Sb[h]
        for c in range(NCH):
            cs = slice(c * CH, (c + 1) * CH)
            k.op("pe", lambda e: e.matmul(po[:, cs], lhsT=Sb[:], rhs=b_qe[:, cs], start=True, stop=False), [Sb, b_qe], [po])
            k.op("pe", lambda e: e.matmul(po[:, cs], lhsT=b_vtm[0:64, c, :], rhs=b_aT[0:64, cs], start=False, stop=True),
                 [b_vtm, b_aT], [po])
            k.op("pe", lambda e: e.matmul(prw[:, 1, :], lhsT=b_kd[0:64, c, :], rhs=b_vtm[0:64, c, :], start=True, stop=True),
                 [b_kd, b_vtm], [prw.p[1]])
            k.op("dve", lambda e: e.scalar_tensor_tensor(out=S[:], in0=S[:], scalar=b_eb[:, c * CH + CH - 1:c * CH + CH], in1=prw[:, 1, :],
                                                         op0=ALU.mult, op1=ALU.add), [S, b_eb, prw.p[1]], [S])
            k.op("act", lambda e: e.activation(out=Sb[:], in_=S[:], func=AF.Copy), [S], [Sb])
            yield 'r'
        k.op("act", lambda e: e.activation(out=b_or[:], in_=po[:], func=AF.Copy), [po], [b_or])
        if "ob_raw" in dbg:
            dump("ob_raw", b_or, b_or[:], dbg_d["ob_raw"][h * 128:(h + 1) * 128, blk * BLK:(blk + 1) * BLK])
        out_norm(h, blk, b_or, b_gate, CV_BON, ob_scr, b_sq, b_rs, b_ob)
        yield 'r'

    a_beta, a_g, a_Gb, a_cv, a_qs, a_ks, a_vs, a_rn, a_kb, a_or, a_rs = [
        T(f"a_{n}") for n in ("beta", "g", "Gb", "cv", "qs", "ks", "vs", "rn", "kb", "or", "rs")]
    a_EL, a_qn, a_kn = a_g, a_qs, a_ks
    a_sq, a_qnb, a_knb, a_kbb, a_rwT, a_kdT, a_ruT, a_ob, a_sq2 = [
        T(f"a_{n}", dt=BF16) for n in ("sq", "qnb", "knb", "kbb", "rwT", "kdT", "ruT", "ob", "sq2")]
    a_E2 = [T(f"a_E{i}") for i in range(2)]
    a_gate2 = [T(f"a_gate{i}") for i in range(2)]
    a_qd2 = [T(f"a_qd{i}", dt=BF16) for i in range(2)]
    a_aqk2 = [T(f"a_aqk{i}", dt=BF16, rows=64) for i in range(2)]
    a_t64, a_d, a_Dm, a_DTs, a_DTi = [T(f"a_{n}", rows=64) for n in ("t64", "d", "Dm", "DTs", "DTi")]
    a_Gtm = k.sb("a_Gtm", [64, NCH], F32)
    a_A = [T(f"a_A{i}", rows=64) for i in range(2)]
    a_B = [T(f"a_B{i}", rows=64) for i in range(2)]
    a_Y = [T(f"a_Y{i}", rows=64) for i in range(2)]
    a_Y5 = T("a_Y5", dt=BF16, rows=64)
    a_ru = k.sb("a_ru", [64, NCH, 128], BF16)
    a_rw = k.sb("a_rw", [64, NCH, 128], BF16)
    a_kd2 = [k.sb(f"a_kd{i}", [64, NCH, 128], BF16) for i in range(2)]
    a_u2 = [k.sb(f"a_u{i}", [64, NCH, 128], F32) for i in range(2)]
    a_wT2 = [T(f"a_wT{i}", dt=BF16) for i in range(2)]
    a_vn = k.sb("a_vn", [64, 128], BF16)
    a_x = [[k.sb(f"a_x{h}{qi}", [128, BLK + 3], F32) for qi in range(3)] for h in range(2)]
    a_S = [k.sb(f"a_S{h}", [128, 128], F32) for h in range(2)]
    a_Sb = [k.sb(f"a_Sb{h}", [128, 128], BF16) for h in range(2)]
    for h in range(2):
        k.op("dve", lambda e: e.memset(a_S[h][:], 0.0), [], [a_S[h]])
        k.op("dve", lambda e: e.memset(a_Sb[h][:], 0.0), [], [a_Sb[h]])
        for qi in range(3):
            k.op("dve", lambda e: e.memset(a_x[h][qi][:], 0.0), [], [a_x[h][qi]])

    def v3(t, rows=128):
        return t.t[0:rows, :].rearrange("p (c j) -> p c j", j=CH)

    print("sbuf bytes remaining (phase 1):", nc.sbuf_bytes_remaining)

    def mixer_a(h, blk):
        a_E, a_gate, a_qd, a_aqk = a_E2[h], a_gate2[h], a_qd2[h], a_aqk2[h]
        a_kd, a_u, a_wT = a_kd2[h], a_u2[h], a_wT2[h]
        c0 = h * 1280
        nA = cder[:, h:h + 1]
        dtb = cvec[:, CV_DTB + h:CV_DTB + h + 1]
        p = proj(c0 + CB_ABETA * 128)
        k.op("act", lambda e: e.activation(out=a_beta[:], in_=p[:], func=AF.Sigmoid), [p], [a_beta])
        yield 'p'
        p = proj(c0 + CB_AALPHA * 128)
        k.op("act", lambda e: e.activation(out=a_g[:], in_=p[:], func=AF.Exp, bias=dtb), [p, cvec], [a_g])
        yield 'p'
        k.op("act", lambda e: e.activation(out=a_g[:], in_=a_g[:], func=AF.Ln, bias=onec), [a_g, cone], [a_g])
        yield 'p'
        k.op("dve", lambda e: e.tensor_scalar(out=a_g[:], in0=a_g[:], scalar1=nA, scalar2=None, op0=ALU.mult), [a_g, cder], [a_g])
        yield 'p'
        k.op("dve", lambda e: e.tensor_tensor_scan(out=a_Gb[:], data0=RST, data1=a_g[:], initial=0.0, op0=ALU.mult, op1=ALU.add),
             [cmat, a_g], [a_Gb])
        yield 'p'
        k.op("act", lambda e: e.activation(out=a_E[:], in_=a_Gb[:], func=AF.Exp), [a_Gb], [a_E])
        yield 'p'
        G3 = v3(a_Gb)
        k.op("dve", lambda e: e.tensor_tensor(out=v3(a_EL), in0=G3[:, :, CH - 1:CH].to_broadcast([128, NCH, CH]), in1=G3, op=ALU.subtract),
             [a_Gb], [a_EL])
        yield 'p'
        k.op("act", lambda e: e.activation(out=a_EL[:], in_=a_EL[:], func=AF.Exp), [a_EL], [a_EL])
        yield 'p'
        for qi, (cb, dst) in enumerate(((CB_AQ, a_qs), (CB_AK, a_ks), (CB_AV, a_vs))):
            xb = a_x[h][qi]
            p = proj(c0 + cb * 128)
            k.op("dve", lambda e: e.tensor_copy(xb[:, 0:3], xb[:, BLK:BLK + 3]), [xb], [xb])
            yield 'p'
            k.op("act", lambda e: e.activation(out=xb[:, 3:BLK + 3], in_=p[:], func=AF.Copy), [p], [xb])
            yield 'p'
            wc = CV_CONV + h * 12 + qi * 4
            k.op("dve", lambda e: e.tensor_scalar(out=a_cv[:], in0=xb[:, 0:BLK], scalar1=cvec[:, wc:wc + 1], scalar2=None, op0=ALU.mult),
                 [xb, cvec], [a_cv])
            yield 'p'
            for j in range(1, 4):
                k.op("dve", lambda e: e.scalar_tensor_tensor(out=a_cv[:], in0=xb[:, j:j + BLK], scalar=cvec[:, wc + j:wc + j + 1],
                                                             in1=a_cv[:], op0=ALU.mult, op1=ALU.add), [xb, cvec, a_cv], [a_cv])
                yield 'p'
            k.op("act", lambda e: e.activation(out=dst[:], in_=a_cv[:], func=AF.Silu), [a_cv], [dst])
            yield 'p'
        for src, dst, scl in ((a_qs, a_qn, 128.0 ** -0.5), (a_ks, a_kn, 1.0)):
            k.op("act", lambda e: e.activation(out=a_sq[:], in_=src[:], func=AF.Square), [src], [a_sq])
            yield 'p'
            k.op("pe", lambda e: e.matmul(pm[:], lhsT=ones_bf[:], rhs=a_sq[:], start=True, stop=True), [ones_bf, a_sq], [pm])
            yield 'p'
            k.op("act", lambda e: e.activation(out=a_rn[:], in_=pm[:], func=AF.Sqrt, bias=epsc), [pm, cder], [a_rn])
            yield 'p'
            k.op("dve", lambda e: e.reciprocal(out=a_rn[:], in_=a_rn[:]), [a_rn], [a_rn])
            yield 'p'
            k.op("dve", lambda e: e.scalar_tensor_tensor(out=dst[:], in0=src[:], scalar=scl, in1=a_rn[:], op0=ALU.mult, op1=ALU.mult),
                 [src, a_rn], [dst])
            yield 'p'
        k.op("act", lambda e: e.activation(out=a_qnb[:], in_=a_qn[:], func=AF.Copy), [a_qn], [a_qnb])
        yield 'p'
        k.op("act", lambda e: e.activation(out=a_knb[:], in_=a_kn[:], func=AF.Copy), [a_kn], [a_knb])
        yield 'p'
        k.op("dve", lambda e: e.tensor_tensor(out=a_qd[:], in0=a_qn[:], in1=a_E[:], op=ALU.mult), [a_qn, a_E], [a_qd])
        yield 'p'
        k.op("dve", lambda e: e.tensor_tensor(out=a_kb[:], in0=a_kn[:], in1=a_beta[:], op=ALU.mult), [a_kn, a_beta], [a_kb])
        yield 'p'
        k.op("act", lambda e: e.activation(out=a_kbb[:], in_=a_kb[:], func=AF.Copy), [a_kb], [a_kbb])
        yield 'p'
        k.op("dve", lambda e: e.tensor_tensor(out=a_rwT[:], in0=a_kb[:], in1=a_E[:], op=ALU.mult), [a_kb, a_E], [a_rwT])
        yield 'p'
        k.op("dve", lambda e: e.tensor_tensor(out=a_kdT[:], in0=a_kn[:], in1=a_EL[:], op=ALU.mult), [a_kn, a_EL], [a_kdT])
        yield 'p'
        k.op("dve", lambda e: e.tensor_tensor(out=a_ruT[:], in0=a_vs[:], in1=a_beta[:], op=ALU.mult), [a_vs, a_beta], [a_ruT])
        yield 'p'
        p = proj(c0 + CB_AGATE * 128)
        k.op("act", lambda e: e.activation(out=a_gate[:], in_=p[:], func=AF.Silu), [p], [a_gate])
        yield 'p'
        k.op("dve", lambda e: e.tensor_tensor(out=v3(a_t64, 64), in0=v3(a_Gb, 64), in1=bc8(I64), op=ALU.mult), [a_Gb, cmat], [a_t64])
        yield 'p'
        k.op("dve", lambda e: e.tensor_reduce(out=a_Gtm[:], in_=v3(a_t64, 64), axis=AX.X, op=ALU.add), [a_t64], [a_Gtm])
        yield 'p'
        k.op("dve", lambda e: e.tensor_tensor(out=v3(a_d, 64), in0=v3(a_Gb, 64), in1=a_Gtm.t[:, :].unsqueeze(2).to_broadcast([64, NCH, CH]),
                                              op=ALU.subtract), [a_Gb, a_Gtm], [a_d])
        yield 'p'
        k.op("dve", lambda e: e.tensor_scalar(out=a_t64[:], in0=a_d[:], scalar1=0.0, scalar2=None, op0=ALU.max), [a_d], [a_t64])
        yield 'p'
        k.op("act", lambda e: e.activation(out=a_t64[:], in_=a_t64[:], func=AF.Exp, scale=-1.0), [a_t64], [a_t64])
        yield 'p'
        k.op("dve", lambda e: e.tensor_tensor(out=v3(a_Dm, 64), in0=v3(a_t64, 64), in1=bc8(ML), op=ALU.mult), [a_t64, cmat], [a_Dm])
        yield 'p'
        k.op("dve", lambda e: e.tensor_scalar(out=a_d[:], in0=a_d[:], scalar1=0.0, scalar2=None, op0=ALU.min), [a_d], [a_d])
        yield 'p'
        k.op("act", lambda e: e.activation(out=a_d[:], in_=a_d[:], func=AF.Exp), [a_d], [a_d])
        yield 'p'
        k.op("dve", lambda e: e.tensor_tensor(out=v3(a_DTs, 64), in0=v3(a_d, 64), in1=bc8(MUS), op=ALU.mult), [a_d, cmat], [a_DTs])
        yield 'p'
        k.op("dve", lambda e: e.tensor_tensor(out=v3(a_DTi, 64), in0=v3(a_d, 64), in1=bc8(MUI), op=ALU.mult), [a_d, cmat], [a_DTi])
        yield 'p'
        for (l, r, msk, dst) in ((a_kbb, a_knb, a_Dm, a_A[0]), (a_knb, a_kbb, a_DTs, a_B[0]), (a_knb, a_qnb, a_DTi, a_aqk)):
            for c in range(NCH):
                cs = slice(c * CH, (c + 1) * CH)
                k.op("pe", lambda e: e.matmul(pm[0:64, cs], lhsT=l[:, cs], rhs=r[:, cs], start=True, stop=True), [l, r], [pm])
                yield 'p'
            k.op("dve", lambda e: e.tensor_tensor(out=(dst[:] if dst is a_aqk else dst[:].bitcast(mybir.dt.float32r)), in0=pm[0:64, :], in1=msk[:], op=ALU.mult),
                 [pm, msk], [dst])
            yield 'p'
        k.op("dve", lambda e: e.tensor_tensor(out=a_Y[0].t[0:64, :].bitcast(mybir.dt.float32r).rearrange("p (c j) -> p c j", j=CH), in0=bc8(I64), in1=v3(a_B[0], 64), op=ALU.subtract), [cmat, a_B[0]], [a_Y[0]])
        yield 'p'
        for s_ in range(1, 6):
            A0, B0, Y0 = a_A[(s_ - 1) % 2], a_B[(s_ - 1) % 2], a_Y[(s_ - 1) % 2]
            A1, B1, Y1 = a_A[s_ % 2], a_B[s_ % 2], a_Y[s_ % 2]
            for c in range(NCH):
                cs = slice(c * CH, (c + 1) * CH)
                k.op("pe", lambda e: e.matmul(pa[0:64, cs], lhsT=B0[:, cs].bitcast(mybir.dt.float32r), rhs=A0[:, cs].bitcast(mybir.dt.float32r), start=True, stop=True), [A0, B0], [pa])
                yield 'p'
            k.op("act", lambda e: e.activation(out=A1[:].bitcast(mybir.dt.float32r), in_=pa[0:64, :], func=AF.Copy), [pa], [A1])
            yield 'p'
            if s_ < 5:
                for c in range(NCH):
                    cs = slice(c * CH, (c + 1) * CH)
                    k.op("pe", lambda e: e.matmul(pb[0:64, cs], lhsT=A0[:, cs].bitcast(mybir.dt.float32r), rhs=B0[:, cs].bitcast(mybir.dt.float32r), start=True, stop=True), [A0, B0], [pb])
                k.op("act", lambda e: e.activation(out=B1[:].bitcast(mybir.dt.float32r), in_=pb[0:64, :], func=AF.Copy), [pb], [B1])
                yield 'p'
            for c in range(NCH):
                cs = slice(c * CH, (c + 1) * CH)
                k.op("pe", lambda e: e.matmul(pm[0:64, cs], lhsT=A1[:, cs].bitcast(mybir.dt.float32r), rhs=Y0[:, cs].bitcast(mybir.dt.float32r), start=True, stop=True), [A1, Y0], [pm])
                yield 'p'
            Yd = a_Y5 if s_ == 5 else Y1
            k.op("dve", lambda e: e.tensor_tensor(out=(Yd[:] if s_ == 5 else Yd[:].bitcast(mybir.dt.float32r)), in0=Y0[:], in1=pm[0:64, :], op=ALU.add), [Y0, pm], [Yd])
            yield 'p'
        for src, dst in ((a_ruT, a_ru), (a_rwT, a_rw), (a_kdT, a_kd)):
            for c in range(NCH):
                k.op("pe", lambda e: e.transpose(out=ptr[0:64, c, :], in_=src[:, c * CH:(c + 1) * CH], identity=ident_bf[:]),
                     [src, ident_bf], [ptr])
                yield 'p'
            k.op("dve", lambda e: e.tensor_copy(dst[:], ptr[0:64, :, :]), [ptr], [dst])
            yield 'p'
        for half, pu in ((0, pa), (1, pb)):
            for c4 in range(4):
                c = half * 4 + c4
                cs = slice(c * CH, (c + 1) * CH)
                k.op("pe", lambda e: e.matmul(pu[0:64, c4 * 128:(c4 + 1) * 128], lhsT=a_Y5[:, cs], rhs=a_ru[:, c, :], start=True, stop=True),
                     [a_Y5, a_ru], [pu])
                yield 'p'
            k.op("act", lambda e: e.activation(out=a_u[:, half * 4:half * 4 + 4, :], in_=pu[0:64, :].rearrange("p (c d) -> p c d", d=128),
                                               func=AF.Copy), [pu], [a_u])
            yield 'p'
        for c in range(NCH):
            cs = slice(c * CH, (c + 1) * CH)
            k.op("pe", lambda e: e.matmul(pm[:, cs], lhsT=a_rw[:, c, :], rhs=a_Y5[:, cs], start=True, stop=True), [a_rw, a_Y5], [pm])
            yield 'p'
        k.op("act", lambda e: e.activation(out=a_wT[:], in_=pm[:], func=AF.Copy), [pm], [a_wT])
        yield 'p'
        yield 'REC'
        S, Sb = a_S[h], a_Sb[h]
        for c in range(NCH):
            cs = slice(c * CH, (c + 1) * CH)
            k.op("pe", lambda e: e.matmul(prw[0:64, 0, :], lhsT=a_wT[:, cs], rhs=Sb[:], start=True, stop=True), [a_wT, Sb], [prw.p[0]])
            k.op("dve", lambda e: e.tensor_tensor(out=a_vn[:], in0=a_u[:, c, :], in1=prw[0:64, 0, :], op=ALU.subtract),
                 [a_u, prw.p[0]], [a_vn])
            k.op("pe", lambda e: e.matmul(po[:, cs], lhsT=Sb[:], rhs=a_qd[:, cs], start=True, stop=False), [Sb, a_qd], [po])
            k.op("pe", lambda e: e.matmul(po[:, cs], lhsT=a_vn[:], rhs=a_aqk[:, cs], start=False, stop=True), [a_vn, a_aqk], [po])
            k.op("pe", lambda e: e.matmul(prw[:, 1, :], lhsT=a_kd[:, c, :], rhs=a_vn[:], start=True, stop=True), [a_kd, a_vn], [prw.p[1]])
            k.op("dve", lambda e: e.scalar_tensor_tensor(out=S[:], in0=S[:], scalar=a_E[:, c * CH + CH - 1:c * CH + CH], in1=prw[:, 1, :],
                                                         op0=ALU.mult, op1=ALU.add), [S, a_E, prw.p[1]], [S])
            k.op("act", lambda e: e.activation(out=Sb[:], in_=S[:], func=AF.Copy), [S], [Sb])
            yield 'r'
        k.op("act", lambda e: e.activation(out=a_or[:], in_=po[:], func=AF.Copy), [po], [a_or])
        if "oa_raw" in dbg:
            dump("oa_raw", a_or, a_or[:], dbg_d["oa_raw"][h * 128:(h + 1) * 128, blk * BLK:(blk + 1) * BLK])
        out_norm(h, blk, a_or, a_gate, CV_AON, oa_scr, a_sq2, a_rs, a_ob)
        yield 'r'

    def coll(blk):
        k.dma("pool", None, None, reads=[o_scr[blk]], writes=[gath[blk]],
              fn=lambda e: e.collective_compute("AllGather", ALU.bypass, replica_groups=[[0, 1, 2, 3], [4, 5, 6, 7]],
                                                ins=[o_scr[blk].t.ap().opt()], outs=[gath[blk].t.ap().opt()]), inc=1)

    def unit(kind, h, blk):
        if kind == "b" and h == 0:
            yield from front(blk * BLK)
        if kind == "b":
            yield from mixer_b(h, blk)
        else:
            yield from mixer_a(h, blk)
        if phase3 and kind == last_kind and h == 1:
            coll(blk)

    kinds = (["b"] if do_b else []) + (["a"] if do_a else [])
    last_kind = kinds[-1]
    units = [unit(kd, h, blk) for blk in range(nblk) for h in range(2) for kd in kinds]
    PRE_PER_STEP = 24

    def run_pre(g):
        for v in g:
            if v == 'REC':
                return True
        return False

    nU = len(units)
    ready = [False] * nU
    is_a = [(kd == "a") for blk in range(nblk) for h in range(2) for kd in kinds]

    def adv(ui_, n):
        c = 0
        while c < n and not ready[ui_]:
            v2 = next(units[ui_], 'END')
            c += 1
            if v2 == 'REC' or v2 == 'END':
                ready[ui_] = True
        return c

    adv(0, 10 ** 9)
    for ui in range(nU):
        for v in units[ui]:
            budget = PRE_PER_STEP
            if ui + 1 < nU:
                budget -= adv(ui + 1, budget)
            if budget > 0 and is_a[ui] and ui + 2 < nU and ready[ui + 1] and is_a[ui + 2]:
                adv(ui + 2, budget)
        if ui + 1 < nU:
            adv(ui + 1, 10 ** 9)

    if phase3:
        NT3 = SEQ // 4
        wG_d = nc.dram_tensor("wG", [D, 2048], F32, kind="ExternalInput")
        wBa_d = nc.dram_tensor("wBa", [D, D], F32, kind="ExternalInput")
        wBb_d = nc.dram_tensor("wBb", [D, D], F32, kind="ExternalInput")
        wO_d = nc.dram_tensor("wO", [D, D], F32, kind="ExternalInput")
        wPQ_d = nc.dram_tensor("wPQ", [D, 2048], F32, kind="ExternalInput")
        skT_d = nc.dram_tensor("skT", [128, 2048], F32, kind="ExternalInput")
        n2b_d = nc.dram_tensor("n2b", [128, D], F32, kind="ExternalInput")
        fnb_d = nc.dram_tensor("fnb", [128, D], F32, kind="ExternalInput")

        x3_d = nc.dram_tensor("x3", [NT3, D], F32, kind="ExternalInput")
        sel_d = nc.dram_tensor("sel", [128, 4], F32, kind="ExternalInput")
        out_d = k.dram("out", [NT3, D], F32, kind="ExternalOutput")
        x1_scr = k.dram("x1_scr", [NT3, D], F32)
        k.barrier()
        es1.close()

        if stop3 == "gather":
            k.finish()
            return nc, es
        es3 = ExitStack()
        k.es = es3
        wst2 = k.sb("wst2", [128, 2, 8, 256], F32, nparts=2)
        ld_i = [0]

        def load_w(dst, src_d, ncols):
            v = src_d.ap().rearrange("(kc p) c -> p kc c", p=128)
            for i in range(ncols // 256):
                s_ = ld_i[0] % 2
                ld_i[0] += 1
                k.dma("sp", wst2[:, s_, :, :], v[:, :, i * 256:(i + 1) * 256], writes=[wst2.p[s_]])
                if s_:
                    k.op("act", lambda e: e.activation(out=dst[:, :, i * 256:(i + 1) * 256], in_=wst2[:, s_, :, :], func=AF.Copy),
                         [wst2.p[s_]], [dst])
                else:
                    k.op("dve", lambda e: e.tensor_copy(dst[:, :, i * 256:(i + 1) * 256], wst2[:, s_, :, :]), [wst2.p[s_]], [dst])

        Wg = k.sb("Wg", [128, 8, 2048], BF16)
        Wa = k.sb("Wa", [128, 8, D], BF16)
        Wb = k.sb("Wb", [128, 8, D], BF16)
        Wo = k.sb("Wo", [128, 8, D], BF16)
        load_w(Wg, wG_d, 2048)
        load_w(Wa, wBa_d, D)
        load_w(Wb, wBb_d, D)
        load_w(Wo, wO_d, D)
        sga = k.sb("sga", [128, 8, BLK], BF16)
        sgb = k.sb("sgb", [128, 8, BLK], BF16)
        oaT = k.sb("oaT", [128, 8, BLK], BF16)
        obT = k.sb("obT", [128, 8, BLK], BF16)
        mixT = k.sb("mixT", [128, 8, BLK], BF16)
        m_t1 = k.sb("m_t1", [128, BLK], F32)
        m_t2 = k.sb("m_t2", [128, BLK], F32)
        xr = k.sb("xr", [128, D], F32)
        x1t = k.sb("x1t", [128, D], F32)
        sel = k.sb("sel", [128, 4], F32)
        k.dma("sp", sel[:], sel_d.ap()[:, :], writes=[sel])
        gv = [g_.t.ap().rearrange("(r ab hl p) t -> ab p r hl t", r=4, ab=2, hl=2, p=128) for g_ in gath]
        x3_v = x3_d.ap()
        cand = k.sb("cand", [128, 2, 8, BLK], BF16, nparts=8)
        for blk in range(NT3 // BLK):
            for _ in front(blk * BLK, x3_v):
                pass
            ci = 0
            for ab, dst in ((0, oaT), (1, obT)):
                for q in range(4):
                    s_ = ci % 2
                    ci += 1
                    gi = q * 4 + blk
                    for r in range(4):
                        k.dma("sp", cand[:, s_, 2 * r:2 * r + 2, :], gv[gi][ab][:, r, :, :],
                              reads=[gath[gi]], writes=[cand.p[s_ * 4 + r]])
                    if q == 0:
                        k.op("dve", lambda e: e.tensor_scalar(out=dst[:], in0=cand[:, s_, :, :], scalar1=sel[:, 0:1], scalar2=None, op0=ALU.mult),
                             [cand.p[s_ * 4:s_ * 4 + 4], sel], [dst])
                    else:
                        k.op("dve", lambda e: e.scalar_tensor_tensor(out=dst[:], in0=cand[:, s_, :, :], scalar=sel[:, q:q + 1], in1=dst[:],
                                                                     op0=ALU.mult, op1=ALU.add), [cand.p[s_ * 4:s_ * 4 + 4], sel, dst], [dst])
            for cb in range(8):
                p = proj(cb * 128, Wg)
                k.op("act", lambda e: e.activation(out=sga[:, cb, :], in_=p[:], func=AF.Sigmoid), [p], [sga])
                p = proj(D + cb * 128, Wg)
                k.op("act", lambda e: e.activation(out=sgb[:, cb, :], in_=p[:], func=AF.Sigmoid), [p], [sgb])
            for cb in range(8):
                for kc in range(8):
                    k.op("pe", lambda e: e.matmul(pm[:], lhsT=Wa[:, kc, cb * 128:(cb + 1) * 128], rhs=oaT[:, kc, :],
                                                  start=(kc == 0), stop=(kc == 7)), [Wa, oaT], [pm])
                k.op("dve", lambda e: e.tensor_tensor(out=m_t1[:], in0=pm[:], in1=sga[:, cb, :], op=ALU.mult), [pm, sga], [m_t1])
                for kc in range(8):
                    k.op("pe", lambda e: e.matmul(pm[:], lhsT=Wb[:, kc, cb * 128:(cb + 1) * 128], rhs=obT[:, kc, :],
                                                  start=(kc == 0), stop=(kc == 7)), [Wb, obT], [pm])
                k.op("dve", lambda e: e.tensor_tensor(out=m_t2[:], in0=pm[:], in1=sgb[:, cb, :], op=ALU.mult), [pm, sgb], [m_t2])
                k.op("dve", lambda e: e.tensor_tensor(out=mixT[:, cb, :], in0=m_t1[:], in1=m_t2[:], op=ALU.add), [m_t1, m_t2], [mixT])
            for tt in range(4):
                r0 = blk * BLK + tt * 128
                k.dma("sp", xr[:], x3_v[r0:r0 + 128, :], writes=[xr])
                for half in range(2):
                    p = pp[0]
                    for kc in range(8):
                        k.op("pe", lambda e: e.matmul(p[:], lhsT=mixT[:, kc, tt * 128:(tt + 1) * 128], rhs=Wo[:, kc, half * 512:(half + 1) * 512],
                                                      start=(kc == 0), stop=(kc == 7)), [mixT, Wo], [p])
                    k.op("dve", lambda e: e.tensor_tensor(out=x1t[:, half * 512:(half + 1) * 512], in0=p[:], in1=xr[:, half * 512:(half + 1) * 512],
                                                          op=ALU.add), [p, xr], [x1t])
                k.dma("sp", x1_scr[r0:r0 + 128, :], x1t[:], reads=[x1t], writes=[x1_scr])
                if "x1" in dbg:
                    dump("x1", x1t, x1t[:], dbg_d["x1"][r0:r0 + 128, :])
        k.barrier()
        es3.close()
        if stop3 == "3a":
            k.finish()
            return nc, es

        es4 = ExitStack()
        k.es = es4
        Wpq = k.sb("Wpq", [128, 8, 2048], BF16)
        wpq_v = wPQ_d.ap().rearrange("(kc p) c -> p kc c", p=128)
        for i in range(4):
            k.dma("pool", Wpq[:, :, i * 512:(i + 1) * 512], wpq_v[:, :, i * 512:(i + 1) * 512], writes=[Wpq], nowaw=True)
        skT = k.sb("skTb", [128, 16, 128], BF16)
        k.dma("pool", skT[:, :, :].rearrange("p g k -> p (g k)"), skT_d.ap()[:, :], writes=[skT])
        n2b = k.sb("n2bs", [128, D], F32)
        fnb = k.sb("fnbs", [128, D], F32)
        k.dma("sp", n2b[:], n2b_d.ap()[:, :], writes=[n2b])
        k.dma("sp", fnb[:], fnb_d.ap()[:, :], writes=[fnb])
        x1p2 = [k.sb(f"x1p{i}", [128, D], F32) for i in range(2)]
        h2n2 = [k.sb(f"h2n{i}", [128, D], F32) for i in range(2)]
        h2b2 = [k.sb(f"h2b{i}", [128, D], BF16) for i in range(2)]
        prodb = k.sb("prodb", [128, 2, D], BF16, nparts=2)
        h2T = k.sb("h2T", [128, 8, 128], BF16)
        qTb = k.sb("qTb", [128, 16, 128], BF16)
        s_sb = k.sb("s_sb", [128, 16, 128], F32)
        tv = k.sb("tv", [128, 16, 16], F32)
        ti = k.sb("ti", [128, 16, 16], U32)
        tif = k.sb("tif", [128, 16, 16], F32)
        cs_ = k.sb("cand_s", [128, 8, 256], F32)
        bs_ = k.sb("best_s", [128, 8, 16], F32)
        pos = k.sb("pos", [128, 8, 16], U32)
        posf = k.sb("posf", [128, 8, 16], F32)
        pa_ = k.sb("pa_", [128, 8, 16], F32)
        pb_ = k.sb("pb_", [128, 8, 16], F32)
        eq = k.sb("eq", [128, 8, 16, 16], F32)
        i1 = k.sb("i1", [128, 8, 16], F32)
        i2 = k.sb("i2", [128, 8, 16], F32)
        idxf = k.sb("idxf", [128, 128], F32)
        idxu2 = [k.sb(f"idxu{i}", [128, 128], U32) for i in range(2)]
        gsm = k.sb("gsm", [128, 16], F32)
        gate2 = [k.sb(f"gate{i}", [128, 8, 16], F32) for i in range(2)]
        gatef2 = [g_.t[:, :, :].rearrange("p h k -> p (h k)") for g_ in gate2]
        BG_PER_STEP = 8
        NG = 14
        GS = 4
        NGR = 128 // GS
        hpre = k.sb("hpre", [128, 128], F32, nparts=32)
        gcol = k.sb("gcol", [128, 128], F32, nparts=8)
        gw = k.sb("gw", [128, 128], F32, nparts=8)
        UVg = k.sb("UVg", [128, NG, 2 * D], BF16, nparts=NG)
        Dm = k.sb("Dm", [128, NG, 128], BF16, nparts=NG)
        scrU = k.sb("scrU", [128, D], BF16)
        x2t = k.sb("x2t", [128, D], F32)
        obuf = k.sb("obuf", [128, D], F32)
        print("sbuf bytes remaining (3b):", nc.sbuf_bytes_remaining)
        ps_s = k.ps("ps_s", [128, 16, 128], F32, nparts=4)
        py1 = k.ps("py1", [128, 512], F32)
        pq = pm
        py0 = pp[0]
        IOTA = cmat.t[:, CM_IOTA:CM_IOTA + 16]
        THR = cmat.t[:, CM_THR:CM_THR + 16]
        tv4 = tv.t[:, :, :].rearrange("p (h two) k -> p h two k", two=2)
        tif4 = tif.t[:, :, :].rearrange("p (h two) k -> p h two k", two=2)
        euv_v = euvb.t.ap()
        def tile_front(tile_i):
            pb_i = tile_i % 2
            x1p, h2n, idxu, gate, gatef = x1p2[pb_i], h2n2[pb_i], idxu2[pb_i], gate2[pb_i], gatef2[pb_i]
            h2b = h2b2[pb_i]
            r0 = tile_i * 128
            k.dma("sp", x1p[:], x1_scr[r0:r0 + 128, :], reads=[x1_scr], writes=[x1p])
            yield
            k.op("act", lambda e: e.activation(out=junk[:], in_=x1p[:], func=AF.Square, accum_out=st[:, 0:1]), [x1p], [junk, st.p[0]])
            yield
            k.op("act", lambda e: e.activation(out=st[:, 1:2], in_=st[:, 0:1], func=AF.Sqrt, bias=epsc, scale=1.0 / D), [st.p[0], cder], [st.p[0]])
            yield
            k.op("dve", lambda e: e.reciprocal(out=st[:, 1:2], in_=st[:, 1:2]), [st.p[0]], [st.p[0]])
            yield
            k.op("dve", lambda e: e.scalar_tensor_tensor(out=h2n[:], in0=x1p[:], scalar=st[:, 1:2], in1=n2b[:], op0=ALU.mult, op1=ALU.mult),
                 [x1p, st.p[0], n2b], [h2n])
            yield
            k.op("act", lambda e: e.activation(out=h2b[:], in_=h2n[:], func=AF.Copy), [h2n], [h2b])
            yield
            for kc in range(8):
                k.op("pe", lambda e: e.transpose(out=pT[:, kc, :], in_=h2b[:, kc * 128:(kc + 1) * 128], identity=ident_bf[:]), [h2b, ident_bf], [pT])
                yield
            k.op("act", lambda e: e.activation(out=h2T[:], in_=pT[:], func=AF.Copy), [pT], [h2T])
            yield
            for g4 in range(4):
                for gg in range(4):
                    g = g4 * 4 + gg
                    for kc in range(8):
                        k.op("pe", lambda e: e.matmul(pq[:, gg * 128:(gg + 1) * 128], lhsT=Wpq[:, kc, g * 128:(g + 1) * 128], rhs=h2T[:, kc, :],
                                                      start=(kc == 0), stop=(kc == 7)), [Wpq, h2T], [pq])
                k.op("act", lambda e: e.activation(out=qTb[:, g4 * 4:(g4 + 1) * 4, :], in_=pq[:, :].rearrange("p (g t) -> p g t", t=128),
                                                   func=AF.Copy), [pq], [qTb])
                yield
            for g in range(16):
                k.op("pe", lambda e: e.matmul(ps_s[:, g, :], lhsT=qTb[:, g, :], rhs=skT[:, g, :], start=True, stop=True), [qTb, skT], [ps_s.p[g // 4]])
                yield
            for q4 in range(4):
                k.op("act", lambda e: e.activation(out=s_sb[:, q4 * 4:(q4 + 1) * 4, :], in_=ps_s[:, q4 * 4:(q4 + 1) * 4, :], func=AF.Copy),
                     [ps_s.p[q4]], [s_sb])
                yield
            for g in range(16):
                k.op("dve", lambda e: e.max(out=tv[:, g, 0:8], in_=s_sb[:, g, :]), [s_sb], [tv])
                yield
                k.op("dve", lambda e: e.max_index(out=ti[:, g, 0:8], in_max=tv[:, g, 0:8], in_values=s_sb[:, g, :]), [tv, s_sb], [ti])
                yield
                k.op("dve", lambda e: e.match_replace(out=s_sb[:, g, :], in_to_replace=tv[:, g, 0:8], in_values=s_sb[:, g, :], imm_value=-1e30),
                     [tv, s_sb], [s_sb])
                yield
                k.op("dve", lambda e: e.max(out=tv[:, g, 8:16], in_=s_sb[:, g, :]), [s_sb], [tv])
                yield
                k.op("dve", lambda e: e.max_index(out=ti[:, g, 8:16], in_max=tv[:, g, 8:16], in_values=s_sb[:, g, :]), [tv, s_sb], [ti])
                yield
            k.op("dve", lambda e: e.tensor_copy(tif[:], ti[:]), [ti], [tif])
            yield
            cs4 = cs_.t[:, :, :].rearrange("p h (a b) -> p h a b", b=16)
            k.op("dve", lambda e: e.tensor_tensor(out=cs4, in0=tv4[:, :, 0, :].unsqueeze(3).to_broadcast([128, 8, 16, 16]),
                                                  in1=tv4[:, :, 1, :].unsqueeze(2).to_broadcast([128, 8, 16, 16]), op=ALU.add), [tv], [cs_])
            yield
            for h in range(8):
                k.op("dve", lambda e: e.max(out=bs_[:, h, 0:8], in_=cs_[:, h, :]), [cs_], [bs_])
                yield
                k.op("dve", lambda e: e.max_index(out=pos[:, h, 0:8], in_max=bs_[:, h, 0:8], in_values=cs_[:, h, :]), [bs_, cs_], [pos])
                yield
                k.op("dve", lambda e: e.match_replace(out=cs_[:, h, :], in_to_replace=bs_[:, h, 0:8], in_values=cs_[:, h, :], imm_value=-1e30),
                     [bs_, cs_], [cs_])
                yield
                k.op("dve", lambda e: e.max(out=bs_[:, h, 8:16], in_=cs_[:, h, :]), [cs_], [bs_])
                yield
                k.op("dve", lambda e: e.max_index(out=pos[:, h, 8:16], in_max=bs_[:, h, 8:16], in_values=cs_[:, h, :]), [bs_, cs_], [pos])
                yield
            k.op("dve", lambda e: e.tensor_copy(posf[:], pos[:]), [pos], [posf])
            yield
            k.op("dve", lambda e: e.tensor_tensor(out=eq[:], in0=posf.t[:, :, :].unsqueeze(3).to_broadcast([128, 8, 16, 16]),
                                                  in1=THR.unsqueeze(1).unsqueeze(1).to_broadcast([128, 8, 16, 16]), op=ALU.is_ge), [posf, cmat], [eq])
            yield
            k.op("dve", lambda e: e.tensor_reduce(out=pa_[:], in_=eq[:], axis=AX.X, op=ALU.add), [eq], [pa_])
            yield
            k.op("dve", lambda e: e.scalar_tensor_tensor(out=pb_[:], in0=pa_[:], scalar=-16.0, in1=posf[:], op0=ALU.mult, op1=ALU.add),
                 [pa_, posf], [pb_])
            yield
            for (pp_, half, dst) in ((pa_, 0, i1), (pb_, 1, i2)):
                k.op("dve", lambda e: e.tensor_tensor(out=eq[:], in0=pp_.t[:, :, :].unsqueeze(3).to_broadcast([128, 8, 16, 16]),
                                                      in1=IOTA.unsqueeze(1).unsqueeze(1).to_broadcast([128, 8, 16, 16]), op=ALU.is_equal),
                     [pp_, cmat], [eq])
                yield
                k.op("dve", lambda e: e.tensor_tensor(out=eq[:], in0=eq[:], in1=tif4[:, :, half, :].unsqueeze(2).to_broadcast([128, 8, 16, 16]),
                                                      op=ALU.mult), [eq, tif], [eq])
                yield
                k.op("dve", lambda e: e.tensor_reduce(out=dst[:], in_=eq[:], axis=AX.X, op=ALU.add), [eq], [dst])
                yield
            k.op("dve", lambda e: e.scalar_tensor_tensor(out=idxf[:, :].rearrange("p (h k) -> p h k", k=16), in0=i1[:], scalar=128.0, in1=i2[:],
                                                         op0=ALU.mult, op1=ALU.add), [i1, i2], [idxf])
            yield
            k.op("dve", lambda e: e.tensor_copy(idxu[:], idxf[:]), [idxf], [idxu])
            yield
            k.op("dve", lambda e: e.tensor_tensor(out=gate[:], in0=bs_[:], in1=bs_[:, :, 0:1].to_broadcast([128, 8, 16]), op=ALU.subtract), [bs_], [gate])
            yield
            k.op("act", lambda e: e.activation(out=gate[:], in_=gate[:], func=AF.Exp), [gate], [gate])
            yield
            k.op("dve", lambda e: e.tensor_reduce(out=gsm[:, 0:8], in_=gate[:], axis=AX.X, op=ALU.add), [gate], [gsm])
            yield
            k.op("dve", lambda e: e.reciprocal(out=gsm[:, 8:16], in_=gsm[:, 0:8]), [gsm], [gsm])
            yield
            k.op("dve", lambda e: e.tensor_tensor(out=gate[:], in0=gate[:], in1=gsm[:, 8:16].unsqueeze(2).to_broadcast([128, 8, 16]), op=ALU.mult),
                 [gate, gsm], [gate])
            yield

            yield

        def tile_slots(tile_i):
            pb_i = tile_i % 2
            x1p, h2n, idxu, gate, gatef = x1p2[pb_i], h2n2[pb_i], idxu2[pb_i], gate2[pb_i], gatef2[pb_i]
            h2b = h2b2[pb_i]
            r0 = tile_i * 128
            def vside(gr):
                lf = gr % 8
                c0_, c1_ = gr * GS, (gr + 1) * GS
                for sl in range(c0_, c1_):
                    j = sl % NG
                    k.op("act", lambda e: e.activation(out=gw[:, sl:sl + 1], in_=gcol[:, sl:sl + 1], func=AF.Copy, scale=gatef[:, sl:sl + 1]),
                         [gcol.p[lf], gate], [gw.p[lf]])
                    k.op("act", lambda e: e.activation(out=Dm[:, j, :], in_=ident_bf[:], func=AF.Copy, scale=gw[:, sl:sl + 1]),
                         [ident_bf, gw.p[lf]], [Dm.p[j]])
                    k.op("pe", lambda e: e.matmul(py0[:], lhsT=Dm[:, j, :], rhs=UVg[:, j, D:D + 512], start=(sl == 0), stop=(sl == 127)),
                         [Dm.p[j], UVg.p[j]], [py0])
                    k.op("pe", lambda e: e.matmul(py1[:], lhsT=Dm[:, j, :], rhs=UVg[:, j, D + 512:2 * D], start=(sl == 0), stop=(sl == 127)),
                         [Dm.p[j], UVg.p[j]], [py1])

            for gr in range(NGR):
                lf = gr % 8
                for sl in range(gr * GS, (gr + 1) * GS):
                    j = sl % NG
                    k.dma("pool", None, None, reads=[idxu, euvb], writes=[UVg.p[j]],
                          fn=lambda e: e.indirect_dma_start(out=UVg[:, j, :], out_offset=None, in_=euv_v[:, :],
                                                            in_offset=bass.IndirectOffsetOnAxis(ap=idxu[:, sl:sl + 1], axis=0)))
                    if sl % 3 == 2:
                        pj = (sl // 3) % 2
                        k.op("dve", lambda e: e.tensor_tensor(out=prodb[:, pj, :], in0=UVg[:, j, 0:D], in1=h2b[:], op=ALU.mult),
                             [UVg.p[j], h2b], [prodb.p[pj]])
                        k.op("act", lambda e: e.activation(out=junk[:], in_=prodb[:, pj, :], func=AF.Copy, accum_out=hpre[:, sl:sl + 1]),
                             [prodb.p[pj]], [junk, hpre.p[sl % 32]])
                    else:
                        k.op("dve", lambda e: e.scalar_tensor_tensor(out=scrU[:], in0=UVg[:, j, 0:D], scalar=1.0, in1=h2n[:], op0=ALU.mult,
                                                                     op1=ALU.mult, accum_out=hpre[:, sl:sl + 1]),
                             [UVg.p[j], h2n], [scrU, hpre.p[sl % 32]])
                k.op("act", lambda e: e.activation(out=gcol[:, gr * GS:(gr + 1) * GS], in_=hpre[:, gr * GS:(gr + 1) * GS], func=AF.Gelu),
                     [hpre.p[(gr * GS) % 32:(gr * GS) % 32 + GS]], [gcol.p[lf]])
                vside(gr)
                yield
            k.op("dve", lambda e: e.tensor_tensor(out=x2t[:, 0:512], in0=py0[:], in1=x1p[:, 0:512], op=ALU.add), [py0, x1p], [x2t])
            k.op("dve", lambda e: e.tensor_tensor(out=x2t[:, 512:D], in0=py1[:], in1=x1p[:, 512:D], op=ALU.add), [py1, x1p], [x2t])
            if "x2" in dbg:
                dump("x2", x2t, x2t[:], dbg_d["x2"][r0:r0 + 128, :])
            k.op("act", lambda e: e.activation(out=junk[:], in_=x2t[:], func=AF.Square, accum_out=st[:, 2:3]), [x2t], [junk, st.p[1]])
            k.op("act", lambda e: e.activation(out=st[:, 3:4], in_=st[:, 2:3], func=AF.Sqrt, bias=epsc, scale=1.0 / D), [st.p[1], cder], [st.p[1]])
            k.op("dve", lambda e: e.reciprocal(out=st[:, 3:4], in_=st[:, 3:4]), [st.p[1]], [st.p[1]])
            k.op("dve", lambda e: e.scalar_tensor_tensor(out=obuf[:], in0=x2t[:], scalar=st[:, 3:4], in1=fnb[:], op0=ALU.mult, op1=ALU.mult),
                 [x2t, st.p[1], fnb], [obuf])
            k.dma("sp", out_d[r0:r0 + 128, :], obuf[:], reads=[obuf], writes=[out_d])

            yield

        fr = tile_front(0)
        for _ in fr:
            pass
        for tile_i in range(ntile3):
            bg = tile_front(tile_i + 1) if tile_i + 1 < ntile3 else None
            for _ in tile_slots(tile_i):
                if bg is not None:
                    for _n in range(BG_PER_STEP):
                        if next(bg, 'end') == 'end':
                            bg = None
                            break
            if bg is not None:
                for _ in bg:
                    pass

    k.finish()
    print("instructions:", k.n_ins, "dma sems:", len(k.dsems))
    return nc, es


def host_prep(inputs):
    x = np.asarray(inputs["x"], np.float32)
    w_in = np.asarray(inputs["w_in"], np.float32)[0]
    conv = np.asarray(inputs["conv_a"], np.float32)[0]
    cmat = np.zeros((128, NCM), np.float32)
    cmat[:, CM_ID:CM_ID + 128] = np.eye(128, dtype=np.float32)
    i = np.arange(64)
    cmat[0:64, CM_ML:CM_ML + 64] = (i[:, None] > i[None, :])
    cmat[0:64, CM_MUS:CM_MUS + 64] = (i[:, None] < i[None, :])
    cmat[0:64, CM_MUI:CM_MUI + 64] = (i[:, None] <= i[None, :])
    cmat[0:64, CM_I64:CM_I64 + 64] = np.eye(64, dtype=np.float32)
    cmat[:, CM_RST:CM_RST + 512] = (np.arange(512) % 64 != 0)[None, :]
    cmat[:, CM_ONE:CM_ONE + 128] = 1.0
    cmat[:, CM_IOTA:CM_IOTA + 16] = np.arange(16, dtype=np.float32)[None, :]
    thr = 16.0 * (np.arange(16, dtype=np.float32) + 1.0)
    thr[15] = 1e9
    cmat[:, CM_THR:CM_THR + 16] = thr[None, :]
    offs = {CB_AQ: 0, CB_AK: 1024, CB_AV: 2048, CB_AGATE: 3088, CB_BF: 4112, CB_BI: 5136, CB_BQ: 6160, CB_BGATE: 7184}
    w_pq = np.asarray(inputs["w_pq"], np.float32)[0]
    sk = np.asarray(inputs["sub_keys"], np.float32)[0]
    skT = np.ascontiguousarray(sk.transpose(3, 1, 0, 2).reshape(128, 16 * 128))
    shared = {
        "wG": np.ascontiguousarray(w_in[:, 8208:8208 + 2048]),
        "wBa": np.ascontiguousarray(inputs["w_branch_a"][0]), "wBb": np.ascontiguousarray(inputs["w_branch_b"][0]),
        "wO": np.ascontiguousarray(inputs["w_out"][0]), "wPQ": w_pq, "skT": skT,
        "n2b": np.ascontiguousarray(np.broadcast_to(inputs["norm2"][0][None, :], (128, D))),
        "fnb": np.ascontiguousarray(np.broadcast_to(inputs["final_norm"][None, :], (128, D))),
        "euv": np.concatenate([inputs["expert_u"][0], inputs["expert_v"][0]], axis=1),
    }
    maps = []
    for c in range(NCORE):
        b, hp = c // 4, c % 4
        wA = np.empty((D, NWA), np.float32)
        cvec = np.zeros((128, NCV), np.float32)
        cvec[:, CV_N1:CV_N1 + 8] = inputs["norm1"][0].reshape(8, 128).T
        cvec[:, CV_N2:CV_N2 + 8] = inputs["norm2"][0].reshape(8, 128).T
        cvec[:, CV_AON] = inputs["a_onorm"][0]
        cvec[:, CV_BON] = inputs["b_onorm"][0]
        for hl in range(2):
            h = 2 * hp + hl
            for cb, o in offs.items():
                wA[:, hl * 1280 + cb * 128: hl * 1280 + (cb + 1) * 128] = w_in[:, o + 128 * h: o + 128 * (h + 1)]
            wA[:, hl * 1280 + CB_ABETA * 128: hl * 1280 + (CB_ABETA + 1) * 128] = w_in[:, 3072 + h: 3073 + h]
            wA[:, hl * 1280 + CB_AALPHA * 128: hl * 1280 + (CB_AALPHA + 1) * 128] = w_in[:, 3080 + h: 3081 + h]
            for qi in range(3):
                cvec[:, CV_CONV + hl * 12 + qi * 4: CV_CONV + hl * 12 + qi * 4 + 4] = conv[:, qi * 1024 + 128 * h: qi * 1024 + 128 * (h + 1)].T
            cvec[:, CV_ALOG + hl] = inputs["a_log"][0, h]
            cvec[:, CV_DTB + hl] = inputs["dt_bias"][0, h]
            cvec[:, CV_BLB + 2 * hl] = inputs["b_lower_bound"][0, 128 * h:128 * (h + 1)]
            cvec[:, CV_BLB + 2 * hl + 1] = inputs["b_lower_bound"][1, 128 * h:128 * (h + 1)]
        ts = c % 4
        sel = np.zeros((128, 4), np.float32)
        sel[:, ts] = 1.0
        m = {"x": np.ascontiguousarray(x[b]), "wA": wA, "cvec": cvec, "cmat": cmat,
             "x3": np.ascontiguousarray(x[b, ts * 2048:(ts + 1) * 2048]), "sel": sel}
        m.update(shared)
        maps.append(m)
    return maps


_CACHE = {}


def kernel(**inputs):
    inputs = {k_: np.asarray(v) for k_, v in inputs.items()}
    maps = host_prep(inputs)
    if "nc" not in _CACHE:
        _CACHE["nc"] = build()
    nc, _ = _CACHE["nc"]
    res = run_bass_kernel_spmd(nc, maps, core_ids=list(range(NCORE)))
    out = np.empty((2, SEQ, D), np.float32)
    for c in range(NCORE):
        b, ts = c // 4, c % 4
        out[b, ts * 2048:(ts + 1) * 2048] = res.results[c]["out"]
    return out
```

```python
import numpy as np
from contextlib import ExitStack
import concourse.bass as bass
import concourse.mybir as mybir
from concourse.bass_utils import run_bass_kernel_spmd

F32 = mybir.dt.float32
BF16 = mybir.dt.bfloat16
I32 = mybir.dt.int32
U32 = mybir.dt.uint32
ALU = mybir.AluOpType
AF = mybir.ActivationFunctionType
AX = mybir.AxisListType

D = 1024
SEQ = 8192
NCORE = 8
BLK = 512
CH = 64
NCH = BLK // CH
EPS = 1e-6
NWA = 2560
CB_AQ, CB_AK, CB_AV, CB_ABETA, CB_AALPHA, CB_AGATE, CB_BF, CB_BI, CB_BQ, CB_BGATE = range(10)
CV_N1, CV_N2, CV_CONV, CV_ALOG, CV_DTB, CV_AON, CV_BON, CV_BLB = 0, 8, 16, 40, 42, 44, 45, 46
NCV = 50
CM_ID, CM_ML, CM_MUS, CM_MUI, CM_I64, CM_RST, CM_ONE, CM_IOTA, CM_THR = 0, 128, 192, 256, 320, 384, 896, 1024, 1040
NCM = 1056


class Reg:
    __slots__ = ("name", "w", "r", "dsem", "dcnt")

    def __init__(self, name):
        self.name = name
        self.w = None
        self.r = []
        self.dsem = None
        self.dcnt = 0


class Tile:
    def __init__(self, t, name, nparts=1):
        self.t = t
        self.name = name
        self.p = [Reg(f"{name}.{i}") for i in range(nparts)]

    def __getitem__(self, idx):
        return self.t[idx]


def _leaves(xs):
    out = []
    for x in xs:
        if x is None:
            continue
        if isinstance(x, Tile):
            out.extend(x.p)
        elif isinstance(x, (list, tuple)):
            out.extend(_leaves(x))
        else:
            out.append(x)
    return out


class K:
    def __init__(self, nc, es):
        self.nc = nc
        self.es = es
        self.es_sem = es
        self.eng = {"pe": nc.tensor, "dve": nc.vector, "act": nc.scalar, "pool": nc.gpsimd, "sp": nc.sync}
        self.sem = {}
        self.cnt = {}
        for e in self.eng:
            self.sem[e] = es.enter_context(nc.semaphore(f"s_{e}"))
            self.cnt[e] = 0
        self.seen = {e: {} for e in self.eng}
        self.dsems = []
        self.n_ins = 0

    def sb(self, name, shape, dt, nparts=1):
        t = self.es.enter_context(self.nc.sbuf_tensor("sb_" + name, list(shape), dt))
        return Tile(t, name, nparts)

    def ps(self, name, shape, dt=F32, nparts=1):
        t = self.es.enter_context(self.nc.psum_tensor("ps_" + name, list(shape), dt))
        return Tile(t, name, nparts)

    def dram(self, name, shape, dt, kind="Internal", nparts=1):
        t = self.nc.dram_tensor(name, list(shape), dt, kind=kind)
        return Tile(t, name, nparts)

    def _wait(self, e, toks):
        E = self.eng[e]
        seen = self.seen[e]
        best = {}
        for (sem, val, owner) in toks:
            k = id(sem)
            if best.get(k, (None, 0))[1] < val:
                best[k] = (sem, val)
        for k, (sem, val) in best.items():
            if seen.get(k, 0) < val:
                E.wait_ge(sem, val)
                seen[k] = val

    def _deps(self, e, reads, writes):
        toks = []
        for r in reads:
            if r.w is not None:
                if r.w[2] == e and e == "pe":
                    continue
                toks.append(r.w)
        for w in writes:
            if w.w is not None and (w.w[2] != e or e != "pe"):
                toks.append(w.w)
            for t in w.r:
                if t[2] != e or e != "pe":
                    toks.append(t)
        return toks

    def op(self, e, fn, reads=(), writes=()):
        reads = _leaves(reads)
        writes = _leaves(writes)
        self._wait(e, self._deps(e, reads, writes))
        ins = fn(self.eng[e])
        self.cnt[e] += 1
        ins.then_inc(self.sem[e], 1)
        tok = (self.sem[e], self.cnt[e], e)
        for w in writes:
            w.w = tok
            w.r = []
        for r in reads:
            r.r.append(tok)
        self.n_ins += 1
        return ins

    def _dsem(self, reg):
        if reg.dsem is None:
            reg.dsem = self.es_sem.enter_context(self.nc.semaphore(f"d{len(self.dsems)}"))
            self.dsems.append(reg)
        return reg.dsem

    def dma(self, q, out, in_, reads=(), writes=(), fn=None, inc=16, nowaw=False, **kw):
        reads = _leaves(reads)
        writes = _leaves(writes)
        deps = self._deps("dma", reads, writes)
        if nowaw:
            mine = {id(w.dsem) for w in writes if w.dsem is not None}
            deps = [t for t in deps if not (t[2] == "dma" and id(t[0]) in mine)]
        self._wait(q, deps)
        holder = writes[0] if writes else reads[0]
        sem = self._dsem(holder)
        if fn is None:
            ins = self.eng[q].dma_start(out=out, in_=in_, **kw)
        else:
            ins = fn(self.eng[q])
        ins.then_inc(sem, inc)
        holder.dcnt += inc
        tok = (sem, holder.dcnt, "dma")
        for w in writes:
            w.w = tok
            w.r = []
        for r in reads:
            r.r.append(tok)
        self.n_ins += 1
        return ins

    def barrier(self):
        toks = [(self.sem[e], self.cnt[e], e) for e in self.eng if self.cnt[e] > 0]
        toks += [(r.dsem, r.dcnt, "dma") for r in self.dsems if r.dcnt > 0]
        for e in self.eng:
            self._wait(e, [t for t in toks if t[2] != e])

    def finish(self):
        toks = [(r.dsem, r.dcnt, "dma") for r in self.dsems if r.dcnt > 0]
        toks += [(self.sem[e], self.cnt[e], e) for e in self.eng if self.cnt[e] > 0 and e != "sp"]
        self._wait("sp", toks)


def build(nblk=SEQ // BLK, dbg=None, phase3=True, do_a=True, do_b=True, ntile3=16, stop3=None):
    nc = bass.Bass("TRN2", target_bir_lowering=False)
    es = ExitStack()
    k = K(nc, es)
    dbg = dbg or {}

    x_d = nc.dram_tensor("x", [SEQ, D], F32, kind="ExternalInput")
    wA_d = nc.dram_tensor("wA", [D, NWA], F32, kind="ExternalInput")
    cvec_d = nc.dram_tensor("cvec", [128, NCV], F32, kind="ExternalInput")
    cmat_d = nc.dram_tensor("cmat", [128, NCM], F32, kind="ExternalInput")
    dbg_d = {}
    for name, shape in dbg.items():
        dbg_d[name] = k.dram(name, shape, F32, kind="ExternalOutput")

    cvec = k.sb("cvec", [128, NCV], F32)
    cmat = k.sb("cmat", [128, NCM], F32)
    k.dma("sp", cvec[:], cvec_d.ap()[:, :], writes=[cvec])
    k.dma("sp", cmat[:], cmat_d.ap()[:, :], writes=[cmat])
    ident_bf = k.sb("ident_bf", [128, 128], BF16)
    ones_bf = k.sb("ones_bf", [128, 128], BF16)
    k.op("dve", lambda e: e.tensor_copy(ident_bf[:], cmat[:, CM_ID:CM_ID + 128]), [cmat], [ident_bf])
    k.op("dve", lambda e: e.tensor_copy(ones_bf[:], cmat[:, CM_ONE:CM_ONE + 128]), [cmat], [ones_bf])
    ML = cmat.t[0:64, CM_ML:CM_ML + 64]
    MUS = cmat.t[0:64, CM_MUS:CM_MUS + 64]
    MUI = cmat.t[0:64, CM_MUI:CM_MUI + 64]
    I64 = cmat.t[0:64, CM_I64:CM_I64 + 64]
    RST = cmat.t[:, CM_RST:CM_RST + 512]

    def bc8(m):
        return m.unsqueeze(1).to_broadcast([64, NCH, 64])

    cder = k.sb("cder", [128, 8], F32)
    for h in range(2):
        k.op("act", lambda e, h=h: e.activation(out=cder[:, h:h + 1], in_=cvec[:, CV_ALOG + h:CV_ALOG + h + 1], func=AF.Exp),
             [cvec], [cder])
        k.op("dve", lambda e, h=h: e.tensor_scalar(out=cder[:, h:h + 1], in0=cder[:, h:h + 1], scalar1=-1.0, scalar2=None,
                                                   op0=ALU.mult), [cder], [cder])
        k.op("dve", lambda e, h=h: e.tensor_tensor(out=cder[:, 6 + h:7 + h], in0=cvec[:, CV_BLB + 2 * h:CV_BLB + 2 * h + 1],
                                                   in1=cvec[:, CV_BLB + 2 * h + 1:CV_BLB + 2 * h + 2], op=ALU.subtract),
             [cvec], [cder])
        k.op("act", lambda e, h=h: e.activation(out=cder[:, 2 + h:3 + h], in_=cder[:, 6 + h:7 + h], func=AF.Sigmoid),
             [cder], [cder])
        k.op("dve", lambda e, h=h: e.tensor_scalar(out=cder[:, 4 + h:5 + h], in0=cder[:, 2 + h:3 + h], scalar1=-1.0, scalar2=1.0,
                                                   op0=ALU.mult, op1=ALU.add), [cder], [cder])

    k.op("dve", lambda e: e.memset(cder[:, 7:8], EPS), [], [cder])
    epsc = cder[:, 7:8]
    cone = k.sb("cone", [128, 1], F32)
    k.op("dve", lambda e: e.memset(cone[:], 1.0), [], [cone])
    onec = cone[:, 0:1]
    xt = k.sb("xt", [128, 2, D], F32, nparts=2)
    junk = k.sb("junk", [128, D], BF16)
    xn = k.sb("xn", [128, 2, D], BF16, nparts=2)
    st = k.sb("st", [128, 4], F32, nparts=2)
    hT = k.sb("hT", [128, 8, BLK], BF16)

    pT = k.ps("pT", [128, 8, 128], BF16)
    pp = [k.ps("pp0", [128, BLK], F32)]
    pm = k.ps("pm", [128, BLK], F32)
    es1 = ExitStack()
    k.es = es1
    W = k.sb("W", [128, 8, NWA], BF16)
    wA_v = wA_d.ap().rearrange("(kc p) c -> p kc c", p=128)
    for i in range(8):
        k.dma("pool", W[:, :, i * 320:(i + 1) * 320], wA_v[:, :, i * 320:(i + 1) * 320], writes=[W], nowaw=True)
    if phase3 and ntile3 > 0:
        euv_d = nc.dram_tensor("euv", [16384, 2 * D], F32, kind="ExternalInput")
        euvb = k.dram("euvb", [16384, 2 * D], BF16)
        for i in range(16):
            k.dma("pool", euvb[i * 1024:(i + 1) * 1024, :], euv_d.ap()[i * 1024:(i + 1) * 1024, :], writes=[euvb], nowaw=True)

    k.es = es1
    pa = k.ps("pa", [128, BLK], F32)
    pb = k.ps("pb", [128, BLK], F32)
    ptr = k.ps("ptr", [128, NCH, 128], BF16)
    po = k.ps("po", [128, BLK], F32)
    prw = k.ps("prw", [128, 2, 128], F32, nparts=2)

    x_v = x_d.ap()
    pp_i = [0]

    def front(tok0, x_v=x_v):
        for tt in range(4):
            s = tt % 2
            k.dma("sp", xt[:, s, :], x_v[tok0 + tt * 128: tok0 + (tt + 1) * 128, :], writes=[xt.p[s]])
            k.op("act", lambda e: e.activation(out=junk[:], in_=xt[:, s, :], func=AF.Square, accum_out=st[:, 2 * s:2 * s + 1]),
                 [xt.p[s]], [junk, st.p[s]])
            k.op("act", lambda e: e.activation(out=st[:, 2 * s + 1:2 * s + 2], in_=st[:, 2 * s:2 * s + 1], func=AF.Sqrt,
                                               bias=epsc, scale=1.0 / D), [st.p[s], cder], [st.p[s]])
            k.op("dve", lambda e: e.reciprocal(out=st[:, 2 * s + 1:2 * s + 2], in_=st[:, 2 * s + 1:2 * s + 2]), [st.p[s]], [st.p[s]])
            k.op("act", lambda e: e.activation(out=xn[:, s, :], in_=xt[:, s, :], func=AF.Copy, scale=st[:, 2 * s + 1:2 * s + 2]),
                 [xt.p[s], st.p[s]], [xn.p[s]])
            for kc in range(8):
                k.op("pe", lambda e, kc=kc: e.transpose(out=pT[:, kc, :], in_=xn[:, s, kc * 128:(kc + 1) * 128], identity=ident_bf[:]),
                     [xn.p[s], ident_bf], [pT])
            k.op("dve", lambda e: e.tensor_tensor(out=hT[:, :, tt * 128:(tt + 1) * 128], in0=pT[:],
                                                  in1=cvec[:, CV_N1:CV_N1 + 8].unsqueeze(2).to_broadcast([128, 8, 128]),
                                                  op=ALU.mult), [pT, cvec], [hT])
            yield 'p'

    def proj(col0, Wt=None):
        Wt = Wt or W
        p = pp[0]
        for kc in range(8):
            k.op("pe", lambda e, kc=kc: e.matmul(p[:], lhsT=Wt[:, kc, col0:col0 + 128], rhs=hT[:, kc, :],
                                                 start=(kc == 0), stop=(kc == 7)), [Wt, hT], [p])
        return p

    def dump(name, src_tile, src_ap, dst_ap):
        k.dma("sp", dst_ap, src_ap, reads=[src_tile], writes=[dbg_d[name]])

    def T(name, cols=BLK, dt=F32, rows=128):
        return k.sb(name, [rows, cols], dt)

    b_sg, b_lf, b_cs, b_eb, b_enb, b_qs, b_omf = [T(f"b_{n}") for n in ("sg", "lf", "cs", "eb", "enb", "qs", "omf")]
    b_df = b_lf
    b_qe, b_ke, b_kdT, b_vT, b_aT, b_sq, b_ob = [T(f"b_{n}", dt=BF16) for n in ("qe", "ke", "kdT", "vT", "aT", "sq", "ob")]
    b_vtm = k.sb("b_vtm", [64, NCH, 128], BF16)
    b_kd = k.sb("b_kd", [64, NCH, 128], BF16)
    b_gate, b_or, b_rs = T("b_gate"), T("b_or"), T("b_rs")
    b_S = [k.sb(f"b_S{h}", [128, 128], F32) for h in range(2)]
    b_Sb = [k.sb(f"b_Sb{h}", [128, 128], BF16) for h in range(2)]
    for h in range(2):
        k.op("dve", lambda e, h=h: e.memset(b_S[h][:], 0.0), [], [b_S[h]])
        k.op("dve", lambda e, h=h: e.memset(b_Sb[h][:], 0.0), [], [b_Sb[h]])
    NB1 = SEQ // BLK
    o_scr = [k.dram(f"o_scr{q}", [512, BLK], BF16) for q in range(NB1)]
    gath = [k.dram(f"gath{q}", [4 * 512, BLK], BF16) for q in range(NB1)]
    ob_scr, oa_scr = 256, 0

    def out_norm(h, blk, o_raw, gate, onorm_col, scr, sq, rs, ob):
        k.op("act", lambda e: e.activation(out=sq[:], in_=o_raw[:], func=AF.Square), [o_raw], [sq])
        k.op("pe", lambda e: e.matmul(pm[:], lhsT=ones_bf[:], rhs=sq[:], start=True, stop=True), [ones_bf, sq], [pm])
        k.op("act", lambda e: e.activation(out=rs[:], in_=pm[:], func=AF.Sqrt, bias=epsc, scale=1.0 / 128), [pm, cder], [rs])
        k.op("dve", lambda e: e.reciprocal(out=rs[:], in_=rs[:]), [rs], [rs])
        k.op("dve", lambda e: e.scalar_tensor_tensor(out=rs[:], in0=o_raw[:], scalar=cvec[:, onorm_col:onorm_col + 1], in1=rs[:],
                                                     op0=ALU.mult, op1=ALU.mult), [o_raw, cvec, rs], [rs])
        k.op("dve", lambda e: e.tensor_tensor(out=ob[:], in0=rs[:], in1=gate[:], op=ALU.mult), [rs, gate], [ob])
        sq_ = o_scr[blk]
        k.dma("sp", sq_[scr + h * 128:scr + (h + 1) * 128, :], ob[:], reads=[ob], writes=[sq_])

    def mixer_b(h, blk):
        c0 = h * 1280
        lb = cder[:, 2 + h:3 + h]
        oml = cder[:, 4 + h:5 + h]
        p = proj(c0 + CB_BF * 128)
        k.op("act", lambda e: e.activation(out=b_sg[:], in_=p[:], func=AF.Sigmoid), [p], [b_sg])
        yield 'p'
        k.op("dve", lambda e: e.tensor_scalar(out=b_sg[:], in0=b_sg[:], scalar1=oml, scalar2=lb, op0=ALU.mult, op1=ALU.add),
             [b_sg, cder], [b_sg])
        yield 'p'
        k.op("act", lambda e: e.activation(out=b_lf[:], in_=b_sg[:], func=AF.Ln), [b_sg], [b_lf])
        yield 'p'
        k.op("dve", lambda e: e.tensor_tensor_scan(out=b_cs[:], data0=RST, data1=b_lf[:], initial=0.0, op0=ALU.mult, op1=ALU.add),
             [cmat, b_lf], [b_cs])
        yield 'p'
        k.op("act", lambda e: e.activation(out=b_eb[:], in_=b_cs[:], func=AF.Exp), [b_cs], [b_eb])
        yield 'p'
        k.op("act", lambda e: e.activation(out=b_enb[:], in_=b_cs[:], func=AF.Exp, scale=-1.0), [b_cs], [b_enb])
        yield 'p'
        k.op("dve", lambda e: e.tensor_scalar(out=b_omf[:], in0=b_sg[:], scalar1=-1.0, scalar2=1.0, op0=ALU.mult, op1=ALU.add),
             [b_sg], [b_omf])
        yield 'p'
        k.op("dve", lambda e: e.tensor_tensor(out=b_ke[:], in0=b_omf[:], in1=b_enb[:], op=ALU.mult), [b_omf, b_enb], [b_ke])
        yield 'p'
        cs3 = b_cs.t[:, :].rearrange("p (c j) -> p c j", j=CH)
        k.op("dve", lambda e: e.tensor_tensor(out=b_df[:, :].rearrange("p (c j) -> p c j", j=CH),
                                              in0=cs3[:, :, CH - 1:CH].to_broadcast([128, NCH, CH]), in1=cs3, op=ALU.subtract),
             [b_cs], [b_df])
        yield 'p'
        k.op("act", lambda e: e.activation(out=b_df[:], in_=b_df[:], func=AF.Exp), [b_df], [b_df])
        yield 'p'
        k.op("dve", lambda e: e.tensor_tensor(out=b_kdT[:], in0=b_omf[:], in1=b_df[:], op=ALU.mult), [b_omf, b_df], [b_kdT])
        yield 'p'
        p = proj(c0 + CB_BQ * 128)
        k.op("act", lambda e: e.activation(out=b_qs[:], in_=p[:], func=AF.Silu), [p], [b_qs])
        yield 'p'
        k.op("dve", lambda e: e.tensor_tensor(out=b_qe[:], in0=b_qs[:], in1=b_eb[:], op=ALU.mult), [b_qs, b_eb], [b_qe])
        yield 'p'
        p = proj(c0 + CB_BI * 128)
        k.op("act", lambda e: e.activation(out=b_vT[:], in_=p[:], func=AF.Copy), [p], [b_vT])
        yield 'p'
        p = proj(c0 + CB_BGATE * 128)
        k.op("act", lambda e: e.activation(out=b_gate[:], in_=p[:], func=AF.Sigmoid), [p], [b_gate])
        yield 'p'
        for src, dst in ((b_vT, b_vtm), (b_kdT, b_kd)):
            for c in range(NCH):
                k.op("pe", lambda e, c=c, src=src: e.transpose(out=ptr[0:64, c, :], in_=src[:, c * CH:(c + 1) * CH], identity=ident_bf[:]),
                     [src, ident_bf], [ptr])
                yield 'p'
            k.op("dve", lambda e, dst=dst: e.tensor_copy(dst[:], ptr[0:64, :, :]), [ptr], [dst])
            yield 'p'
        for c in range(NCH):
            k.op("pe", lambda e, c=c: e.matmul(pm[0:64, c * CH:(c + 1) * CH], lhsT=b_ke[:, c * CH:(c + 1) * CH],
                                               rhs=b_qe[:, c * CH:(c + 1) * CH], start=True, stop=True), [b_ke, b_qe], [pm])
            yield 'p'
        k.op("dve", lambda e: e.tensor_tensor(out=b_aT[0:64, :].rearrange("p (c j) -> p c j", j=CH),
                                              in0=pm[0:64, :].rearrange("p (c j) -> p c j", j=CH), in1=bc8(MUI), op=ALU.mult),
             [pm, cmat], [b_aT])
        yield 'p'
        yield 'REC'
        S, Sb = b_S[h], b_Sb[h]
        for c in range(NCH):
            cs = slice(c * CH, (c + 1) * CH)
            k.op("pe", lambda e: e.matmul(po[:, cs], lhsT=Sb[:], rhs=b_qe[:, cs], start=True, stop=False), [Sb, b_qe], [po])
            k.op("pe", lambda e: e.matmul(po[:, cs], lhsT=b_vtm[0:64, c, :], rhs=b_aT[0:64, cs], start=False, stop=True),
                 [b_vtm, b_aT], [po])
            k.op("pe", lambda e: e.matmul(prw[:, 1, :], lhsT=b_kd[0:64, c, :], rhs=b_vtm[0:64, c, :], start=True, stop=True),
                 [b_kd, b_vtm], [prw.p[1]])
            k.op("dve", lambda e: e.scalar_tensor_tensor(out=S[:], in0=S[:], scalar=b_eb[:, c * CH + CH - 1:c * CH + CH], in1=prw[:, 1, :],
                                                         op0=ALU.mult, op1=ALU.add), [S, b_eb, prw.p[1]], [S])
            k.op("act", lambda e: e.activation(out=Sb[:], in_=S[:], func=AF.Copy), [S], [Sb])
            yield 'r'
        k.op("act", lambda e: e.activation(out=b_or[:], in_=po[:], func=AF.Copy), [po], [b_or])
        if "ob_raw" in dbg:
            dump("ob_raw", b_or, b_or[:], dbg_d["ob_raw"][h * 128:(h + 1) * 128, blk * BLK:(blk + 1) * BLK])
        out_norm(h, blk, b_or, b_gate, CV_BON, ob_scr, b_sq, b_rs, b_ob)
        yield 'r'

    a_beta, a_g, a_Gb, a_cv, a_qs, a_ks, a_vs, a_rn, a_kb, a_or, a_rs = [
        T(f"a_{n}") for n in ("beta", "g", "Gb", "cv", "qs", "ks", "vs", "rn", "kb", "or", "rs")]
    a_EL, a_qn, a_kn = a_g, a_qs, a_ks
    a_sq, a_qnb, a_knb, a_kbb, a_rwT, a_kdT, a_ruT, a_ob, a_sq2 = [
        T(f"a_{n}", dt=BF16) for n in ("sq", "qnb", "knb", "kbb", "rwT", "kdT", "ruT", "ob", "sq2")]
    a_E2 = [T(f"a_E{i}") for i in range(2)]
    a_gate2 = [T(f"a_gate{i}") for i in range(2)]
    a_qd2 = [T(f"a_qd{i}", dt=BF16) for i in range(2)]
    a_aqk2 = [T(f"a_aqk{i}", dt=BF16, rows=64) for i in range(2)]
    a_t64, a_d, a_Dm, a_DTs, a_DTi = [T(f"a_{n}", rows=64) for n in ("t64", "d", "Dm", "DTs", "DTi")]
    a_Gtm = k.sb("a_Gtm", [64, NCH], F32)
    a_A = [T(f"a_A{i}", rows=64) for i in range(2)]
    a_B = [T(f"a_B{i}", rows=64) for i in range(2)]
    a_Y = [T(f"a_Y{i}", rows=64) for i in range(2)]
    a_Y5 = T("a_Y5", dt=BF16, rows=64)
    a_ru = k.sb("a_ru", [64, NCH, 128], BF16)
    a_rw = k.sb("a_rw", [64, NCH, 128], BF16)
    a_kd2 = [k.sb(f"a_kd{i}", [64, NCH, 128], BF16) for i in range(2)]
    a_u2 = [k.sb(f"a_u{i}", [64, NCH, 128], F32) for i in range(2)]
    a_wT2 = [T(f"a_wT{i}", dt=BF16) for i in range(2)]
    a_vn = k.sb("a_vn", [64, 128], BF16)
    a_x = [[k.sb(f"a_x{h}{qi}", [128, BLK + 3], F32) for qi in range(3)] for h in range(2)]
    a_S = [k.sb(f"a_S{h}", [128, 128], F32) for h in range(2)]
    a_Sb = [k.sb(f"a_Sb{h}", [128, 128], BF16) for h in range(2)]
    for h in range(2):
        k.op("dve", lambda e: e.memset(a_S[h][:], 0.0), [], [a_S[h]])
        k.op("dve", lambda e: e.memset(a_Sb[h][:], 0.0), [], [a_Sb[h]])
        for qi in range(3):
            k.op("dve", lambda e: e.memset(a_x[h][qi][:], 0.0), [], [a_x[h][qi]])

    def v3(t, rows=128):
        return t.t[0:rows, :].rearrange("p (c j) -> p c j", j=CH)

    print("sbuf bytes remaining (phase 1):", nc.sbuf_bytes_remaining)

    def mixer_a(h, blk):
        a_E, a_gate, a_qd, a_aqk = a_E2[h], a_gate2[h], a_qd2[h], a_aqk2[h]
        a_kd, a_u, a_wT = a_kd2[h], a_u2[h], a_wT2[h]
        c0 = h * 1280
        nA = cder[:, h:h + 1]
        dtb = cvec[:, CV_DTB + h:CV_DTB + h + 1]
        p = proj(c0 + CB_ABETA * 128)
        k.op("act", lambda e: e.activation(out=a_beta[:], in_=p[:], func=AF.Sigmoid), [p], [a_beta])
        yield 'p'
        p = proj(c0 + CB_AALPHA * 128)
        k.op("act", lambda e: e.activation(out=a_g[:], in_=p[:], func=AF.Exp, bias=dtb), [p, cvec], [a_g])
        yield 'p'
        k.op("act", lambda e: e.activation(out=a_g[:], in_=a_g[:], func=AF.Ln, bias=onec), [a_g, cone], [a_g])
        yield 'p'
        k.op("dve", lambda e: e.tensor_scalar(out=a_g[:], in0=a_g[:], scalar1=nA, scalar2=None, op0=ALU.mult), [a_g, cder], [a_g])
        yield 'p'
        k.op("dve", lambda e: e.tensor_tensor_scan(out=a_Gb[:], data0=RST, data1=a_g[:], initial=0.0, op0=ALU.mult, op1=ALU.add),
             [cmat, a_g], [a_Gb])
        yield 'p'
        k.op("act", lambda e: e.activation(out=a_E[:], in_=a_Gb[:], func=AF.Exp), [a_Gb], [a_E])
        yield 'p'
        G3 = v3(a_Gb)
        k.op("dve", lambda e: e.tensor_tensor(out=v3(a_EL), in0=G3[:, :, CH - 1:CH].to_broadcast([128, NCH, CH]), in1=G3, op=ALU.subtract),
             [a_Gb], [a_EL])
        yield 'p'
        k.op("act", lambda e: e.activation(out=a_EL[:], in_=a_EL[:], func=AF.Exp), [a_EL], [a_EL])
        yield 'p'
        for qi, (cb, dst) in enumerate(((CB_AQ, a_qs), (CB_AK, a_ks), (CB_AV, a_vs))):
            xb = a_x[h][qi]
            p = proj(c0 + cb * 128)
            k.op("dve", lambda e: e.tensor_copy(xb[:, 0:3], xb[:, BLK:BLK + 3]), [xb], [xb])
            yield 'p'
            k.op("act", lambda e: e.activation(out=xb[:, 3:BLK + 3], in_=p[:], func=AF.Copy), [p], [xb])
            yield 'p'
            wc = CV_CONV + h * 12 + qi * 4
            k.op("dve", lambda e: e.tensor_scalar(out=a_cv[:], in0=xb[:, 0:BLK], scalar1=cvec[:, wc:wc + 1], scalar2=None, op0=ALU.mult),
                 [xb, cvec], [a_cv])
            yield 'p'
            for j in range(1, 4):
                k.op("dve", lambda e: e.scalar_tensor_tensor(out=a_cv[:], in0=xb[:, j:j + BLK], scalar=cvec[:, wc + j:wc + j + 1],
                                                             in1=a_cv[:], op0=ALU.mult, op1=ALU.add), [xb, cvec, a_cv], [a_cv])
                yield 'p'
            k.op("act", lambda e: e.activation(out=dst[:], in_=a_cv[:], func=AF.Silu), [a_cv], [dst])
            yield 'p'
        for src, dst, scl in ((a_qs, a_qn, 128.0 ** -0.5), (a_ks, a_kn, 1.0)):
            k.op("act", lambda e: e.activation(out=a_sq[:], in_=src[:], func=AF.Square), [src], [a_sq])
            yield 'p'
            k.op("pe", lambda e: e.matmul(pm[:], lhsT=ones_bf[:], rhs=a_sq[:], start=True, stop=True), [ones_bf, a_sq], [pm])
            yield 'p'
            k.op("act", lambda e: e.activation(out=a_rn[:], in_=pm[:], func=AF.Sqrt, bias=epsc), [pm, cder], [a_rn])
            yield 'p'
            k.op("dve", lambda e: e.reciprocal(out=a_rn[:], in_=a_rn[:]), [a_rn], [a_rn])
            yield 'p'
            k.op("dve", lambda e: e.scalar_tensor_tensor(out=dst[:], in0=src[:], scalar=scl, in1=a_rn[:], op0=ALU.mult, op1=ALU.mult),
                 [src, a_rn], [dst])
            yield 'p'
        k.op("act", lambda e: e.activation(out=a_qnb[:], in_=a_qn[:], func=AF.Copy), [a_qn], [a_qnb])
        yield 'p'
        k.op("act", lambda e: e.activation(out=a_knb[:], in_=a_kn[:], func=AF.Copy), [a_kn], [a_knb])
        yield 'p'
        k.op("dve", lambda e: e.tensor_tensor(out=a_qd[:], in0=a_qn[:], in1=a_E[:], op=ALU.mult), [a_qn, a_E], [a_qd])
        yield 'p'
        k.op("dve", lambda e: e.tensor_tensor(out=a_kb[:], in0=a_kn[:], in1=a_beta[:], op=ALU.mult), [a_kn, a_beta], [a_kb])
        yield 'p'
        k.op("act", lambda e: e.activation(out=a_kbb[:], in_=a_kb[:], func=AF.Copy), [a_kb], [a_kbb])
        yield 'p'
        k.op("dve", lambda e: e.tensor_tensor(out=a_rwT[:], in0=a_kb[:], in1=a_E[:], op=ALU.mult), [a_kb, a_E], [a_rwT])
        yield 'p'
        k.op("dve", lambda e: e.tensor_tensor(out=a_kdT[:], in0=a_kn[:], in1=a_EL[:], op=ALU.mult), [a_kn, a_EL], [a_kdT])
        yield 'p'
        k.op("dve", lambda e: e.tensor_tensor(out=a_ruT[:], in0=a_vs[:], in1=a_beta[:], op=ALU.mult), [a_vs, a_beta], [a_ruT])
        yield 'p'
        p = proj(c0 + CB_AGATE * 128)
        k.op("act", lambda e: e.activation(out=a_gate[:], in_=p[:], func=AF.Silu), [p], [a_gate])
        yield 'p'
        k.op("dve", lambda e: e.tensor_tensor(out=v3(a_t64, 64), in0=v3(a_Gb, 64), in1=bc8(I64), op=ALU.mult), [a_Gb, cmat], [a_t64])
        yield 'p'
        k.op("dve", lambda e: e.tensor_reduce(out=a_Gtm[:], in_=v3(a_t64, 64), axis=AX.X, op=ALU.add), [a_t64], [a_Gtm])
        yield 'p'
        k.op("dve", lambda e: e.tensor_tensor(out=v3(a_d, 64), in0=v3(a_Gb, 64), in1=a_Gtm.t[:, :].unsqueeze(2).to_broadcast([64, NCH, CH]),
                                              op=ALU.subtract), [a_Gb, a_Gtm], [a_d])
        yield 'p'
        k.op("dve", lambda e: e.tensor_scalar(out=a_t64[:], in0=a_d[:], scalar1=0.0, scalar2=None, op0=ALU.max), [a_d], [a_t64])
        yield 'p'
        k.op("act", lambda e: e.activation(out=a_t64[:], in_=a_t64[:], func=AF.Exp, scale=-1.0), [a_t64], [a_t64])
        yield 'p'
        k.op("dve", lambda e: e.tensor_tensor(out=v3(a_Dm, 64), in0=v3(a_t64, 64), in1=bc8(ML), op=ALU.mult), [a_t64, cmat], [a_Dm])
        yield 'p'
        k.op("dve", lambda e: e.tensor_scalar(out=a_d[:], in0=a_d[:], scalar1=0.0, scalar2=None, op0=ALU.min), [a_d], [a_d])
        yield 'p'
        k.op("act", lambda e: e.activation(out=a_d[:], in_=a_d[:], func=AF.Exp), [a_d], [a_d])
        yield 'p'
        k.op("dve", lambda e: e.tensor_tensor(out=v3(a_DTs, 64), in0=v3(a_d, 64), in1=bc8(MUS), op=ALU.mult), [a_d, cmat], [a_DTs])
        yield 'p'
        k.op("dve", lambda e: e.tensor_tensor(out=v3(a_DTi, 64), in0=v3(a_d, 64), in1=bc8(MUI), op=ALU.mult), [a_d, cmat], [a_DTi])
        yield 'p'
        for (l, r, msk, dst) in ((a_kbb, a_knb, a_Dm, a_A[0]), (a_knb, a_kbb, a_DTs, a_B[0]), (a_knb, a_qnb, a_DTi, a_aqk)):
            for c in range(NCH):
                cs = slice(c * CH, (c + 1) * CH)
                k.op("pe", lambda e: e.matmul(pm[0:64, cs], lhsT=l[:, cs], rhs=r[:, cs], start=True, stop=True), [l, r], [pm])
                yield 'p'
            k.op("dve", lambda e: e.tensor_tensor(out=(dst[:] if dst is a_aqk else dst[:].bitcast(mybir.dt.float32r)), in0=pm[0:64, :], in1=msk[:], op=ALU.mult),
                 [pm, msk], [dst])
            yield 'p'
        k.op("dve", lambda e: e.tensor_tensor(out=a_Y[0].t[0:64, :].bitcast(mybir.dt.float32r).rearrange("p (c j) -> p c j", j=CH), in0=bc8(I64), in1=v3(a_B[0], 64), op=ALU.subtract), [cmat, a_B[0]], [a_Y[0]])
        yield 'p'
        for s_ in range(1, 6):
            A0, B0, Y0 = a_A[(s_ - 1) % 2], a_B[(s_ - 1) % 2], a_Y[(s_ - 1) % 2]
            A1, B1, Y1 = a_A[s_ % 2], a_B[s_ % 2], a_Y[s_ % 2]
            for c in range(NCH):
                cs = slice(c * CH, (c + 1) * CH)
                k.op("pe", lambda e: e.matmul(pa[0:64, cs], lhsT=B0[:, cs].bitcast(mybir.dt.float32r), rhs=A0[:, cs].bitcast(mybir.dt.float32r), start=True, stop=True), [A0, B0], [pa])
                yield 'p'
            k.op("act", lambda e: e.activation(out=A1[:].bitcast(mybir.dt.float32r), in_=pa[0:64, :], func=AF.Copy), [pa], [A1])
            yield 'p'
            if s_ < 5:
                for c in range(NCH):
                    cs = slice(c * CH, (c + 1) * CH)
                    k.op("pe", lambda e: e.matmul(pb[0:64, cs], lhsT=A0[:, cs].bitcast(mybir.dt.float32r), rhs=B0[:, cs].bitcast(mybir.dt.float32r), start=True, stop=True), [A0, B0], [pb])
                k.op("act", lambda e: e.activation(out=B1[:].bitcast(mybir.dt.float32r), in_=pb[0:64, :], func=AF.Copy), [pb], [B1])
                yield 'p'
            for c in range(NCH):
                cs = slice(c * CH, (c + 1) * CH)
                k.op("pe", lambda e: e.matmul(pm[0:64, cs], lhsT=A1[:, cs].bitcast(mybir.dt.float32r), rhs=Y0[:, cs].bitcast(mybir.dt.float32r), start=True, stop=True), [A1, Y0], [pm])
                yield 'p'
            Yd = a_Y5 if s_ == 5 else Y1
            k.op("dve", lambda e: e.tensor_tensor(out=(Yd[:] if s_ == 5 else Yd[:].bitcast(mybir.dt.float32r)), in0=Y0[:], in1=pm[0:64, :], op=ALU.add), [Y0, pm], [Yd])
            yield 'p'
        for src, dst in ((a_ruT, a_ru), (a_rwT, a_rw), (a_kdT, a_kd)):
            for c in range(NCH):
                k.op("pe", lambda e: e.transpose(out=ptr[0:64, c, :], in_=src[:, c * CH:(c + 1) * CH], identity=ident_bf[:]),
                     [src, ident_bf], [ptr])
                yield 'p'
            k.op("dve", lambda e: e.tensor_copy(dst[:], ptr[0:64, :, :]), [ptr], [dst])
            yield 'p'
        for half, pu in ((0, pa), (1, pb)):
            for c4 in range(4):
                c = half * 4 + c4
                cs = slice(c * CH, (c + 1) * CH)
                k.op("pe", lambda e: e.matmul(pu[0:64, c4 * 128:(c4 + 1) * 128], lhsT=a_Y5[:, cs], rhs=a_ru[:, c, :], start=True, stop=True),
                     [a_Y5, a_ru], [pu])
                yield 'p'
            k.op("act", lambda e: e.activation(out=a_u[:, half * 4:half * 4 + 4, :], in_=pu[0:64, :].rearrange("p (c d) -> p c d", d=128),
                                               func=AF.Copy), [pu], [a_u])
            yield 'p'
        for c in range(NCH):
            cs = slice(c * CH, (c + 1) * CH)
            k.op("pe", lambda e: e.matmul(pm[:, cs], lhsT=a_rw[:, c, :], rhs=a_Y5[:, cs], start=True, stop=True), [a_rw, a_Y5], [pm])
            yield 'p'
        k.op("act", lambda e: e.activation(out=a_wT[:], in_=pm[:], func=AF.Copy), [pm], [a_wT])
        yield 'p'
        yield 'REC'
        S, Sb = a_S[h], a_Sb[h]
        for c in range(NCH):
            cs = slice(c * CH, (c + 1) * CH)
            k.op("pe", lambda e: e.matmul(prw[0:64, 0, :], lhsT=a_wT[:, cs], rhs=Sb[:], start=True, stop=True), [a_wT, Sb], [prw.p[0]])
            k.op("dve", lambda e: e.tensor_tensor(out=a_vn[:], in0=a_u[:, c, :], in1=prw[0:64, 0, :], op=ALU.subtract),
                 [a_u, prw.p[0]], [a_vn])
            k.op("pe", lambda e: e.matmul(po[:, cs], lhsT=Sb[:], rhs=a_qd[:, cs], start=True, stop=False), [Sb, a_qd], [po])
            k.op("pe", lambda e: e.matmul(po[:, cs], lhsT=a_vn[:], rhs=a_aqk[:, cs], start=False, stop=True), [a_vn, a_aqk], [po])
            k.op("pe", lambda e: e.matmul(prw[:, 1, :], lhsT=a_kd[:, c, :], rhs=a_vn[:], start=True, stop=True), [a_kd, a_vn], [prw.p[1]])
            k.op("dve", lambda e: e.scalar_tensor_tensor(out=S[:], in0=S[:], scalar=a_E[:, c * CH + CH - 1:c * CH + CH], in1=prw[:, 1, :],
                                                         op0=ALU.mult, op1=ALU.add), [S, a_E, prw.p[1]], [S])
            k.op("act", lambda e: e.activation(out=Sb[:], in_=S[:], func=AF.Copy), [S], [Sb])
            yield 'r'
        k.op("act", lambda e: e.activation(out=a_or[:], in_=po[:], func=AF.Copy), [po], [a_or])
        if "oa_raw" in dbg:
            dump("oa_raw", a_or, a_or[:], dbg_d["oa_raw"][h * 128:(h + 1) * 128, blk * BLK:(blk + 1) * BLK])
        out_norm(h, blk, a_or, a_gate, CV_AON, oa_scr, a_sq2, a_rs, a_ob)
        yield 'r'

    def coll(blk):
        k.dma("pool", None, None, reads=[o_scr[blk]], writes=[gath[blk]],
              fn=lambda e: e.collective_compute("AllGather", ALU.bypass, replica_groups=[[0, 1, 2, 3], [4, 5, 6, 7]],
                                                ins=[o_scr[blk].t.ap().opt()], outs=[gath[blk].t.ap().opt()]), inc=1)

    def unit(kind, h, blk):
        if kind == "b" and h == 0:
            yield from front(blk * BLK)
        if kind == "b":
            yield from mixer_b(h, blk)
        else:
            yield from mixer_a(h, blk)
        if phase3 and kind == last_kind and h == 1:
            coll(blk)

    kinds = (["b"] if do_b else []) + (["a"] if do_a else [])
    last_kind = kinds[-1]
    units = [unit(kd, h, blk) for blk in range(nblk) for h in range(2) for kd in kinds]
    PRE_PER_STEP = 24

    def run_pre(g):
        for v in g:
            if v == 'REC':
                return True
        return False

    nU = len(units)
    ready = [False] * nU
    is_a = [(kd == "a") for blk in range(nblk) for h in range(2) for kd in kinds]

    def adv(ui_, n):
        c = 0
        while c < n and not ready[ui_]:
            v2 = next(units[ui_], 'END')
            c += 1
            if v2 == 'REC' or v2 == 'END':
                ready[ui_] = True
        return c

    adv(0, 10 ** 9)
    for ui in range(nU):
        for v in units[ui]:
            budget = PRE_PER_STEP
            if ui + 1 < nU:
                budget -= adv(ui + 1, budget)
            if budget > 0 and is_a[ui] and ui + 2 < nU and ready[ui + 1] and is_a[ui + 2]:
                adv(ui + 2, budget)
        if ui + 1 < nU:
            adv(ui + 1, 10 ** 9)

    if phase3:
        NT3 = SEQ // 4
        wG_d = nc.dram_tensor("wG", [D, 2048], F32, kind="ExternalInput")
        wBa_d = nc.dram_tensor("wBa", [D, D], F32, kind="ExternalInput")
        wBb_d = nc.dram_tensor("wBb", [D, D], F32, kind="ExternalInput")
        wO_d = nc.dram_tensor("wO", [D, D], F32, kind="ExternalInput")
        wPQ_d = nc.dram_tensor("wPQ", [D, 2048], F32, kind="ExternalInput")
        skT_d = nc.dram_tensor("skT", [128, 2048], F32, kind="ExternalInput")
        n2b_d = nc.dram_tensor("n2b", [128, D], F32, kind="ExternalInput")
        fnb_d = nc.dram_tensor("fnb", [128, D], F32, kind="ExternalInput")

        x3_d = nc.dram_tensor("x3", [NT3, D], F32, kind="ExternalInput")
        sel_d = nc.dram_tensor("sel", [128, 4], F32, kind="ExternalInput")
        out_d = k.dram("out", [NT3, D], F32, kind="ExternalOutput")
        x1_scr = k.dram("x1_scr", [NT3, D], F32)
        k.barrier()
        es1.close()

        if stop3 == "gather":
            k.finish()
            return nc, es
        es3 = ExitStack()
        k.es = es3
        def load_w(dst, src_d, ncols):
            v = src_d.ap().rearrange("(kc p) c -> p kc c", p=128)
            for i in range(ncols // 512):
                k.dma("pool", dst[:, :, i * 512:(i + 1) * 512], v[:, :, i * 512:(i + 1) * 512], writes=[dst], nowaw=True)

        Wg = k.sb("Wg", [128, 8, 2048], BF16)
        Wa = k.sb("Wa", [128, 8, D], BF16)
        Wb = k.sb("Wb", [128, 8, D], BF16)
        Wo = k.sb("Wo", [128, 8, D], BF16)
        load_w(Wg, wG_d, 2048)
        load_w(Wa, wBa_d, D)
        load_w(Wb, wBb_d, D)
        load_w(Wo, wO_d, D)
        ppB = k.ps("ppB", [128, BLK], F32)
        pmB = k.ps("pmB", [128, BLK], F32)
        p3_i = [0]

        def proj3(col0):
            p = (pp[0], ppB)[p3_i[0] % 2]
            p3_i[0] += 1
            for kc in range(8):
                k.op("pe", lambda e: e.matmul(p[:], lhsT=Wg[:, kc, col0:col0 + 128], rhs=hT[:, kc, :], start=(kc == 0), stop=(kc == 7)),
                     [Wg, hT], [p])
            return p

        sga = k.sb("sga", [128, 8, BLK], BF16)
        sgb = k.sb("sgb", [128, 8, BLK], BF16)
        oaT = k.sb("oaT", [128, 8, BLK], BF16)
        obT = k.sb("obT", [128, 8, BLK], BF16)
        mixT = k.sb("mixT", [128, 8, BLK], BF16)
        m_t1 = k.sb("m_t1", [128, BLK], F32)
        m_t2 = k.sb("m_t2", [128, BLK], F32)
        xr = k.sb("xr", [128, D], F32)
        x1t = k.sb("x1t", [128, D], F32)
        sel = k.sb("sel", [128, 4], F32)
        k.dma("sp", sel[:], sel_d.ap()[:, :], writes=[sel])
        gv = [g_.t.ap().rearrange("(r ab hl p) t -> ab p r hl t", r=4, ab=2, hl=2, p=128) for g_ in gath]
        x3_v = x3_d.ap()
        cand = k.sb("cand", [128, 2, 8, BLK], BF16, nparts=8)
        for blk in range(NT3 // BLK):
            for _ in front(blk * BLK, x3_v):
                pass
            ci = 0
            for ab, dst in ((0, oaT), (1, obT)):
                for q in range(4):
                    s_ = ci % 2
                    ci += 1
                    gi = q * 4 + blk
                    for r in range(4):
                        k.dma("sp", cand[:, s_, 2 * r:2 * r + 2, :], gv[gi][ab][:, r, :, :],
                              reads=[gath[gi]], writes=[cand.p[s_ * 4 + r]])
                    if q == 0:
                        k.op("dve", lambda e: e.tensor_scalar(out=dst[:], in0=cand[:, s_, :, :], scalar1=sel[:, 0:1], scalar2=None, op0=ALU.mult),
                             [cand.p[s_ * 4:s_ * 4 + 4], sel], [dst])
                    else:
                        k.op("dve", lambda e: e.scalar_tensor_tensor(out=dst[:], in0=cand[:, s_, :, :], scalar=sel[:, q:q + 1], in1=dst[:],
                                                                     op0=ALU.mult, op1=ALU.add), [cand.p[s_ * 4:s_ * 4 + 4], sel, dst], [dst])
            for cb in range(8):
                p = proj3(cb * 128)
                k.op("act", lambda e: e.activation(out=sga[:, cb, :], in_=p[:], func=AF.Sigmoid), [p], [sga])
                p = proj3(D + cb * 128)
                k.op("act", lambda e: e.activation(out=sgb[:, cb, :], in_=p[:], func=AF.Sigmoid), [p], [sgb])
            for cb in range(8):
                for kc in range(8):
                    k.op("pe", lambda e: e.matmul(pm[:], lhsT=Wa[:, kc, cb * 128:(cb + 1) * 128], rhs=oaT[:, kc, :],
                                                  start=(kc == 0), stop=(kc == 7)), [Wa, oaT], [pm])
                k.op("dve", lambda e: e.tensor_tensor(out=m_t1[:], in0=pm[:], in1=sga[:, cb, :], op=ALU.mult), [pm, sga], [m_t1])
                for kc in range(8):
                    k.op("pe", lambda e: e.matmul(pmB[:], lhsT=Wb[:, kc, cb * 128:(cb + 1) * 128], rhs=obT[:, kc, :],
                                                  start=(kc == 0), stop=(kc == 7)), [Wb, obT], [pmB])
                k.op("dve", lambda e: e.tensor_tensor(out=m_t2[:], in0=pmB[:], in1=sgb[:, cb, :], op=ALU.mult), [pmB, sgb], [m_t2])
                k.op("dve", lambda e: e.tensor_tensor(out=mixT[:, cb, :], in0=m_t1[:], in1=m_t2[:], op=ALU.add), [m_t1, m_t2], [mixT])
            for tt in range(4):
                r0 = blk * BLK + tt * 128
                k.dma("sp", xr[:], x3_v[r0:r0 + 128, :], writes=[xr])
                for half in range(2):
                    p = (pp[0], ppB)[half]
                    for kc in range(8):
                        k.op("pe", lambda e: e.matmul(p[:], lhsT=mixT[:, kc, tt * 128:(tt + 1) * 128], rhs=Wo[:, kc, half * 512:(half + 1) * 512],
                                                      start=(kc == 0), stop=(kc == 7)), [mixT, Wo], [p])
                    k.op("dve", lambda e: e.tensor_tensor(out=x1t[:, half * 512:(half + 1) * 512], in0=p[:], in1=xr[:, half * 512:(half + 1) * 512],
                                                          op=ALU.add), [p, xr], [x1t])
                k.dma("sp", x1_scr[r0:r0 + 128, :], x1t[:], reads=[x1t], writes=[x1_scr])
                if "x1" in dbg:
                    dump("x1", x1t, x1t[:], dbg_d["x1"][r0:r0 + 128, :])
        k.barrier()
        es3.close()
        if stop3 == "3a":
            k.finish()
            return nc, es

        es4 = ExitStack()
        k.es = es4
        Wpq = k.sb("Wpq", [128, 8, 2048], BF16)
        wpq_v = wPQ_d.ap().rearrange("(kc p) c -> p kc c", p=128)
        for i in range(4):
            k.dma("pool", Wpq[:, :, i * 512:(i + 1) * 512], wpq_v[:, :, i * 512:(i + 1) * 512], writes=[Wpq], nowaw=True)
        skT = k.sb("skTb", [128, 16, 128], BF16)
        k.dma("pool", skT[:, :, :].rearrange("p g k -> p (g k)"), skT_d.ap()[:, :], writes=[skT])
        n2b = k.sb("n2bs", [128, D], F32)
        fnb = k.sb("fnbs", [128, D], F32)
        k.dma("sp", n2b[:], n2b_d.ap()[:, :], writes=[n2b])
        k.dma("sp", fnb[:], fnb_d.ap()[:, :], writes=[fnb])
        x1p2 = [k.sb(f"x1p{i}", [128, D], F32) for i in range(2)]
        h2n2 = [k.sb(f"h2n{i}", [128, D], F32) for i in range(2)]
        h2b2 = [k.sb(f"h2b{i}", [128, D], BF16) for i in range(2)]
        prodb = k.sb("prodb", [128, 2, D], BF16, nparts=2)
        h2T = k.sb("h2T", [128, 8, 128], BF16)
        qTb = k.sb("qTb", [128, 16, 128], BF16)
        s_sb = k.sb("s_sb", [128, 16, 128], F32)
        tv = k.sb("tv", [128, 16, 16], F32)
        ti = k.sb("ti", [128, 16, 16], U32)
        tif = k.sb("tif", [128, 16, 16], F32)
        cs_ = k.sb("cand_s", [128, 8, 256], F32)
        bs_ = k.sb("best_s", [128, 8, 16], F32)
        pos = k.sb("pos", [128, 8, 16], U32)
        posf = k.sb("posf", [128, 8, 16], F32)
        pa_ = k.sb("pa_", [128, 8, 16], F32)
        pb_ = k.sb("pb_", [128, 8, 16], F32)
        eq = k.sb("eq", [128, 8, 16, 16], F32)
        i1 = k.sb("i1", [128, 8, 16], F32)
        i2 = k.sb("i2", [128, 8, 16], F32)
        idxf = k.sb("idxf", [128, 128], F32)
        idxu2 = [k.sb(f"idxu{i}", [128, 128], U32) for i in range(2)]
        gsm = k.sb("gsm", [128, 16], F32)
        gate2 = [k.sb(f"gate{i}", [128, 8, 16], F32) for i in range(2)]
        gatef2 = [g_.t[:, :, :].rearrange("p h k -> p (h k)") for g_ in gate2]
        BG_PER_STEP = 8
        NG = 14
        GS = 4
        NGR = 128 // GS
        hpre = k.sb("hpre", [128, 128], F32, nparts=32)
        gcol = k.sb("gcol", [128, 128], F32, nparts=8)
        gw = k.sb("gw", [128, 128], F32, nparts=8)
        UVg = k.sb("UVg", [128, NG, 2 * D], BF16, nparts=NG)
        Dm = k.sb("Dm", [128, NG, 128], BF16, nparts=NG)
        scrU = k.sb("scrU", [128, D], BF16)
        x2t = k.sb("x2t", [128, D], F32)
        obuf = k.sb("obuf", [128, D], F32)
        print("sbuf bytes remaining (3b):", nc.sbuf_bytes_remaining)
        ps_s = k.ps("ps_s", [128, 16, 128], F32, nparts=4)
        py1 = k.ps("py1", [128, 512], F32)
        pq = pm
        py0 = pp[0]
        IOTA = cmat.t[:, CM_IOTA:CM_IOTA + 16]
        THR = cmat.t[:, CM_THR:CM_THR + 16]
        tv4 = tv.t[:, :, :].rearrange("p (h two) k -> p h two k", two=2)
        tif4 = tif.t[:, :, :].rearrange("p (h two) k -> p h two k", two=2)
        euv_v = euvb.t.ap()
        def tile_front(tile_i):
            pb_i = tile_i % 2
            x1p, h2n, idxu, gate, gatef = x1p2[pb_i], h2n2[pb_i], idxu2[pb_i], gate2[pb_i], gatef2[pb_i]
            h2b = h2b2[pb_i]
            r0 = tile_i * 128
            k.dma("sp", x1p[:], x1_scr[r0:r0 + 128, :], reads=[x1_scr], writes=[x1p])
            yield
            k.op("act", lambda e: e.activation(out=junk[:], in_=x1p[:], func=AF.Square, accum_out=st[:, 0:1]), [x1p], [junk, st.p[0]])
            yield
            k.op("act", lambda e: e.activation(out=st[:, 1:2], in_=st[:, 0:1], func=AF.Sqrt, bias=epsc, scale=1.0 / D), [st.p[0], cder], [st.p[0]])
            yield
            k.op("dve", lambda e: e.reciprocal(out=st[:, 1:2], in_=st[:, 1:2]), [st.p[0]], [st.p[0]])
            yield
            k.op("dve", lambda e: e.scalar_tensor_tensor(out=h2n[:], in0=x1p[:], scalar=st[:, 1:2], in1=n2b[:], op0=ALU.mult, op1=ALU.mult),
                 [x1p, st.p[0], n2b], [h2n])
            yield
            k.op("act", lambda e: e.activation(out=h2b[:], in_=h2n[:], func=AF.Copy), [h2n], [h2b])
            yield
            for kc in range(8):
                k.op("pe", lambda e: e.transpose(out=pT[:, kc, :], in_=h2b[:, kc * 128:(kc + 1) * 128], identity=ident_bf[:]), [h2b, ident_bf], [pT])
                yield
            k.op("act", lambda e: e.activation(out=h2T[:], in_=pT[:], func=AF.Copy), [pT], [h2T])
            yield
            for g4 in range(4):
                for gg in range(4):
                    g = g4 * 4 + gg
                    for kc in range(8):
                        k.op("pe", lambda e: e.matmul(pq[:, gg * 128:(gg + 1) * 128], lhsT=Wpq[:, kc, g * 128:(g + 1) * 128], rhs=h2T[:, kc, :],
                                                      start=(kc == 0), stop=(kc == 7)), [Wpq, h2T], [pq])
                k.op("act", lambda e: e.activation(out=qTb[:, g4 * 4:(g4 + 1) * 4, :], in_=pq[:, :].rearrange("p (g t) -> p g t", t=128),
                                                   func=AF.Copy), [pq], [qTb])
                yield
            for g in range(16):
                k.op("pe", lambda e: e.matmul(ps_s[:, g, :], lhsT=qTb[:, g, :], rhs=skT[:, g, :], start=True, stop=True), [qTb, skT], [ps_s.p[g // 4]])
                yield
            for q4 in range(4):
                k.op("act", lambda e: e.activation(out=s_sb[:, q4 * 4:(q4 + 1) * 4, :], in_=ps_s[:, q4 * 4:(q4 + 1) * 4, :], func=AF.Copy),
                     [ps_s.p[q4]], [s_sb])
                yield
            for g in range(16):
                k.op("dve", lambda e: e.max(out=tv[:, g, 0:8], in_=s_sb[:, g, :]), [s_sb], [tv])
                yield
                k.op("dve", lambda e: e.max_index(out=ti[:, g, 0:8], in_max=tv[:, g, 0:8], in_values=s_sb[:, g, :]), [tv, s_sb], [ti])
                yield
                k.op("dve", lambda e: e.match_replace(out=s_sb[:, g, :], in_to_replace=tv[:, g, 0:8], in_values=s_sb[:, g, :], imm_value=-1e30),
                     [tv, s_sb], [s_sb])
                yield
                k.op("dve", lambda e: e.max(out=tv[:, g, 8:16], in_=s_sb[:, g, :]), [s_sb], [tv])
                yield
                k.op("dve", lambda e: e.max_index(out=ti[:, g, 8:16], in_max=tv[:, g, 8:16], in_values=s_sb[:, g, :]), [tv, s_sb], [ti])
                yield
            k.op("dve", lambda e: e.tensor_copy(tif[:], ti[:]), [ti], [tif])
            yield
            cs4 = cs_.t[:, :, :].rearrange("p h (a b) -> p h a b", b=16)
            k.op("dve", lambda e: e.tensor_tensor(out=cs4, in0=tv4[:, :, 0, :].unsqueeze(3).to_broadcast([128, 8, 16, 16]),
                                                  in1=tv4[:, :, 1, :].unsqueeze(2).to_broadcast([128, 8, 16, 16]), op=ALU.add), [tv], [cs_])
            yield
            for h in range(8):
                k.op("dve", lambda e: e.max(out=bs_[:, h, 0:8], in_=cs_[:, h, :]), [cs_], [bs_])
                yield
                k.op("dve", lambda e: e.max_index(out=pos[:, h, 0:8], in_max=bs_[:, h, 0:8], in_values=cs_[:, h, :]), [bs_, cs_], [pos])
                yield
                k.op("dve", lambda e: e.match_replace(out=cs_[:, h, :], in_to_replace=bs_[:, h, 0:8], in_values=cs_[:, h, :], imm_value=-1e30),
                     [bs_, cs_], [cs_])
                yield
                k.op("dve", lambda e: e.max(out=bs_[:, h, 8:16], in_=cs_[:, h, :]), [cs_], [bs_])
                yield
                k.op("dve", lambda e: e.max_index(out=pos[:, h, 8:16], in_max=bs_[:, h, 8:16], in_values=cs_[:, h, :]), [bs_, cs_], [pos])
                yield
            k.op("dve", lambda e: e.tensor_copy(posf[:], pos[:]), [pos], [posf])
            yield
            k.op("dve", lambda e: e.tensor_tensor(out=eq[:], in0=posf.t[:, :, :].unsqueeze(3).to_broadcast([128, 8, 16, 16]),
                                                  in1=THR.unsqueeze(1).unsqueeze(1).to_broadcast([128, 8, 16, 16]), op=ALU.is_ge), [posf, cmat], [eq])
            yield
            k.op("dve", lambda e: e.tensor_reduce(out=pa_[:], in_=eq[:], axis=AX.X, op=ALU.add), [eq], [pa_])
            yield
            k.op("dve", lambda e: e.scalar_tensor_tensor(out=pb_[:], in0=pa_[:], scalar=-16.0, in1=posf[:], op0=ALU.mult, op1=ALU.add),
                 [pa_, posf], [pb_])
            yield
            for (pp_, half, dst) in ((pa_, 0, i1), (pb_, 1, i2)):
                k.op("dve", lambda e: e.tensor_tensor(out=eq[:], in0=pp_.t[:, :, :].unsqueeze(3).to_broadcast([128, 8, 16, 16]),
                                                      in1=IOTA.unsqueeze(1).unsqueeze(1).to_broadcast([128, 8, 16, 16]), op=ALU.is_equal),
                     [pp_, cmat], [eq])
                yield
                k.op("dve", lambda e: e.tensor_tensor(out=eq[:], in0=eq[:], in1=tif4[:, :, half, :].unsqueeze(2).to_broadcast([128, 8, 16, 16]),
                                                      op=ALU.mult), [eq, tif], [eq])
                yield
                k.op("dve", lambda e: e.tensor_reduce(out=dst[:], in_=eq[:], axis=AX.X, op=ALU.add), [eq], [dst])
                yield
            k.op("dve", lambda e: e.scalar_tensor_tensor(out=idxf[:, :].rearrange("p (h k) -> p h k", k=16), in0=i1[:], scalar=128.0, in1=i2[:],
                                                         op0=ALU.mult, op1=ALU.add), [i1, i2], [idxf])
            yield
            k.op("dve", lambda e: e.tensor_copy(idxu[:], idxf[:]), [idxf], [idxu])
            yield
            k.op("dve", lambda e: e.tensor_tensor(out=gate[:], in0=bs_[:], in1=bs_[:, :, 0:1].to_broadcast([128, 8, 16]), op=ALU.subtract), [bs_], [gate])
            yield
            k.op("act", lambda e: e.activation(out=gate[:], in_=gate[:], func=AF.Exp), [gate], [gate])
            yield
            k.op("dve", lambda e: e.tensor_reduce(out=gsm[:, 0:8], in_=gate[:], axis=AX.X, op=ALU.add), [gate], [gsm])
            yield
            k.op("dve", lambda e: e.reciprocal(out=gsm[:, 8:16], in_=gsm[:, 0:8]), [gsm], [gsm])
            yield
            k.op("dve", lambda e: e.tensor_tensor(out=gate[:], in0=gate[:], in1=gsm[:, 8:16].unsqueeze(2).to_broadcast([128, 8, 16]), op=ALU.mult),
                 [gate, gsm], [gate])
            yield

            yield

        def tile_slots(tile_i):
            pb_i = tile_i % 2
            x1p, h2n, idxu, gate, gatef = x1p2[pb_i], h2n2[pb_i], idxu2[pb_i], gate2[pb_i], gatef2[pb_i]
            h2b = h2b2[pb_i]
            r0 = tile_i * 128
            def vside(gr):
                lf = gr % 8
                c0_, c1_ = gr * GS, (gr + 1) * GS
                for sl in range(c0_, c1_):
                    j = sl % NG
                    k.op("act", lambda e: e.activation(out=gw[:, sl:sl + 1], in_=gcol[:, sl:sl + 1], func=AF.Copy, scale=gatef[:, sl:sl + 1]),
                         [gcol.p[lf], gate], [gw.p[lf]])
                    k.op("act", lambda e: e.activation(out=Dm[:, j, :], in_=ident_bf[:], func=AF.Copy, scale=gw[:, sl:sl + 1]),
                         [ident_bf, gw.p[lf]], [Dm.p[j]])
                    k.op("pe", lambda e: e.matmul(py0[:], lhsT=Dm[:, j, :], rhs=UVg[:, j, D:D + 512], start=(sl == 0), stop=(sl == 127)),
                         [Dm.p[j], UVg.p[j]], [py0])
                    k.op("pe", lambda e: e.matmul(py1[:], lhsT=Dm[:, j, :], rhs=UVg[:, j, D + 512:2 * D], start=(sl == 0), stop=(sl == 127)),
                         [Dm.p[j], UVg.p[j]], [py1])

            for gr in range(NGR):
                lf = gr % 8
                for sl in range(gr * GS, (gr + 1) * GS):
                    j = sl % NG
                    k.dma("pool", None, None, reads=[idxu, euvb], writes=[UVg.p[j]],
                          fn=lambda e: e.indirect_dma_start(out=UVg[:, j, :], out_offset=None, in_=euv_v[:, :],
                                                            in_offset=bass.IndirectOffsetOnAxis(ap=idxu[:, sl:sl + 1], axis=0)))
                    if sl % 3 == 2:
                        pj = (sl // 3) % 2
                        k.op("dve", lambda e: e.tensor_tensor(out=prodb[:, pj, :], in0=UVg[:, j, 0:D], in1=h2b[:], op=ALU.mult),
                             [UVg.p[j], h2b], [prodb.p[pj]])
                        k.op("act", lambda e: e.activation(out=junk[:], in_=prodb[:, pj, :], func=AF.Copy, accum_out=hpre[:, sl:sl + 1]),
                             [prodb.p[pj]], [junk, hpre.p[sl % 32]])
                    else:
                        k.op("dve", lambda e: e.scalar_tensor_tensor(out=scrU[:], in0=UVg[:, j, 0:D], scalar=1.0, in1=h2n[:], op0=ALU.mult,
                                                                     op1=ALU.mult, accum_out=hpre[:, sl:sl + 1]),
                             [UVg.p[j], h2n], [scrU, hpre.p[sl % 32]])
                k.op("act", lambda e: e.activation(out=gcol[:, gr * GS:(gr + 1) * GS], in_=hpre[:, gr * GS:(gr + 1) * GS], func=AF.Gelu),
                     [hpre.p[(gr * GS) % 32:(gr * GS) % 32 + GS]], [gcol.p[lf]])
                vside(gr)
                yield
            k.op("dve", lambda e: e.tensor_tensor(out=x2t[:, 0:512], in0=py0[:], in1=x1p[:, 0:512], op=ALU.add), [py0, x1p], [x2t])
            k.op("dve", lambda e: e.tensor_tensor(out=x2t[:, 512:D], in0=py1[:], in1=x1p[:, 512:D], op=ALU.add), [py1, x1p], [x2t])
            if "x2" in dbg:
                dump("x2", x2t, x2t[:], dbg_d["x2"][r0:r0 + 128, :])
            k.op("act", lambda e: e.activation(out=junk[:], in_=x2t[:], func=AF.Square, accum_out=st[:, 2:3]), [x2t], [junk, st.p[1]])
            k.op("act", lambda e: e.activation(out=st[:, 3:4], in_=st[:, 2:3], func=AF.Sqrt, bias=epsc, scale=1.0 / D), [st.p[1], cder], [st.p[1]])
            k.op("dve", lambda e: e.reciprocal(out=st[:, 3:4], in_=st[:, 3:4]), [st.p[1]], [st.p[1]])
            k.op("dve", lambda e: e.scalar_tensor_tensor(out=obuf[:], in0=x2t[:], scalar=st[:, 3:4], in1=fnb[:], op0=ALU.mult, op1=ALU.mult),
                 [x2t, st.p[1], fnb], [obuf])
            k.dma("sp", out_d[r0:r0 + 128, :], obuf[:], reads=[obuf], writes=[out_d])

            yield

        fr = tile_front(0)
        for _ in fr:
            pass
        for tile_i in range(ntile3):
            bg = tile_front(tile_i + 1) if tile_i + 1 < ntile3 else None
            for _ in tile_slots(tile_i):
                if bg is not None:
                    for _n in range(BG_PER_STEP):
                        if next(bg, 'end') == 'end':
                            bg = None
                            break
            if bg is not None:
                for _ in bg:
                    pass

    k.finish()
    print("instructions:", k.n_ins, "dma sems:", len(k.dsems))
    return nc, es


def host_prep(inputs):
    x = np.asarray(inputs["x"], np.float32)
    w_in = np.asarray(inputs["w_in"], np.float32)[0]
    conv = np.asarray(inputs["conv_a"], np.float32)[0]
    cmat = np.zeros((128, NCM), np.float32)
    cmat[:, CM_ID:CM_ID + 128] = np.eye(128, dtype=np.float32)
    i = np.arange(64)
    cmat[0:64, CM_ML:CM_ML + 64] = (i[:, None] > i[None, :])
    cmat[0:64, CM_MUS:CM_MUS + 64] = (i[:, None] < i[None, :])
    cmat[0:64, CM_MUI:CM_MUI + 64] = (i[:, None] <= i[None, :])
    cmat[0:64, CM_I64:CM_I64 + 64] = np.eye(64, dtype=np.float32)
    cmat[:, CM_RST:CM_RST + 512] = (np.arange(512) % 64 != 0)[None, :]
    cmat[:, CM_ONE:CM_ONE + 128] = 1.0
    cmat[:, CM_IOTA:CM_IOTA + 16] = np.arange(16, dtype=np.float32)[None, :]
    thr = 16.0 * (np.arange(16, dtype=np.float32) + 1.0)
    thr[15] = 1e9
    cmat[:, CM_THR:CM_THR + 16] = thr[None, :]
    offs = {CB_AQ: 0, CB_AK: 1024, CB_AV: 2048, CB_AGATE: 3088, CB_BF: 4112, CB_BI: 5136, CB_BQ: 6160, CB_BGATE: 7184}
    w_pq = np.asarray(inputs["w_pq"], np.float32)[0]
    sk = np.asarray(inputs["sub_keys"], np.float32)[0]
    skT = np.ascontiguousarray(sk.transpose(3, 1, 0, 2).reshape(128, 16 * 128))
    shared = {
        "wG": np.ascontiguousarray(w_in[:, 8208:8208 + 2048]),
        "wBa": np.ascontiguousarray(inputs["w_branch_a"][0]), "wBb": np.ascontiguousarray(inputs["w_branch_b"][0]),
        "wO": np.ascontiguousarray(inputs["w_out"][0]), "wPQ": w_pq, "skT": skT,
        "n2b": np.ascontiguousarray(np.broadcast_to(inputs["norm2"][0][None, :], (128, D))),
        "fnb": np.ascontiguousarray(np.broadcast_to(inputs["final_norm"][None, :], (128, D))),
        "euv": np.concatenate([inputs["expert_u"][0], inputs["expert_v"][0]], axis=1),
    }
    maps = []
    for c in range(NCORE):
        b, hp = c // 4, c % 4
        wA = np.empty((D, NWA), np.float32)
        cvec = np.zeros((128, NCV), np.float32)
        cvec[:, CV_N1:CV_N1 + 8] = inputs["norm1"][0].reshape(8, 128).T
        cvec[:, CV_N2:CV_N2 + 8] = inputs["norm2"][0].reshape(8, 128).T
        cvec[:, CV_AON] = inputs["a_onorm"][0]
        cvec[:, CV_BON] = inputs["b_onorm"][0]
        for hl in range(2):
            h = 2 * hp + hl
            for cb, o in offs.items():
                wA[:, hl * 1280 + cb * 128: hl * 1280 + (cb + 1) * 128] = w_in[:, o + 128 * h: o + 128 * (h + 1)]
            wA[:, hl * 1280 + CB_ABETA * 128: hl * 1280 + (CB_ABETA + 1) * 128] = w_in[:, 3072 + h: 3073 + h]
            wA[:, hl * 1280 + CB_AALPHA * 128: hl * 1280 + (CB_AALPHA + 1) * 128] = w_in[:, 3080 + h: 3081 + h]
            for qi in range(3):
                cvec[:, CV_CONV + hl * 12 + qi * 4: CV_CONV + hl * 12 + qi * 4 + 4] = conv[:, qi * 1024 + 128 * h: qi * 1024 + 128 * (h + 1)].T
            cvec[:, CV_ALOG + hl] = inputs["a_log"][0, h]
            cvec[:, CV_DTB + hl] = inputs["dt_bias"][0, h]
            cvec[:, CV_BLB + 2 * hl] = inputs["b_lower_bound"][0, 128 * h:128 * (h + 1)]
            cvec[:, CV_BLB + 2 * hl + 1] = inputs["b_lower_bound"][1, 128 * h:128 * (h + 1)]
        ts = c % 4
        sel = np.zeros((128, 4), np.float32)
        sel[:, ts] = 1.0
        m = {"x": np.ascontiguousarray(x[b]), "wA": wA, "cvec": cvec, "cmat": cmat,
             "x3": np.ascontiguousarray(x[b, ts * 2048:(ts + 1) * 2048]), "sel": sel}
        m.update(shared)
        maps.append(m)
    return maps


_CACHE = {}


def kernel(**inputs):
    inputs = {k_: np.asarray(v) for k_, v in inputs.items()}
    maps = host_prep(inputs)
    if "nc" not in _CACHE:
        _CACHE["nc"] = build()
    nc, _ = _CACHE["nc"]
    res = run_bass_kernel_spmd(nc, maps, core_ids=list(range(NCORE)))
    out = np.empty((2, SEQ, D), np.float32)
    for c in range(NCORE):
        b, ts = c // 4, c % 4
        out[b, ts * 2048:(ts + 1) * 2048] = res.results[c]["out"]
    return out
```
